# Optimizing a Trainium2 kernel written in Bass

```python
import jax
import jax.numpy as jnp
from jax import lax
import numpy as np

D_MODEL = 1024
BATCH = 8
SEQ = 2048
DEPTH = 4

GRID_W = 64
CTX_LEN = 256
N_MIXERS = 4
N_LAYERS_NA = (DEPTH + 3) // 4
N_LAYERS_ML = (DEPTH + 2) // 4
N_LAYERS_SW = (DEPTH + 1) // 4
N_LAYERS_GL = DEPTH // 4
RMS_EPS = 1e-6
NEG_INF = -1e30
ROPE_BASE = 10000.0
F32 = jnp.float32

NA_HEADS = 8
NA_HEAD_DIM = D_MODEL // NA_HEADS
NA_WIN_R = 8
NA_WIN_C = 16
ML_HEADS = 8
ML_DK = D_MODEL // ML_HEADS // 2
ML_DV = D_MODEL // ML_HEADS
ML_CHUNK = 64
ML_GATE_CAP = 15.0
ML_FGATE_BIAS = 3.0
ML_IN = ML_HEADS * (2 * ML_DK + 2 * ML_DV) + 4 * ML_HEADS
SW_HEADS = 16
SW_KV_HEADS = 4
SW_HEAD_DIM = D_MODEL // SW_HEADS
SW_WINDOW = 128
SW_BLOCK = 128
SW_IN = (SW_HEADS + 2 * SW_KV_HEADS) * SW_HEAD_DIM
GL_HEADS = 4
GL_DK = D_MODEL // 2 // GL_HEADS
GL_DV = D_MODEL // GL_HEADS
GL_GATE_RANK = 16
GL_GATE_TAU = 16.0
GL_CHUNK = 64
GL_IN = GL_HEADS * (2 * GL_DK + 2 * GL_DV) + 2 * GL_GATE_RANK
MOE_GROUPS = 4
MOE_PER_GROUP = 8
MOE_EXPERTS = MOE_GROUPS * MOE_PER_GROUP
MOE_TOPK = 2
MOE_FF = 512
MOE_BLOCK = 128

kernel_name = 'hybrid_nat_mlstm_swa_gla_hmoe_dit'


def rmsnorm(x, g):
    xf = x.astype(F32)
    y = xf * lax.rsqrt(jnp.mean(xf * xf, axis=-1, keepdims=True) + RMS_EPS)
    return (y * g.astype(F32)).astype(x.dtype)


def modulate(h, shift, scale):
    return h * (1.0 + scale) + shift


def flip_time(t, direction):
    return jnp.flip(t, axis=2) if direction == 1 else t


def rope_2d(x, rows, cols):
    hd = x.shape[-1]
    half = hd // 2
    nf = half // 2
    inv = ROPE_BASE ** (-jnp.arange(nf, dtype=F32) / nf)
    bshape = (x.shape[1],) + (1,) * (x.ndim - 3) + (nf,)
    xf = x.astype(F32)

    def rot(xa, p):
        ang = (p.astype(F32)[:, None] * inv).reshape(bshape)
        cs, sn = jnp.cos(ang), jnp.sin(ang)
        x1, x2 = xa[..., :nf], xa[..., nf:]
        return jnp.concatenate([x1 * cs - x2 * sn, x1 * sn + x2 * cs], axis=-1)

    return jnp.concatenate([rot(xf[..., :half], rows), rot(xf[..., half:], cols)], axis=-1).astype(x.dtype)


def ctx_self_attention(q, k, v, sink):
    s = jnp.einsum('bqhgd,bkhd->bhgqk', q, k, preferred_element_type=F32) * (q.shape[-1] ** -0.5)
    if sink is None:
        p = jax.nn.softmax(s, axis=-1)
    else:
        sk = jnp.broadcast_to(sink[None, :, :, None, None], s.shape[:-1] + (1,))
        p = jax.nn.softmax(jnp.concatenate([sk, s], axis=-1), axis=-1)[..., 1:]
    return jnp.einsum('bhgqk,bkhd->bqhgd', p.astype(v.dtype), v)


def neighbourhood_attention(hc, hl, w_qkv, qk_g, rpb, w_o, need_ctx_out):
    bsz, seq, _ = hl.shape
    lc = hc.shape[1]
    H, hd = NA_HEADS, NA_HEAD_DIM
    n_rows = seq // GRID_W
    wr = min(NA_WIN_R, n_rows)
    scale = hd ** -0.5

    def proj(h):
        qkv = (h @ w_qkv).reshape(h.shape[0], h.shape[1], 3, H, hd)
        return rmsnorm(qkv[:, :, 0], qk_g[0]), rmsnorm(qkv[:, :, 1], qk_g[1]), qkv[:, :, 2]

    qc, kc, vc = proj(hc)
    ql, kl, vl = proj(hl)
    kg = kl.reshape(bsz, n_rows, GRID_W, H, hd)
    vg = vl.reshape(bsz, n_rows, GRID_W, H, hd)
    cq = jnp.arange(GRID_W)[:, None]
    ck = jnp.arange(GRID_W)[None, :]
    c0 = jnp.clip(cq - NA_WIN_C // 2, 0, GRID_W - NA_WIN_C)
    col_ok = (ck >= c0) & (ck < c0 + NA_WIN_C)
    dc = jnp.clip(ck - cq + NA_WIN_C - 1, 0, 2 * NA_WIN_C - 2)
    rpb_c = rpb[:, :, dc].astype(F32)

    def row_block(args):
        q_r, r = args
        r0 = jnp.clip(r - wr // 2, 0, n_rows - wr)
        k_b = lax.dynamic_slice_in_dim(kg, r0, wr, axis=1)
        v_b = lax.dynamic_slice_in_dim(vg, r0, wr, axis=1)
        bias = jnp.transpose(rpb_c[:, r0 + jnp.arange(wr) - r + NA_WIN_R - 1], (0, 2, 1, 3))
        s_nb = jnp.einsum('bqhd,brkhd->bhqrk', q_r, k_b, preferred_element_type=F32) * scale + bias
        s_nb = jnp.where(col_ok[:, None, :], s_nb, NEG_INF)
        s_cx = jnp.einsum('bqhd,bkhd->bhqk', q_r, kc, preferred_element_type=F32) * scale
        logits = jnp.concatenate([s_nb.reshape(bsz, H, GRID_W, wr * GRID_W), s_cx], axis=-1)
        p = jax.nn.softmax(logits, axis=-1).astype(v_b.dtype)
        p_nb = p[..., :wr * GRID_W].reshape(bsz, H, GRID_W, wr, GRID_W)
        return (jnp.einsum('bhqrk,brkhd->bqhd', p_nb, v_b)
                + jnp.einsum('bhqk,bkhd->bqhd', p[..., wr * GRID_W:], vc))

    q_rows = jnp.moveaxis(ql.reshape(bsz, n_rows, GRID_W, H, hd), 1, 0)
    o_rows = lax.map(row_block, (q_rows, jnp.arange(n_rows)))
    ol = jnp.moveaxis(o_rows, 0, 1).reshape(bsz, seq, H * hd) @ w_o
    oc = None
    if need_ctx_out:
        oc = ctx_self_attention(qc[:, :, :, None], kc, vc, None).reshape(bsz, lc, H * hd) @ w_o
    return oc, ol


def mlstm_chunkwise(q, k, v, log_i, log_f, state, need_out):
    bsz, H, L, dk = q.shape
    dv = v.shape[-1]
    nc, lc = L // ML_CHUNK, ML_CHUNK
    q = q.reshape(bsz, H, nc, lc, dk) * (dk ** -0.5)
    k = k.reshape(bsz, H, nc, lc, dk)
    v = v.reshape(bsz, H, nc, lc, dv)
    li = log_i.reshape(bsz, H, nc, lc)
    bf = jnp.cumsum(log_f.reshape(bsz, H, nc, lc), axis=-1)
    g = bf[..., -1]
    a = g[..., None] - bf + li
    m_loc = jnp.max(a, axis=-1)
    w = jnp.exp(a - m_loc[..., None])
    c_loc = jnp.einsum('bhcsv,bhcsk->bhcvk', v * w[..., None], k)
    n_loc = jnp.einsum('bhcs,bhcsk->bhck', w, k)

    def step(carry, xs):
        c_st, n_st, m_st = carry
        g_c, m_c, c_c, n_c = xs
        m_new = jnp.maximum(g_c + m_st, m_c)
        dec = jnp.exp(g_c + m_st - m_new)
        inc = jnp.exp(m_c - m_new)
        new = (dec[..., None, None] * c_st + inc[..., None, None] * c_c,
               dec[..., None] * n_st + inc[..., None] * n_c, m_new)
        return new, (c_st, n_st, m_st)

    xs = tuple(jnp.moveaxis(t, 2, 0) for t in (g, m_loc, c_loc, n_loc))
    final, (c_s, n_s, m_s) = lax.scan(step, state, xs)
    if not need_out:
        return None, final
    c_s, n_s, m_s = (jnp.moveaxis(t, 0, 2) for t in (c_s, n_s, m_s))
    tri = jnp.tril(jnp.ones((lc, lc), bool))
    dmat = jnp.where(tri, bf[..., :, None] - bf[..., None, :] + li[..., None, :], NEG_INF)
    inter = bf + m_s[..., None]
    m_t = jnp.maximum(inter, jnp.max(dmat, axis=-1))
    sc = jnp.einsum('bhctd,bhcsd->bhcts', q, k) * jnp.exp(dmat - m_t[..., None])
    e_inter = jnp.exp(inter - m_t)
    num = (jnp.einsum('bhcts,bhcsv->bhctv', sc, v)
           + e_inter[..., None] * jnp.einsum('bhctk,bhcvk->bhctv', q, c_s))
    den = jnp.sum(sc, axis=-1) + e_inter * jnp.einsum('bhctk,bhck->bhct', q, n_s)
    h = num / jnp.maximum(jnp.abs(den), jnp.exp(-m_t))[..., None]
    return h.reshape(bsz, H, L, dv), final


def mlstm_mixer(hc, hl, w_in, b_gates, norm_g, w_o, need_ctx_out):
    H, dk, dv = ML_HEADS, ML_DK, ML_DV
    nq, nv = H * dk, H * dv

    def proj(h):
        b_, l_ = h.shape[:2]
        u = h @ w_in
        uf = u[..., :2 * nq + nv].astype(F32)
        q = uf[..., :nq].reshape(b_, l_, H, dk).transpose(0, 2, 1, 3)
        k = uf[..., nq:2 * nq].reshape(b_, l_, H, dk).transpose(0, 2, 1, 3)
        v = uf[..., 2 * nq:].reshape(b_, l_, H, dv).transpose(0, 2, 1, 3)
        o_gate = jax.nn.sigmoid(u[..., 2 * nq + nv:2 * nq + 2 * nv])
        pre = (u[..., 2 * nq + 2 * nv:] + b_gates).astype(F32)
        pre = ML_GATE_CAP * jnp.tanh(pre / ML_GATE_CAP)
        pre = pre.reshape(b_, l_, 2, 2, H).transpose(2, 3, 0, 4, 1)
        return q, k, v, pre[:, 0], jax.nn.log_sigmoid(pre[:, 1]), o_gate

    cq, ck, cv, cli, clf, cog = proj(hc)
    lq, lk, lv, lli, llf, log_ = proj(hl)
    bsz = hc.shape[0]
    zero = (jnp.zeros((bsz, H, dv, dk), F32), jnp.zeros((bsz, H, dk), F32), jnp.zeros((bsz, H), F32))
    h_c, h_l = 0.0, 0.0
    for d in range(2):
        oc, st = mlstm_chunkwise(*[flip_time(t, d) for t in (cq, ck, cv, cli[d], clf[d])], zero, need_ctx_out)
        ol, _ = mlstm_chunkwise(*[flip_time(t, d) for t in (lq, lk, lv, lli[d], llf[d])], st, True)
        h_l = h_l + flip_time(ol, d)
        if need_ctx_out:
            h_c = h_c + flip_time(oc, d)

    def readout(hs, og):
        b_, _, l_, _ = hs.shape
        y = rmsnorm(hs.transpose(0, 2, 1, 3), norm_g.reshape(H, dv)).reshape(b_, l_, nv)
        return (y.astype(og.dtype) * og) @ w_o

    oc_out = readout(h_c, cog) if need_ctx_out else None
    return oc_out, readout(h_l, log_)


def window_gqa_attention(hc, hl, w_qkv, qk_g, sink, w_o, rows, cols, need_ctx_out):
    bsz, seq, _ = hl.shape
    lc = hc.shape[1]
    Hq, Hk, hd = SW_HEADS, SW_KV_HEADS, SW_HEAD_DIM
    G = Hq // Hk
    scale = hd ** -0.5

    def proj(h):
        b_, l_ = h.shape[:2]
        qkv = h @ w_qkv
        q = rmsnorm(qkv[..., :Hq * hd].reshape(b_, l_, Hk, G, hd), qk_g[0])
        k = rmsnorm(qkv[..., Hq * hd:(Hq + Hk) * hd].reshape(b_, l_, Hk, hd), qk_g[1])
        v = qkv[..., (Hq + Hk) * hd:].reshape(b_, l_, Hk, hd)
        return q, k, v

    qc, kc, vc = proj(hc)
    ql, kl, vl = proj(hl)
    ql = rope_2d(ql, rows, cols)
    kl = rope_2d(kl, rows, cols)
    sink_hg = sink.reshape(Hk, G).astype(F32)
    pad = ((0, 0), (SW_WINDOW, SW_WINDOW), (0, 0), (0, 0))
    kp = jnp.pad(kl, pad)
    vp = jnp.pad(vl, pad)
    n_keys = SW_BLOCK + 2 * SW_WINDOW
    n_blk = seq // SW_BLOCK

    def block(args):
        q_b, jb = args
        k_b = lax.dynamic_slice_in_dim(kp, jb * SW_BLOCK, n_keys, axis=1)
        v_b = lax.dynamic_slice_in_dim(vp, jb * SW_BLOCK, n_keys, axis=1)
        qpos = jb * SW_BLOCK + jnp.arange(SW_BLOCK)
        kpos = jb * SW_BLOCK - SW_WINDOW + jnp.arange(n_keys)
        ok = (jnp.abs(qpos[:, None] - kpos[None, :]) <= SW_WINDOW) & (kpos >= 0) & (kpos < seq)
        s_w = jnp.einsum('bqhgd,bkhd->bhgqk', q_b, k_b, preferred_element_type=F32) * scale
        s_w = jnp.where(ok, s_w, NEG_INF)
        s_c = jnp.einsum('bqhgd,bkhd->bhgqk', q_b, kc, preferred_element_type=F32) * scale
        sk = jnp.broadcast_to(sink_hg[None, :, :, None, None], s_w.shape[:-1] + (1,))
        p = jax.nn.softmax(jnp.concatenate([sk, s_w, s_c], axis=-1), axis=-1).astype(v_b.dtype)
        return (jnp.einsum('bhgqk,bkhd->bqhgd', p[..., 1:1 + n_keys], v_b)
                + jnp.einsum('bhgqk,bkhd->bqhgd', p[..., 1 + n_keys:], vc))

    q_blocks = jnp.moveaxis(ql.reshape(bsz, n_blk, SW_BLOCK, Hk, G, hd), 1, 0)
    o = lax.map(block, (q_blocks, jnp.arange(n_blk)))
    ol = jnp.moveaxis(o, 0, 1).reshape(bsz, seq, Hq * hd) @ w_o
    oc = None
    if need_ctx_out:
        oc = ctx_self_attention(qc, kc, vc, sink_hg).reshape(bsz, lc, Hq * hd) @ w_o
    return oc, ol


def gla_chunkwise(q, k, v, log_a, state, need_out):
    bsz, H, L, dk = q.shape
    dv = v.shape[-1]
    nc, lc = L // GL_CHUNK, GL_CHUNK
    q = q.reshape(bsz, H, nc, lc, dk) * (dk ** -0.5)
    k = k.reshape(bsz, H, nc, lc, dk)
    v = v.reshape(bsz, H, nc, lc, dv)
    bc = jnp.cumsum(log_a.reshape(bsz, H, nc, lc, dk), axis=3)
    g = bc[:, :, :, -1]
    s_loc = jnp.einsum('bhcsk,bhcsv->bhckv', k * jnp.exp(g[:, :, :, None, :] - bc), v)

    def step(s_st, xs):
        g_c, s_c = xs
        return jnp.exp(g_c)[..., None] * s_st + s_c, s_st

    final, s_s = lax.scan(step, state, (jnp.moveaxis(g, 2, 0), jnp.moveaxis(s_loc, 2, 0)))
    if not need_out:
        return None, final
    s_s = jnp.moveaxis(s_s, 0, 2)
    q_t = q * jnp.exp(bc)
    k_t = k * jnp.exp(-bc)
    tri = jnp.tril(jnp.ones((lc, lc), bool))
    att = jnp.where(tri, jnp.einsum('bhctk,bhcsk->bhcts', q_t, k_t), 0.0)
    o = jnp.einsum('bhcts,bhcsv->bhctv', att, v) + jnp.einsum('bhctk,bhckv->bhctv', q_t, s_s)
    return o.reshape(bsz, H, L, dv), final


def gla_mixer(hc, hl, w_in, w_a2, b_a, norm_g, w_o, need_ctx_out):
    H, dk, dv, rk = GL_HEADS, GL_DK, GL_DV, GL_GATE_RANK
    nq, nv = H * dk, H * dv

    def proj(h):
        b_, l_ = h.shape[:2]
        u = h @ w_in
        uf = u[..., :2 * nq + nv].astype(F32)
        q = uf[..., :nq].reshape(b_, l_, H, dk).transpose(0, 2, 1, 3)
        k = uf[..., nq:2 * nq].reshape(b_, l_, H, dk).transpose(0, 2, 1, 3)
        v = uf[..., 2 * nq:].reshape(b_, l_, H, dv).transpose(0, 2, 1, 3)
        gate = jax.nn.silu(u[..., 2 * nq + nv:2 * nq + 2 * nv])
        z = u[..., 2 * nq + 2 * nv:].reshape(b_, l_, 2, rk)
        z = jnp.einsum('blxr,xrk->xblk', z, w_a2) + b_a[:, None, None, :]
        log_a = (jax.nn.log_sigmoid(z.astype(F32)) / GL_GATE_TAU).reshape(2, b_, l_, H, dk).transpose(0, 1, 3, 2, 4)
        return q, k, v, log_a, gate

    cq, ck, cv, cla, cgate = proj(hc)
    lq, lk, lv, lla, lgate = proj(hl)
    bsz = hc.shape[0]
    zero = jnp.zeros((bsz, H, dk, dv), F32)
    h_c, h_l = 0.0, 0.0
    for d in range(2):
        oc, st = gla_chunkwise(*[flip_time(t, d) for t in (cq, ck, cv, cla[d])], zero, need_ctx_out)
        ol, _ = gla_chunkwise(*[flip_time(t, d) for t in (lq, lk, lv, lla[d])], st, True)
        h_l = h_l + flip_time(ol, d)
        if need_ctx_out:
            h_c = h_c + flip_time(oc, d)

    def readout(hs, gate):
        b_, _, l_, _ = hs.shape
        y = rmsnorm(hs.transpose(0, 2, 1, 3), norm_g.reshape(H, dv)).reshape(b_, l_, nv)
        return (y.astype(gate.dtype) * gate) @ w_o

    oc_out = readout(h_c, cgate) if need_ctx_out else None
    return oc_out, readout(h_l, lgate)


def hierarchical_moe(h, w_grp, b_grp, w_exp, b_exp, w_gate, w_up, w_down):
    T, D = h.shape
    lg = (h @ w_grp).astype(F32) + b_grp.astype(F32)
    g_idx = jnp.argmax(lg, axis=-1)
    g_w = jnp.take_along_axis(jax.nn.softmax(lg, axis=-1), g_idx[:, None], axis=-1)
    le = ((h @ w_exp).astype(F32) + b_exp.astype(F32)).reshape(T, MOE_GROUPS, MOE_PER_GROUP)
    le = jnp.take_along_axis(le, g_idx[:, None, None], axis=1)[:, 0]
    top_v, top_i = lax.top_k(le, MOE_TOPK)
    wts = (jax.nn.softmax(top_v, axis=-1) * g_w).reshape(-1)
    eid = (g_idx[:, None] * MOE_PER_GROUP + top_i).reshape(-1)
    tok = jnp.repeat(jnp.arange(T, dtype=jnp.int32), MOE_TOPK)
    n = T * MOE_TOPK
    order = jnp.argsort(eid)
    e_sorted = eid[order]
    counts = jnp.bincount(eid, length=MOE_EXPERTS)
    padded = (counts + MOE_BLOCK - 1) // MOE_BLOCK * MOE_BLOCK
    p_end = jnp.cumsum(padded)
    p_start = p_end - padded
    start = jnp.cumsum(counts) - counts
    dest = p_start[e_sorted] + jnp.arange(n) - start[e_sorted]
    n_slots = -(-(n + MOE_EXPERTS * (MOE_BLOCK - 1)) // MOE_BLOCK) * MOE_BLOCK
    slot_tok = jnp.full((n_slots,), T, jnp.int32).at[dest].set(tok[order])
    slot_w = jnp.zeros((n_slots,), F32).at[dest].set(wts[order])
    n_blk = n_slots // MOE_BLOCK
    blk_e = jnp.minimum(jnp.searchsorted(p_end, jnp.arange(n_blk) * MOE_BLOCK, side='right'), MOE_EXPERTS - 1)
    xb = jnp.take(h, slot_tok, axis=0, mode='clip').reshape(n_blk, MOE_BLOCK, D)

    def expert_block(args):
        xe, e = args
        return (jax.nn.silu(xe @ w_gate[e]) * (xe @ w_up[e])) @ w_down[e]

    y = lax.map(expert_block, (xb, blk_e)).reshape(n_slots, D)
    return jnp.zeros_like(h).at[slot_tok].add(y * slot_w[:, None].astype(y.dtype), mode='drop')


def setup_inputs(seed: int = 0) -> dict:
    key = jax.random.key(seed)
    keys = iter(list(jax.random.split(key, 40)))

    def nrm(shape, scale):
        return jax.random.normal(next(keys), shape, F32) * scale

    def gain(shape):
        return 1.0 + nrm(shape, 0.02)

    D = D_MODEL
    din = D ** -0.5
    ml_b = nrm((N_LAYERS_ML, 2, 2, ML_HEADS), 0.1) + jnp.array([0.0, ML_FGATE_BIAS], F32)[:, None]
    return {
        'x': nrm((BATCH, SEQ, D), 1.0),
        'c': nrm((BATCH, D), 1.0),
        'ctx': nrm((BATCH, CTX_LEN, D), 1.0),
        'c_ctx': nrm((D,), 1.0),
        'ada_w': nrm((DEPTH, D, 6 * D), 0.5 * din),
        'ada_b': nrm((DEPTH, 6 * D), 0.02),
        'norm_mix_g': gain((DEPTH, D)),
        'norm_ffn_g': gain((DEPTH, D)),
        'na_w_qkv': nrm((N_LAYERS_NA, D, 3 * NA_HEADS * NA_HEAD_DIM), din),
        'na_qk_g': gain((N_LAYERS_NA, 2, NA_HEAD_DIM)),
        'na_rpb': nrm((N_LAYERS_NA, NA_HEADS, 2 * NA_WIN_R - 1, 2 * NA_WIN_C - 1), 0.2),
        'na_w_o': nrm((N_LAYERS_NA, NA_HEADS * NA_HEAD_DIM, D), din),
        'ml_w_in': nrm((N_LAYERS_ML, D, ML_IN), din),
        'ml_b_gates': ml_b.reshape(N_LAYERS_ML, 4 * ML_HEADS),
        'ml_norm_g': gain((N_LAYERS_ML, ML_HEADS * ML_DV)),
        'ml_w_o': nrm((N_LAYERS_ML, ML_HEADS * ML_DV, D), (ML_HEADS * ML_DV) ** -0.5),
        'sw_w_qkv': nrm((N_LAYERS_SW, D, SW_IN), din),
        'sw_qk_g': gain((N_LAYERS_SW, 2, SW_HEAD_DIM)),
        'sw_sink': nrm((N_LAYERS_SW, SW_HEADS), 0.5),
        'sw_w_o': nrm((N_LAYERS_SW, SW_HEADS * SW_HEAD_DIM, D), din),
        'gl_w_in': nrm((N_LAYERS_GL, D, GL_IN), din),
        'gl_w_a2': nrm((N_LAYERS_GL, 2, GL_GATE_RANK, GL_HEADS * GL_DK), GL_GATE_RANK ** -0.5),
        'gl_b_a': nrm((N_LAYERS_GL, 2, GL_HEADS * GL_DK), 0.1),
        'gl_norm_g': gain((N_LAYERS_GL, GL_HEADS * GL_DV)),
        'gl_w_o': nrm((N_LAYERS_GL, GL_HEADS * GL_DV, D), (GL_HEADS * GL_DV) ** -0.5),
        'moe_w_grp': nrm((DEPTH, D, MOE_GROUPS), din),
        'moe_b_grp': nrm((DEPTH, MOE_GROUPS), 0.01),
        'moe_w_exp': nrm((DEPTH, D, MOE_EXPERTS), din),
        'moe_b_exp': nrm((DEPTH, MOE_EXPERTS), 0.01),
        'moe_w_gate': nrm((DEPTH, MOE_EXPERTS, D, MOE_FF), din),
        'moe_w_up': nrm((DEPTH, MOE_EXPERTS, D, MOE_FF), din),
        'moe_w_down': nrm((DEPTH, MOE_EXPERTS, MOE_FF, D), MOE_FF ** -0.5),
    }


def reference(x, c, ctx, c_ctx, ada_w, ada_b, norm_mix_g, norm_ffn_g,
              na_w_qkv, na_qk_g, na_rpb, na_w_o,
              ml_w_in, ml_b_gates, ml_norm_g, ml_w_o,
              sw_w_qkv, sw_qk_g, sw_sink, sw_w_o,
              gl_w_in, gl_w_a2, gl_b_a, gl_norm_g, gl_w_o,
              moe_w_grp, moe_b_grp, moe_w_exp, moe_b_exp, moe_w_gate, moe_w_up, moe_w_down):
    bsz, seq, D = x.shape
    pos = jnp.arange(seq, dtype=jnp.int32)
    rows, cols = pos // GRID_W, pos % GRID_W
    cond_l = jax.nn.silu(c)
    cond_c = jax.nn.silu(c_ctx)[None]
    xl, xc = x, ctx
    for i in range(DEPTH):
        last = i == DEPTH - 1
        j = i // N_MIXERS
        kind = i % N_MIXERS
        mod_l = jnp.split((cond_l @ ada_w[i] + ada_b[i])[:, None, :], 6, axis=-1)
        mod_c = jnp.split((cond_c @ ada_w[i] + ada_b[i])[:, None, :], 6, axis=-1)
        hl = modulate(rmsnorm(xl, norm_mix_g[i]), mod_l[0], mod_l[1])
        hc = modulate(rmsnorm(xc, norm_mix_g[i]), mod_c[0], mod_c[1])
        if kind == 0:
            oc, ol = neighbourhood_attention(hc, hl, na_w_qkv[j], na_qk_g[j], na_rpb[j], na_w_o[j], not last)
        elif kind == 1:
            oc, ol = mlstm_mixer(hc, hl, ml_w_in[j], ml_b_gates[j], ml_norm_g[j], ml_w_o[j], not last)
        elif kind == 2:
            oc, ol = window_gqa_attention(hc, hl, sw_w_qkv[j], sw_qk_g[j], sw_sink[j], sw_w_o[j], rows, cols, not last)
        else:
            oc, ol = gla_mixer(hc, hl, gl_w_in[j], gl_w_a2[j], gl_b_a[j], gl_norm_g[j], gl_w_o[j], not last)
        xl = xl + mod_l[2] * ol
        fl = modulate(rmsnorm(xl, norm_ffn_g[i]), mod_l[3], mod_l[4])
        moe_args = (moe_w_grp[i], moe_b_grp[i], moe_w_exp[i], moe_b_exp[i], moe_w_gate[i], moe_w_up[i], moe_w_down[i])
        if last:
            yl = hierarchical_moe(fl.reshape(-1, D), *moe_args).reshape(bsz, seq, D)
        else:
            xc = xc + mod_c[2] * oc
            fc = modulate(rmsnorm(xc, norm_ffn_g[i]), mod_c[3], mod_c[4])
            n_ctx_tok = fc.shape[0] * fc.shape[1]
            y = hierarchical_moe(jnp.concatenate([fc.reshape(-1, D), fl.reshape(-1, D)], axis=0), *moe_args)
            xc = xc + mod_c[5] * y[:n_ctx_tok].reshape(fc.shape)
            yl = y[n_ctx_tok:].reshape(bsz, seq, D)
        xl = xl + mod_l[5] * yl
    return xl
```

```python
import contextlib
import numpy as np
import concourse.bass as bass
import concourse.mybir as mybir
from concourse.bass_utils import run_bass_kernel_spmd

F32 = mybir.dt.float32
BF16 = mybir.dt.bfloat16
AF = mybir.ActivationFunctionType
ALU = mybir.AluOpType
AX = mybir.AxisListType

D = 1024
NCH = 8
TC = 256
TL = 2048
T = TC + TL
SLABS = [(0, 256), (256, 512), (768, 512), (1280, 512), (1792, 512)]
NEG = -30000.0
N_DSEM = 48
U32 = mybir.dt.uint32
BS = 256
CAP = BS
NBLK_MAX = (2 * T + 32 * (BS - 1) + BS - 1) // BS
NSLOT = NBLK_MAX * BS
EXPERT_ELEMS = 1024 * 512
MOE_SPARSE = True
ROUTE_INTERLEAVE = True


class Buf:
    __slots__ = ("w", "r")

    def __init__(self):
        self.w = None
        self.r = {}


class KB:
    def __init__(self):
        self.nc = bass.Bass("TRN2", target_bir_lowering=False)
        nc = self.nc
        self.es = contextlib.ExitStack()
        self.eng = dict(pe=nc.tensor, act=nc.scalar, dve=nc.vector, pool=nc.gpsimd, sp=nc.sync)
        self.semobj = {}
        for e in ("pe", "act", "dve", "pool"):
            self.semobj[e] = self.es.enter_context(nc.semaphore("s_" + e))
        self.cnt = {e: 0 for e in ("pe", "act", "dve", "pool")}
        self.dval = [0] * N_DSEM
        for i in range(N_DSEM):
            self.semobj[("d", i)] = self.es.enter_context(nc.semaphore("d_%d" % i))
        self.dnext = 0
        self.waited = {e: {} for e in self.eng}
        self.nalloc = 0
        self.bound_reg = None

    def sb(self, stack, shape, dt, name=None):
        self.nalloc += 1
        return stack.enter_context(self.nc.sbuf_tensor("%s_%d" % (name or "t", self.nalloc), list(shape), dt))

    def psum(self, stack, shape, dt=F32, name=None):
        self.nalloc += 1
        return stack.enter_context(self.nc.psum_tensor("%s_%d" % (name or "p", self.nalloc), list(shape), dt))

    def _wait(self, e, key, val):
        w = self.waited[e]
        if w.get(key, 0) < val:
            self.eng[e].wait_ge(self.semobj[key], val)
            w[key] = val

    def _deps(self, e, reads, writes):
        deps = {}
        for b in reads:
            if b.w is not None:
                k, v = b.w
                if deps.get(k, 0) < v:
                    deps[k] = v
        for b in writes:
            if b.w is not None:
                k, v = b.w
                if deps.get(k, 0) < v:
                    deps[k] = v
            for k, v in b.r.items():
                if deps.get(k, 0) < v:
                    deps[k] = v
        for k, v in deps.items():
            if e == "pe" and k == "pe":
                continue
            self._wait(e, k, v)

    def _mark(self, tok, reads, writes):
        k, v = tok
        for b in reads:
            if b.r.get(k, 0) < v:
                b.r[k] = v
        for b in writes:
            b.w = tok
            b.r = {}

    def op(self, e, fn, reads=(), writes=()):
        self._deps(e, reads, writes)
        inst = fn(self.eng[e])
        self.cnt[e] += 1
        inst.then_inc(self.semobj[e], 1)
        self._mark((e, self.cnt[e]), reads, writes)

    def dma(self, q, out, in_, reads=(), writes=(), **kw):
        self._deps(q, reads, writes)
        i = self.dnext
        self.dnext = (i + 1) % N_DSEM
        key = ("d", i)
        if self.dval[i] > 0:
            self._wait(q, key, self.dval[i])
        self.dval[i] += 16
        self.eng[q].dma_start(out=out, in_=in_, **kw).then_inc(self.semobj[key], 16)
        self._mark((key, self.dval[i]), reads, writes)
        return (key, self.dval[i])

    def idma(self, out, in_, idx_ap, scatter, reads=(), writes=(), bound=None):
        q = "pool"
        self._deps(q, reads, writes)
        i = self.dnext
        self.dnext = (i + 1) % N_DSEM
        key = ("d", i)
        if self.dval[i] > 0:
            self._wait(q, key, self.dval[i])
        self.dval[i] += 16
        off = bass.IndirectOffsetOnAxis(ap=idx_ap, axis=0)
        kw = {}
        if bound is not None:
            if self.bound_reg is None:
                self.bound_reg = self.es.enter_context(self.nc.gpsimd.register("bound_reg"))
                self.nc.gpsimd.reg_mov(self.bound_reg, bound)
                self.bound_val = bound
            assert self.bound_val == bound
            kw = dict(bounds_check=self.bound_reg, oob_is_err=False)
        if scatter:
            inst = self.eng[q].indirect_dma_start(out=out, out_offset=off, in_=in_, in_offset=None, **kw)
        else:
            inst = self.eng[q].indirect_dma_start(out=out, out_offset=None, in_=in_, in_offset=off, **kw)
        inst.then_inc(self.semobj[key], 16)
        self._mark((key, self.dval[i]), reads, writes)

    def dma_dyn(self, q, out, tensor, reg, pattern, reads=(), writes=()):
        self._deps(q, reads, writes)
        i = self.dnext
        self.dnext = (i + 1) % N_DSEM
        key = ("d", i)
        if self.dval[i] > 0:
            self._wait(q, key, self.dval[i])
        self.dval[i] += 16
        src = bass.AP(tensor, reg, [list(x) for x in pattern])
        self.eng[q].dma_start(out=out, in_=src).then_inc(self.semobj[key], 16)
        self._mark((key, self.dval[i]), reads, writes)

    def barrier(self):
        for e in self.eng:
            for k in ("pe", "act", "dve", "pool"):
                if k != e and self.cnt[k] > 0:
                    self._wait(e, k, self.cnt[k])
            for i in range(N_DSEM):
                if self.dval[i] > 0:
                    self._wait(e, ("d", i), self.dval[i])

    def mm(self, out, lhsT, rhs, start, stop, reads, writes):
        self.op("pe", lambda e: e.matmul(out, lhsT, rhs, start=start, stop=stop), reads, writes)

    def act(self, out, in_, func, reads, writes, bias=None, scale=None, accum_out=None, eng="act"):
        kw = {}
        if bias is not None:
            kw["bias"] = bias
        if scale is not None:
            kw["scale"] = scale
        if accum_out is not None:
            kw["accum_out"] = accum_out
        self.op("act", lambda e: e.activation(out=out, in_=in_, func=func, **kw), reads, writes)

    def tt(self, e, out, in0, in1, op, reads, writes):
        self.op(e, lambda g: g.tensor_tensor(out=out, in0=in0, in1=in1, op=op), reads, writes)

    def ts(self, e, out, in0, s1, s2, op0, op1, reads, writes):
        if op1 is None:
            self.op(e, lambda g: g.tensor_scalar(out=out, in0=in0, scalar1=s1, scalar2=None, op0=op0), reads, writes)
        else:
            self.op(e, lambda g: g.tensor_scalar(out=out, in0=in0, scalar1=s1, scalar2=s2, op0=op0, op1=op1), reads, writes)

    def stt(self, out, in0, scalar, in1, op0, op1, reads, writes):
        self.op("dve", lambda g: g.scalar_tensor_tensor(out=out, in0=in0, scalar=scalar, in1=in1, op0=op0, op1=op1),
                reads, writes)

    def copy(self, e, out, in_, reads, writes):
        if e == "act":
            self.op("act", lambda g: g.copy(out=out, in_=in_), reads, writes)
        else:
            self.op(e, lambda g: g.tensor_copy(out=out, in_=in_), reads, writes)


PENDING = []


def defer(fn):
    PENDING.append([0, fn])


def run_pending(min_age=0):
    keep = []
    for item in list(PENDING):
        if item[0] >= min_age:
            PENDING.remove(item)
            item[1]()


def pipeline(n, front, back, lag):
    for i in range(n + lag):
        if i < n:
            front(i)
        for item in PENDING:
            item[0] += 1
        run_pending(3)
        if i >= lag:
            back(i - lag)


class Rot:
    def __init__(self, items):
        self.items = items
        self.i = 0

    def next(self):
        it = self.items[self.i]
        self.i = (self.i + 1) % len(self.items)
        return it


DEBUG = False


def build_program(stages, out_lat_only=True):
    kb = KB()
    nc = kb.nc
    es = kb.es

    def din(name, shape):
        return nc.dram_tensor(name, list(shape), F32, kind="ExternalInput").ap()

    xT_d = din("xT", [D, T])
    cond_d = din("cond", [D, 2])
    ada_w_d = din("ada_w", [4, D, 6 * D])
    ada_b_d = din("ada_bT", [4, 128, 48])
    ngm_d = din("norm_mix_gT", [128, 4, 8])
    ngf_d = din("norm_ffn_gT", [128, 4, 8])
    ident_d = din("ident", [128, 128])
    wr_d = din("moe_wr", [4, D, 36])
    br_d = din("moe_br", [4, 36])
    if MOE_SPARSE:
        wg_d = din("moe_w_gate", [4 * 32 * 128 * 2, 2048])
        wu_d = din("moe_w_up", [4 * 32 * 128 * 2, 2048])
        wd_d = din("moe_w_down", [4 * 32 * 128 * 2, 2048])
        pcol2_d = din("pcol2", [128, 1])
    else:
        wg_d = din("moe_w_gate", [4, 32, D, 512])
        wu_d = din("moe_w_up", [4, 32, D, 512])
        wd_d = din("moe_w_down", [4, 32, 512, D])
    na_wqkv_d = din("na_w_qkv", [D, 3 * D])
    na_wo_d = din("na_w_o", [D, D])
    na_g_d = din("na_qk_gT", [128, 2])
    na_bt_d = din("na_bt", [128, 8, 14, 64])
    na_mask_d = din("na_mask", [128, 64])
    sw_wqkv_d = din("sw_w_qkv", [D, 1536])
    sw_wo_d = din("sw_w_o", [D, D])
    sw_g_d = din("sw_qk_gT", [64, 2])
    sw_sink_d = din("sw_sink", [1, 16])
    sw_cos_d = din("sw_cos", [64, TL])
    sw_sin_d = din("sw_sin", [64, TL])
    sw_rt_d = din("sw_rt", [64, 64])
    sw_mask_d = din("sw_mask", [128, 2, 128])
    ml_win_d = din("ml_w_in", [D, 3104])
    ml_wo_d = din("ml_w_o", [D, D])
    ml_bg_d = din("ml_b_gates", [32, 1])
    ml_cc_d = din("ml_cc", [32, 4])
    ml_ng_d = din("ml_norm_gT", [128, 8])
    cap_d = din("caps", [128, 2, 4, 512])
    gl_win_d = din("gl_w_in", [D, 3104])
    gl_wo_d = din("gl_w_o", [D, D])
    gl_wa_d = din("gl_wa", [16, 2, 512])
    gl_ba_d = din("gl_baT", [128, 2, 4])
    gl_ng_d = din("gl_norm_gT", [128, 8])
    m01_d = din("mask01", [128, 2, 4, 512])
    ecap_d = din("ecap", [128, 32])
    umat_d = din("umat", [128, 128])
    xs_d = nc.dram_tensor("moe_xs", [NSLOT, D], BF16).ap()
    ys_d = nc.dram_tensor("moe_ys", [NSLOT, D], F32).ap()
    yT_d = nc.dram_tensor("yT", [D, T], F32, kind="ExternalOutput").ap()
    dbg = {}
    if DEBUG:
        for nm, shp in (("dbg_mod", [128, 96]), ("dbg_ht", [128, NCH * T]), ("dbg_wt", [32, T]), ("dbg_a1", [128, 32]), ("dbg_wg", [128, NCH * 512]), ("dbg_act", [128, 4 * 512]), ("dbg_pw", [128, 512])):
            dbg[nm] = nc.dram_tensor(nm, shp, F32, kind="ExternalOutput").ap()

    XT = kb.sb(es, [128, NCH, T], F32, "XT")
    XTb = [[Buf() for _ in SLABS] for _ in range(NCH)]
    HT = kb.sb(es, [128, NCH, T], BF16, "HT")
    HTb = [[Buf() for _ in SLABS] for _ in range(NCH)]
    ident = kb.sb(es, [128, 128], F32, "ident")
    identb = Buf()
    ones_f = kb.sb(es, [128, 128], F32, "ones_f")
    ones_fb = Buf()
    ones_h = kb.sb(es, [128, 128], BF16, "ones_h")
    ones_hb = Buf()
    condT = kb.sb(es, [128, NCH, 2], F32, "condT")
    condb = Buf()
    MOD = kb.sb(es, [128, 48, 2], F32, "MOD")
    MODb = Buf()
    A1 = kb.sb(es, [128, 2, NCH, 2], F32, "A1")
    A1b = Buf()
    ngm = kb.sb(es, [128, 4, 8], F32, "ngm")
    ngf = kb.sb(es, [128, 4, 8], F32, "ngf")
    ngb = Buf()
    adab = kb.sb(es, [128, 48], F32, "adab")
    adabb = Buf()
    epsc = kb.sb(es, [128, 1], F32, "epsc")
    epsb = Buf()
    banks = [kb.psum(es, [128, 512], F32, "bank%d" % i) for i in range(8)]
    bankb = [Buf() for _ in range(8)]

    xT_v = xT_d.rearrange("(c p) t -> p c t", p=128)
    for c in range(NCH):
        for j, (s0, n) in enumerate(SLABS):
            kb.dma("sp", XT[:, c, s0:s0 + n], xT_v[:, c, s0:s0 + n], writes=[XTb[c][j]])
    kb.dma("sp", ident[:], ident_d[:, :], writes=[identb])
    kb.dma("sp", condT[:], cond_d.rearrange("(c p) k -> p c k", p=128), writes=[condb])
    kb.dma("sp", ngm[:], ngm_d[:, :, :], writes=[ngb])
    kb.dma("sp", ngf[:], ngf_d[:, :, :], writes=[ngb])
    kb.op("pool", lambda g: g.memset(ones_f[:], 1.0), writes=[ones_fb])
    kb.op("pool", lambda g: g.memset(ones_h[:], 1.0), writes=[ones_hb])
    kb.op("pool", lambda g: g.memset(epsc[:], 1e-6), writes=[epsb])
    kb.act(condT[:], condT[:], AF.Silu, reads=[condb], writes=[condb])

    def compute_mod(l):
        with contextlib.ExitStack() as st:
            NB = 768
            wts = [kb.sb(st, [128, NCH, NB], BF16, "adaw") for _ in range(2)]
            wtb = [Buf(), Buf()]
            condh = kb.sb(st, [128, NCH, 2], BF16, "condh")
            condhb = Buf()
            kb.copy("dve", condh[:], condT[:], reads=[condb], writes=[condhb])
            kb.dma("sp", adab[:], ada_b_d[l], writes=[adabb])
            for blk in range(6 * D // NB):
                w, wb = wts[blk % 2], wtb[blk % 2]
                kb.dma("pool", w[:], ada_w_d[l, :, blk * NB:(blk + 1) * NB].rearrange("(c p) n -> p c n", p=128),
                       writes=[wb])
                for o in range(NB // 128):
                    oc = blk * (NB // 128) + o
                    pb = banks[oc % 2]
                    pbb = bankb[oc % 2]
                    for c in range(NCH):
                        kb.mm(pb[:, 0:2], w[:, c, o * 128:(o + 1) * 128], condh[:, c, :], c == 0, c == NCH - 1,
                              reads=[wb, condhb], writes=[pbb])
                    kb.ts("dve", MOD[:, oc, :], pb[:, 0:2], adab[:, oc:oc + 1], None, ALU.add, None,
                          reads=[pbb, adabb], writes=[MODb])
            for which, (gt, m) in enumerate(((ngm, 1), (ngf, 4))):
                for k in range(2):
                    kb.stt(A1[:, which, :, k], MOD[:, m * 8:(m + 1) * 8, k], 1.0, gt[:, l, :], ALU.add, ALU.mult,
                           reads=[MODb, ngb], writes=[A1b])
            kb.barrier()

    def norm_slab(st_bufs, j, which, shift_m, fs=None, fsb=None, want_ht=True):
        sq_rot, t1_rot, rstd_rot = st_bufs
        s0, n = SLABS[j]
        k = 0 if j == 0 else 1
        pb, pbb = banks[7], bankb[7]
        for c in range(NCH):
            sq, sqb = sq_rot.next()
            kb.act(sq[:, :n], XT[:, c, s0:s0 + n], AF.Square, reads=[XTb[c][j]], writes=[sqb])
            kb.mm(pb[:, :n], ones_h[:], sq[:, :n], c == 0, c == NCH - 1, reads=[ones_hb, sqb], writes=[pbb])
        rstd, rstdb = rstd_rot.next()
        kb.act(rstd[:, :n], pb[:, :n], AF.Sqrt, reads=[pbb, epsb], writes=[rstdb], bias=epsc[:], scale=1.0 / D)
        kb.op("dve", lambda g: g.reciprocal(out=rstd[:, :n], in_=rstd[:, :n]), reads=[rstdb], writes=[rstdb])
        for c in range(NCH):
            t1, t1b = t1_rot.next()
            kb.stt(t1[:, :n], XT[:, c, s0:s0 + n], A1[:, which, c, k:k + 1], rstd[:, :n], ALU.mult, ALU.mult,
                   reads=[XTb[c][j], A1b, rstdb], writes=[t1b])
            sh = MOD[:, shift_m * 8 + c, k:k + 1]
            if fs is None:
                kb.act(HT[:, c, s0:s0 + n], t1[:, :n], AF.Identity, reads=[t1b, MODb], writes=[HTb[c][j]],
                       bias=sh, scale=1.0)
            else:
                kb.act(fs[:, c, :n], t1[:, :n], AF.Identity, reads=[t1b, MODb], writes=[fsb[c]], bias=sh, scale=1.0)
                if want_ht:
                    kb.copy("pool", HT[:, c, s0:s0 + n], fs[:, c, :n], reads=[fsb[c]], writes=[HTb[c][j]])

    def make_norm_bufs(st):
        sq_rot = Rot([(kb.sb(st, [128, 512], BF16, "sq"), Buf()) for _ in range(2)])
        t1_rot = Rot([(kb.sb(st, [128, 512], F32, "t1"), Buf()) for _ in range(2)])
        rstd_rot = Rot([(kb.sb(st, [128, 512], F32, "rstd"), Buf()) for _ in range(2)])
        return sq_rot, t1_rot, rstd_rot

    def moe_stage(l, do_ctx):
        if MOE_SPARSE:
            return moe_stage_sparse(l, do_ctx)
        with contextlib.ExitStack() as st_outer:
            WT = kb.sb(st_outer, [32, T], BF16, "WT")
            WTb = [Buf() for _ in SLABS]
            moe_stage_inner(l, do_ctx, WT, WTb)


    I32 = mybir.dt.int32

    def moe_stage_sparse(l, do_ctx):
        first_slab = 0 if do_ctx else 1
        tiles = list(range(0 if do_ctx else 2, 18))
        ntok = 128 * len(tiles)
        nblk = (2 * ntok + 32 * (BS - 1) + BS - 1) // BS
        with contextlib.ExitStack() as so:
            SLOT = kb.sb(so, [128, 36], U32, "SLOT")
            SLOTb = Buf()
            WK = kb.sb(so, [128, 18, 2], F32, "WK")
            WKb = [Buf() for _ in range(18)]
            BLKI = kb.sb(so, [128, NBLK_MAX, 2], U32, "BLKI")
            BLKIb = Buf()
            wg = [kb.sb(so, [128, NCH, 512], BF16, "wg") for _ in range(2)]
            wu = [kb.sb(so, [128, NCH, 512], BF16, "wu") for _ in range(2)]
            wdn = [kb.sb(so, [128, 4, D], BF16, "wd") for _ in range(2)]
            wgb, wub, wdb = [[Buf(), Buf()] for _ in range(3)]
            identh = kb.sb(so, [128, 128], BF16, "identh")
            identhb = Buf()
            kb.copy("dve", identh[:], ident[:], reads=[identb], writes=[identhb])
            xsb = [[Buf(), Buf()] for _ in range(18)]
            ysb = [[Buf() for _ in range(BS // 128)] for _ in range(NBLK_MAX)]
            ROWS = HT[:].rearrange("p c t -> p (c t)")
            rowsb = [Buf() for _ in range(18)]

            def load_block(b):
                p = b % 2
                for h in range(2):
                    ix = BLKI[:, b, h:h + 1]
                    wmax = 4 * 32 * 128 * 2 - 1
                    kb.idma(wg[p][:, 4 * h:4 * h + 4, :].rearrange("p c n -> p (c n)"), wg_d, ix, False, reads=[BLKIb], writes=[wgb[p]],
                            bound=wmax)
                    kb.idma(wu[p][:, 4 * h:4 * h + 4, :].rearrange("p c n -> p (c n)"), wu_d, ix, False, reads=[BLKIb], writes=[wub[p]],
                            bound=wmax)
                    kb.idma(wdn[p][:, 2 * h:2 * h + 2, :].rearrange("p c n -> p (c n)"), wd_d, ix, False, reads=[BLKIb], writes=[wdb[p]],
                            bound=wmax)

            with contextlib.ExitStack() as st:
                nb = make_norm_bufs(st)
                FS = kb.sb(st, [128, NCH, 512], F32, "FS")
                FSb = [Buf() for _ in range(NCH)]
                WR = kb.sb(st, [128, NCH, 36], F32, "WR")
                WRb = Buf()
                BR = kb.sb(st, [128, 36], F32, "BR")
                BRb = Buf()
                umat = kb.sb(st, [128, 128], F32, "umat")
                cstb = Buf()
                base = kb.sb(st, [128, 32], F32, "base")
                baseb = Buf()
                MS = kb.sb(st, [128, 36, 32], F32, "MS")
                MSb = Buf()
                PK = kb.sb(st, [128, 36], F32, "PK")
                PKb = Buf()
                kb.dma("sp", WR[:], wr_d[l].rearrange("(c p) n -> p c n", p=128), writes=[WRb])
                kb.dma("sp", BR[:], br_d[l:l + 1, :].to_broadcast([128, 36]), writes=[BRb])
                kb.dma("sp", umat[:], umat_d[:, :], writes=[cstb])
                kb.op("pool", lambda g: g.memset(base[:], 0.0), writes=[baseb])
                kb.op("pool", lambda g: g.memset(MS[:], 0.0), writes=[MSb])
                kb.op("pool", lambda g: g.memset(PK[:], 0.0), writes=[PKb])
                rsets = []
                for _ in range(4):
                    d = dict(
                        lgs=kb.sb(st, [128, 36], F32), gmax=kb.sb(st, [128, 1], F32), ngmax=kb.sb(st, [128, 1], F32),
                        ge=kb.sb(st, [128, 4], F32), gsum=kb.sb(st, [128, 1], F32), gw=kb.sb(st, [128, 1], F32),
                        gm=kb.sb(st, [128, 4], F32), pen=kb.sb(st, [128, 4], F32), lem=kb.sb(st, [128, 32], F32),
                        top8=kb.sb(st, [128, 8], F32), dv=kb.sb(st, [128, 1], F32), e21=kb.sb(st, [128, 1], F32),
                        w1=kb.sb(st, [128, 1], F32), mm_=kb.sb(st, [128, 32], F32), pos=kb.sb(st, [128, 32], F32),
                        tmp=kb.sb(st, [128, 32], F32), b=Buf())
                    rsets.append(d)
                rrot = Rot(rsets)
                for j in range(first_slab, len(SLABS)):
                    s0, n = SLABS[j]
                    norm_slab(nb, j, 1, 3, fs=FS, fsb=FSb, want_ht=False)
                    def tile_gen(tt, s0=s0, n=n, j=j):
                        gi = s0 // 128 + tt
                        r = rrot.next()
                        rb = r["b"]
                        pb, pbb = banks[6], bankb[6]
                        for c in range(NCH):
                            kb.mm(pb[:, 0:36], FS[:, c, tt * 128:(tt + 1) * 128], WR[:, c, :], c == 0, c == NCH - 1,
                                  reads=[FSb[c], WRb], writes=[pbb])
                        kb.tt("dve", r["lgs"][:], pb[:, 0:36], BR[:], ALU.add, reads=[pbb, BRb], writes=[rb])
                        yield
                        kb.op("dve", lambda g: g.tensor_reduce(out=r["gmax"][:], in_=r["lgs"][:, 0:4], axis=AX.X, op=ALU.max),
                              reads=[rb], writes=[rb])
                        yield
                        kb.ts("dve", r["ngmax"][:], r["gmax"][:], -1.0, None, ALU.mult, None, reads=[rb], writes=[rb])
                        yield
                        kb.act(r["ge"][:], r["lgs"][:, 0:4], AF.Exp, reads=[rb], writes=[rb], bias=r["ngmax"][:], scale=1.0,
                               accum_out=r["gsum"][:])
                        yield
                        kb.op("dve", lambda g: g.reciprocal(out=r["gw"][:], in_=r["gsum"][:]), reads=[rb], writes=[rb])
                        yield
                        kb.ts("dve", r["gm"][:], r["lgs"][:, 0:4], r["gmax"][:], None, ALU.is_ge, None, reads=[rb], writes=[rb])
                        yield
                        kb.ts("dve", r["pen"][:], r["gm"][:], -1.0, 1e30, ALU.add, ALU.mult, reads=[rb], writes=[rb])
                        yield
                        for g4 in range(4):
                            kb.ts("dve", r["lem"][:, 8 * g4:8 * g4 + 8], r["lgs"][:, 4 + 8 * g4:12 + 8 * g4],
                                  r["gm"][:, g4:g4 + 1], r["pen"][:, g4:g4 + 1], ALU.mult, ALU.add, reads=[rb], writes=[rb])
                        kb.op("dve", lambda g: g.max(out=r["top8"][:], in_=r["lem"][:]), reads=[rb], writes=[rb])
                        yield
                        kb.tt("dve", r["dv"][:], r["top8"][:, 1:2], r["top8"][:, 0:1], ALU.subtract, reads=[rb], writes=[rb])
                        yield
                        kb.act(r["e21"][:], r["dv"][:], AF.Exp, reads=[rb], writes=[rb])
                        yield
                        kb.ts("dve", r["w1"][:], r["e21"][:], 1.0, None, ALU.add, None, reads=[rb], writes=[rb])
                        yield
                        kb.op("dve", lambda g: g.reciprocal(out=r["w1"][:], in_=r["w1"][:]), reads=[rb], writes=[rb])
                        yield
                        kb.tt("dve", WK[:, gi, 0:1], r["w1"][:], r["gw"][:], ALU.mult, reads=[rb], writes=[WKb[gi]])
                        yield
                        kb.tt("dve", WK[:, gi, 1:2], WK[:, gi, 0:1], r["e21"][:], ALU.mult, reads=[rb, WKb[gi]], writes=[WKb[gi]])
                        yield
                        for k in range(2):
                            kb.ts("dve", MS[:, 2 * gi + k, :], r["lem"][:], r["top8"][:, k:k + 1], None, ALU.is_equal, None,
                                  reads=[rb], writes=[MSb])
                        kb.tt("dve", r["mm_"][:], MS[:, 2 * gi, :], MS[:, 2 * gi + 1, :], ALU.add, reads=[MSb], writes=[rb])
                        yield
                        pp, ppb = banks[5], bankb[5]
                        kb.mm(pp[:, 0:32], umat[:, :], r["mm_"][:], True, True, reads=[cstb, rb], writes=[ppb])
                        kb.mm(pp[:, 32:64], ones_f[:, :], r["mm_"][:], True, True, reads=[ones_fb, rb], writes=[ppb])
                        kb.tt("dve", r["pos"][:], pp[:, 0:32], base[:], ALU.add, reads=[ppb, baseb], writes=[rb])
                        kb.tt("dve", base[:], pp[:, 32:64], base[:], ALU.add, reads=[ppb, baseb], writes=[baseb])
                        yield
                        for k in range(2):
                            kb.tt("dve", r["tmp"][:], MS[:, 2 * gi + k, :], r["pos"][:], ALU.mult, reads=[rb, MSb], writes=[rb])
                            kb.op("dve", lambda g: g.tensor_reduce(out=PK[:, 2 * gi + k:2 * gi + k + 1], in_=r["tmp"][:], axis=AX.X,
                                                                    op=ALU.add), reads=[rb], writes=[PKb])
                        for half in range(2):
                            pt_, ptb_ = banks[half], bankb[half]
                            for q in range(4):
                                c = half * 4 + q
                                kb.op("pe", lambda e: e.transpose(out=pt_[:, q * 128:(q + 1) * 128],
                                                                  in_=FS[:, c, tt * 128:(tt + 1) * 128], identity=ident[:]),
                                      reads=[FSb[c], identb], writes=[ptb_])
                            kb.copy("act", ROWS[:, gi * D + half * 512:gi * D + (half + 1) * 512], pt_[:, :], reads=[ptb_],
                                    writes=[rowsb[gi]])


                    gens = [tile_gen(tt) for tt in range(n // 128)]
                    if not ROUTE_INTERLEAVE:
                        for g_ in gens:
                            for _ in g_:
                                pass
                        gens = []
                    while gens:
                        for g_ in list(gens):
                            try:
                                next(g_)
                            except StopIteration:
                                gens.remove(g_)
                padded = kb.sb(st, [128, 32], F32, "padded")
                pend = kb.sb(st, [128, 32], F32, "pend")
                pstart = kb.sb(st, [128, 32], F32, "pstart")
                one32 = kb.sb(st, [128, 32], F32, "one32")
                lay = Buf()
                kb.op("pool", lambda g: g.memset(one32[:], 1.0), writes=[lay])
                kb.ts("dve", padded[:], base[:], 0.0, None, ALU.is_gt, None, reads=[baseb], writes=[lay])
                for m_ in range(1, (2 * T) // BS + 1):
                    kb.stt(padded[:], base[:], float(m_ * BS), padded[:], ALU.is_gt, ALU.add, reads=[baseb, lay], writes=[lay])
                kb.ts("dve", padded[:], padded[:], float(BS), None, ALU.mult, None, reads=[lay], writes=[lay])
                kb.op("dve", lambda g: g.tensor_tensor_scan(out=pend[:], data0=one32[:], data1=padded[:], initial=0.0,
                                                             op0=ALU.mult, op1=ALU.add), reads=[lay], writes=[lay])
                kb.tt("dve", pstart[:], pend[:], padded[:], ALU.subtract, reads=[lay], writes=[lay])
                slotf = kb.sb(st, [128, 36], F32, "slotf")
                tmp32 = kb.sb(st, [128, 32], F32, "tmp32")
                for gi in tiles:
                    for k in range(2):
                        kb.tt("dve", tmp32[:], MS[:, 2 * gi + k, :], pstart[:], ALU.mult, reads=[MSb, lay], writes=[lay])
                        kb.op("dve", lambda g: g.tensor_reduce(out=slotf[:, 2 * gi + k:2 * gi + k + 1], in_=tmp32[:], axis=AX.X,
                                                                op=ALU.add), reads=[lay], writes=[lay])
                kb.tt("dve", slotf[:], slotf[:], PK[:], ALU.add, reads=[lay, PKb], writes=[lay])
                kb.copy("dve", SLOT[:], slotf[:], reads=[lay], writes=[SLOTb])
                blkf = kb.sb(st, [128, NBLK_MAX], F32, "blkf")
                tb = kb.sb(st, [128, 32], F32, "tb")
                pcol2 = kb.sb(st, [128, 1], F32, "pcol2")
                kb.dma("sp", pcol2[:], pcol2_d[:, :], writes=[lay])
                kb.op("pool", lambda g: g.memset(blkf[:], 0.0), writes=[lay])
                for b in range(nblk):
                    kb.ts("dve", tb[:], pend[:, :], float(BS * b), None, ALU.is_le, None, reads=[lay], writes=[lay])
                    kb.op("dve", lambda g: g.tensor_reduce(out=blkf[:, b:b + 1], in_=tb[:], axis=AX.X, op=ALU.add),
                          reads=[lay], writes=[lay])
                blke = kb.sb(st, [128, NBLK_MAX], F32, "blke")
                kb.ts("dve", blke[:], blkf[:], 31.5, 1.0e7, ALU.is_gt, ALU.mult, reads=[lay], writes=[lay])
                kb.ts("dve", blkf[:], blkf[:], 31.0, float(l * 32), ALU.min, ALU.add, reads=[lay], writes=[lay])
                kb.ts("dve", blkf[:], blkf[:], 256.0, pcol2[:, 0:1], ALU.mult, ALU.add, reads=[lay], writes=[lay])
                kb.tt("dve", blkf[:], blkf[:], blke[:], ALU.add, reads=[lay], writes=[lay])
                blkh = kb.sb(st, [128, NBLK_MAX], F32, "blkh")
                kb.copy("dve", BLKI[:, :, 0], blkf[:], reads=[lay], writes=[BLKIb])
                kb.ts("dve", blkh[:], blkf[:], 1.0, None, ALU.add, None, reads=[lay], writes=[lay])
                kb.copy("dve", BLKI[:, :, 1], blkh[:], reads=[lay], writes=[BLKIb])
                load_block(0)
                load_block(1)
                for gi in tiles:
                    for k in range(2):
                        kb.idma(xs_d, ROWS[:, gi * D:(gi + 1) * D], SLOT[:, 2 * gi + k:2 * gi + k + 1], True,
                                reads=[rowsb[gi], SLOTb], writes=[xsb[gi][k]])
                kb.barrier()
            with contextlib.ExitStack() as st:
                XeS = [kb.sb(st, [128, 2, D], BF16, "XeS") for _ in range(2)]
                XeSb = [Buf(), Buf()]
                XeT = [kb.sb(st, [128, NCH, CAP], BF16, "XeT") for _ in range(2)]
                XeTb = [[Buf(), Buf()] for _ in range(2)]
                actT = [kb.sb(st, [128, 4, CAP], BF16, "actT") for _ in range(2)]
                actb = [[Buf() for _ in range(4)] for _ in range(2)]
                sg_rot = Rot([(kb.sb(st, [128, CAP], F32, "sg"), Buf()) for _ in range(2)])
                yb_rot = Rot([(kb.sb(st, [128, D], F32, "ybuf"), Buf()) for _ in range(2)])
                all_xs = [xsb[gi][k] for gi in tiles for k in range(2)]

                def load_x(e):
                    p = e % 2
                    kb.dma("sp", XeS[p][:], xs_d[e * CAP:(e + 1) * CAP, :].rearrange("(a p) f -> p a f", p=128),
                           reads=all_xs, writes=[XeSb[p]])

                def xpose(e):
                    p = e % 2
                    for a in range(CAP // 128):
                        pt_, ptb_ = banks[6 + a % 2], bankb[6 + a % 2]
                        pv = pt_[:].bitcast(BF16)
                        for c in range(NCH):
                            kb.op("pe", lambda en: en.transpose(out=pv[:, c * 128:(c + 1) * 128], in_=XeS[p][:, a, c * 128:(c + 1) * 128],
                                                                identity=identh[:]),
                                  reads=[XeSb[p], identhb], writes=[ptb_])
                        kb.copy("dve", XeT[p][:, :, a * 128:(a + 1) * 128], pv[:, :].rearrange("p (c s) -> p c s", c=NCH),
                                reads=[ptb_], writes=[XeTb[p][a]])

                load_x(0)
                if nblk > 1:
                    load_x(1)
                xpose(0)
                bi = 0
                for e in range(nblk):
                    p = e % 2
                    if e + 1 < nblk:
                        xpose(e + 1)
                    if e + 2 < nblk:
                        load_x(e + 2)
                    a_ = actT[e % 2]
                    ab = actb[e % 2]
                    for f in range(4):
                        pg, pgb = banks[bi % 3], bankb[bi % 3]
                        bi += 1
                        for c in range(NCH):
                            kb.mm(pg[:, 0:CAP], wg[p][:, c, f * 128:(f + 1) * 128], XeT[p][:, c, :], c == 0, c == NCH - 1,
                                  reads=[wgb[p]] + XeTb[p], writes=[pgb])
                        for c in range(NCH):
                            kb.mm(pg[:, CAP:2 * CAP], wu[p][:, c, f * 128:(f + 1) * 128], XeT[p][:, c, :], c == 0, c == NCH - 1,
                                  reads=[wub[p]] + XeTb[p], writes=[pgb])
                        sg, sgb = sg_rot.next()
                        kb.act(sg[:, :], pg[:, 0:CAP], AF.Silu, reads=[pgb], writes=[sgb])
                        kb.tt("dve", a_[:, f, :], sg[:, :], pg[:, CAP:2 * CAP], ALU.mult, reads=[sgb, pgb], writes=[ab[f]])
                    for a in range(CAP // 128):
                        yb, ybb = yb_rot.next()
                        for half in range(2):
                            py, pyb = banks[3 + half], bankb[3 + half]
                            for f in range(4):
                                kb.mm(py[:, :], a_[:, f, a * 128:(a + 1) * 128], wdn[p][:, f, half * 512:(half + 1) * 512],
                                      f == 0, f == 3, reads=[ab[f], wdb[p]], writes=[pyb])
                            kb.copy("act" if half == 0 else "dve", yb[:, half * 512:(half + 1) * 512], py[:, :], reads=[pyb],
                                    writes=[ybb])
                        kb.dma("sp", ys_d[e * CAP + a * 128:e * CAP + (a + 1) * 128, :], yb[:, :], reads=[ybb], writes=[ysb[e][a]])
                    if e + 2 < nblk:
                        load_block(e + 2)
                kb.barrier()
            with contextlib.ExitStack() as st:
                y_rot = Rot([(kb.sb(st, [128, 2, D], F32, "ygath"), Buf()) for _ in range(4)])
                ys_rot = Rot([(kb.sb(st, [128, D], F32, "ysum"), Buf()) for _ in range(2)])
                all_ys = [ysb[e][a] for e in range(nblk) for a in range(CAP // 128)]
                for gi in tiles:
                    j = slabs_of(gi * 128, 128)[0]
                    kk = 0 if j == 0 else 1
                    yg, ygb = y_rot.next()
                    for k in range(2):
                        kb.idma(yg[:, k, :], ys_d, SLOT[:, 2 * gi + k:2 * gi + k + 1], False, reads=all_ys + [SLOTb], writes=[ygb])
                    ysum, ysumb = ys_rot.next()
                    kb.ts("dve", ysum[:, :], yg[:, 0, :], WK[:, gi, 0:1], None, ALU.mult, None, reads=[ygb, WKb[gi]], writes=[ysumb])
                    kb.stt(ysum[:, :], yg[:, 1, :], WK[:, gi, 1:2], ysum[:, :], ALU.mult, ALU.add, reads=[ygb, WKb[gi], ysumb],
                           writes=[ysumb])
                    for half in range(2):
                        pt_, ptb_ = banks[half], bankb[half]
                        for q in range(4):
                            c = half * 4 + q
                            kb.op("pe", lambda e: e.transpose(out=pt_[:, q * 128:(q + 1) * 128], in_=ysum[:, c * 128:(c + 1) * 128],
                                                              identity=ident[:]),
                                  reads=[ysumb, identb], writes=[ptb_])
                        for q in range(4):
                            c = half * 4 + q
                            kb.stt(XT[:, c, gi * 128:(gi + 1) * 128], pt_[:, q * 128:(q + 1) * 128], MOD[:, 5 * 8 + c, kk:kk + 1],
                                   XT[:, c, gi * 128:(gi + 1) * 128], ALU.mult, ALU.add, reads=[ptb_, MODb, XTb[c][j]],
                                   writes=[XTb[c][j]])
                kb.barrier()

    def moe_stage_inner(l, do_ctx, WT, WTb):
        with contextlib.ExitStack() as st:
            nb = make_norm_bufs(st)
            FS = kb.sb(st, [128, NCH, 512], F32, "FS")
            FSb = [Buf() for _ in range(NCH)]
            WR = kb.sb(st, [128, NCH, 36], F32, "WR")
            WRb = Buf()
            BR = kb.sb(st, [128, 36], F32, "BR")
            BRb = Buf()
            kb.dma("sp", WR[:], wr_d[l].rearrange("(c p) n -> p c n", p=128), writes=[WRb])
            kb.dma("sp", BR[:], br_d[l:l + 1, :].to_broadcast([128, 36]), writes=[BRb])
            rsets = []
            for _ in range(2):
                d = dict(
                    lgs=kb.sb(st, [128, 36], F32), gmax=kb.sb(st, [128, 1], F32), ngmax=kb.sb(st, [128, 1], F32),
                    ge=kb.sb(st, [128, 4], F32), gsum=kb.sb(st, [128, 1], F32), gw=kb.sb(st, [128, 1], F32),
                    gm=kb.sb(st, [128, 4], F32), pen=kb.sb(st, [128, 4], F32), lem=kb.sb(st, [128, 32], F32),
                    top8=kb.sb(st, [128, 8], F32), dv=kb.sb(st, [128, 1], F32), e21=kb.sb(st, [128, 1], F32),
                    w1=kb.sb(st, [128, 1], F32), w2=kb.sb(st, [128, 1], F32), wd1=kb.sb(st, [128, 32], F32),
                    wd2=kb.sb(st, [128, 32], F32), b=Buf())
                rsets.append(d)
            rrot = Rot(rsets)
            first_slab = 0 if do_ctx else 1
            for j in range(first_slab, len(SLABS)):
                s0, n = SLABS[j]
                norm_slab(nb, j, 1, 3, fs=FS, fsb=FSb)
                for tt in range(n // 128):
                    r = rrot.next()
                    rb = r["b"]
                    pb, pbb = banks[6], bankb[6]
                    for c in range(NCH):
                        kb.mm(pb[:, 0:36], FS[:, c, tt * 128:(tt + 1) * 128], WR[:, c, :], c == 0, c == NCH - 1,
                              reads=[FSb[c], WRb], writes=[pbb])
                    kb.tt("dve", r["lgs"][:], pb[:, 0:36], BR[:], ALU.add, reads=[pbb, BRb], writes=[rb])
                    kb.op("dve", lambda g: g.tensor_reduce(out=r["gmax"][:], in_=r["lgs"][:, 0:4], axis=AX.X, op=ALU.max),
                          reads=[rb], writes=[rb])
                    kb.ts("dve", r["ngmax"][:], r["gmax"][:], -1.0, None, ALU.mult, None, reads=[rb], writes=[rb])
                    kb.act(r["ge"][:], r["lgs"][:, 0:4], AF.Exp, reads=[rb], writes=[rb], bias=r["ngmax"][:], scale=1.0,
                           accum_out=r["gsum"][:])
                    kb.op("dve", lambda g: g.reciprocal(out=r["gw"][:], in_=r["gsum"][:]), reads=[rb], writes=[rb])
                    kb.ts("dve", r["gm"][:], r["lgs"][:, 0:4], r["gmax"][:], None, ALU.is_ge, None, reads=[rb], writes=[rb])
                    kb.ts("dve", r["pen"][:], r["gm"][:], -1.0, 1e30, ALU.add, ALU.mult, reads=[rb], writes=[rb])
                    for g4 in range(4):
                        kb.ts("dve", r["lem"][:, 8 * g4:8 * g4 + 8], r["lgs"][:, 4 + 8 * g4:12 + 8 * g4],
                              r["gm"][:, g4:g4 + 1], r["pen"][:, g4:g4 + 1], ALU.mult, ALU.add, reads=[rb], writes=[rb])
                    kb.op("dve", lambda g: g.max(out=r["top8"][:], in_=r["lem"][:]), reads=[rb], writes=[rb])
                    kb.tt("dve", r["dv"][:], r["top8"][:, 1:2], r["top8"][:, 0:1], ALU.subtract, reads=[rb], writes=[rb])
                    kb.act(r["e21"][:], r["dv"][:], AF.Exp, reads=[rb], writes=[rb])
                    kb.ts("dve", r["w1"][:], r["e21"][:], 1.0, None, ALU.add, None, reads=[rb], writes=[rb])
                    kb.op("dve", lambda g: g.reciprocal(out=r["w1"][:], in_=r["w1"][:]), reads=[rb], writes=[rb])
                    kb.tt("dve", r["w1"][:], r["w1"][:], r["gw"][:], ALU.mult, reads=[rb], writes=[rb])
                    kb.tt("dve", r["w2"][:], r["w1"][:], r["e21"][:], ALU.mult, reads=[rb], writes=[rb])
                    kb.ts("dve", r["wd1"][:], r["lem"][:], r["top8"][:, 0:1], r["w1"][:], ALU.is_equal, ALU.mult,
                          reads=[rb], writes=[rb])
                    kb.ts("dve", r["wd2"][:], r["lem"][:], r["top8"][:, 1:2], r["w2"][:], ALU.is_equal, ALU.mult,
                          reads=[rb], writes=[rb])
                    kb.tt("dve", r["wd1"][:], r["wd1"][:], r["wd2"][:], ALU.add, reads=[rb], writes=[rb])
                    pt, ptb = banks[5], bankb[5]
                    kb.op("pe", lambda e: e.transpose(out=pt[0:32, 0:128], in_=r["wd1"][:], identity=ident[:]),
                          reads=[rb, identb], writes=[ptb])
                    kb.copy("act", WT[:, s0 + tt * 128:s0 + (tt + 1) * 128], pt[0:32, 0:128], reads=[ptb], writes=[WTb[j]])
            kb.barrier()
            if DEBUG:
                kb.dma("pool", dbg["dbg_mod"], MOD[:].rearrange("p a b -> p (a b)"), reads=[MODb])
                kb.dma("pool", dbg["dbg_a1"], A1[:].rearrange("p a b c -> p (a b c)"), reads=[A1b])
                kb.dma("pool", dbg["dbg_ht"], HT[:].rearrange("p a b -> p (a b)"), reads=[x for y in HTb for x in y])
                kb.dma("pool", dbg["dbg_wt"], WT[:], reads=WTb)
                kb.barrier()

        with contextlib.ExitStack() as st:
            wg = [kb.sb(st, [128, NCH, 512], BF16, "wg") for _ in range(2)]
            wu = [kb.sb(st, [128, NCH, 512], BF16, "wu") for _ in range(2)]
            wdn = [kb.sb(st, [128, 4, D], BF16, "wd") for _ in range(2)]
            wgb = [Buf(), Buf()]
            wub = [Buf(), Buf()]
            wdb = [Buf(), Buf()]
            wm_rot = Rot([(kb.sb(st, [32, 512], BF16, "wm"), Buf()) for _ in range(2)])
            sg_rot = Rot([(kb.sb(st, [128, 512], F32, "sg"), Buf()) for _ in range(2)])
            m2_rot = Rot([(kb.sb(st, [128, 512], F32, "m2"), Buf()) for _ in range(2)])
            actT = [kb.sb(st, [128, 4, 512], BF16, "actT") for _ in range(2)]
            actb = [[Buf() for _ in range(4)] for _ in range(2)]
            identh = kb.sb(st, [32, 32], F32, "identh")
            first_slab = 0 if do_ctx else 1
            ai = 0
            for e in range(32):
                p = e % 2
                kb.dma("pool", wg[p][:], wg_d[l, e].rearrange("(c p) n -> p c n", p=128), writes=[wgb[p]])
                kb.dma("pool", wu[p][:], wu_d[l, e].rearrange("(c p) n -> p c n", p=128), writes=[wub[p]])
                kb.dma("pool", wdn[p][:], wd_d[l, e].rearrange("(c p) n -> p c n", p=128), writes=[wdb[p]])
                for j in range(first_slab, len(SLABS)):
                    s0, n = SLABS[j]
                    k = 0 if j == 0 else 1
                    wm, wmb = wm_rot.next()
                    kb.ts("dve", wm[:, :n], WT[:, s0:s0 + n], ident[0:32, e:e + 1], None, ALU.mult, None,
                          reads=[WTb[j], identb], writes=[wmb])
                    pw, pwb = banks[4], bankb[4]
                    kb.mm(pw[:, :n], ones_h[0:32, :], wm[:, :n], True, True, reads=[ones_hb, wmb], writes=[pwb])
                    a = actT[ai % 2]
                    ab = actb[ai % 2]
                    ai += 1
                    for f in range(4):
                        pg, pgb = banks[f % 2], bankb[f % 2]
                        pu, pub = banks[2 + f % 2], bankb[2 + f % 2]
                        for c in range(NCH):
                            kb.mm(pg[:, :n], wg[p][:, c, f * 128:(f + 1) * 128], HT[:, c, s0:s0 + n], c == 0, c == NCH - 1,
                                  reads=[wgb[p], HTb[c][j]], writes=[pgb])
                        for c in range(NCH):
                            kb.mm(pu[:, :n], wu[p][:, c, f * 128:(f + 1) * 128], HT[:, c, s0:s0 + n], c == 0, c == NCH - 1,
                                  reads=[wub[p], HTb[c][j]], writes=[pub])
                        sg, sgb = sg_rot.next()
                        kb.act(sg[:, :n], pg[:, :n], AF.Silu, reads=[pgb], writes=[sgb])
                        m2, m2b = m2_rot.next()
                        kb.tt("dve", m2[:, :n], sg[:, :n], pu[:, :n], ALU.mult, reads=[sgb, pub], writes=[m2b])
                        kb.tt("dve", a[:, f, :n], m2[:, :n], pw[:, :n], ALU.mult, reads=[m2b, pwb], writes=[ab[f]])
                    for oc in range(NCH):
                        py, pyb = banks[5 + oc % 2], bankb[5 + oc % 2]
                        for f in range(4):
                            kb.mm(py[:, :n], wdn[p][:, f, oc * 128:(oc + 1) * 128], a[:, f, :n], f == 0, f == 3,
                                  reads=[wdb[p], ab[f]], writes=[pyb])
                        kb.stt(XT[:, oc, s0:s0 + n], py[:, :n], MOD[:, 5 * 8 + oc, k:k + 1], XT[:, oc, s0:s0 + n],
                               ALU.mult, ALU.add, reads=[pyb, MODb, XTb[oc][j]], writes=[XTb[oc][j]])
            kb.barrier()
            if DEBUG:
                kb.dma("pool", dbg["dbg_wg"], wg[1][:].rearrange("p a b -> p (a b)"))
                kb.dma("pool", dbg["dbg_act"], a[:].rearrange("p a b -> p (a b)"))
                sgx, _ = sg_rot.next()
                kb.copy("dve", sgx[:], banks[4][:], reads=[], writes=[])
                kb.barrier()
                kb.dma("pool", dbg["dbg_pw"], sgx[:])
                kb.barrier()


    wo_tmp = Rot([(kb.sb(es, [128, 512], F32, "wo_tmp"), Buf()) for _ in range(1)])
    def slabs_of(off, n):
        return [j for j, (s0, m) in enumerate(SLABS) if off < s0 + m and off + n > s0]

    def load_cols(t, b, w2d, col0, ncols):
        kb.dma("pool", t[:], w2d[:, col0:col0 + ncols].rearrange("(c p) n -> p c n", p=128), writes=[b])

    def pnorm(nb, src, srcb, P, n, gcol, gb, dst, dstb):
        sq, sqb = nb[0].next()
        kb.act(sq[:P, :n], src, AF.Square, reads=[srcb], writes=[sqb])
        pb, pbb = banks[7], bankb[7]
        kb.mm(pb[:P, :n], ones_h[:P, :P], sq[:P, :n], True, True, reads=[ones_hb, sqb], writes=[pbb])
        rstd, rstdb = nb[2].next()
        kb.act(rstd[:P, :n], pb[:P, :n], AF.Sqrt, reads=[pbb, epsb], writes=[rstdb], bias=epsc[:P], scale=1.0 / P)
        kb.op("dve", lambda g: g.reciprocal(out=rstd[:P, :n], in_=rstd[:P, :n]), reads=[rstdb], writes=[rstdb])
        kb.stt(dst, src, gcol, rstd[:P, :n], ALU.mult, ALU.mult, reads=[srcb, gb, rstdb], writes=[dstb])

    def wo_update(Wo, Wob, kparts, OTs, OTbs, j, py_bank=6):
        s0, n = SLABS[j]
        kk = 0 if j == 0 else 1
        for oc in range(NCH):
            pbk = (6, 7)[oc % 2]
            py, pyb = banks[pbk], bankb[pbk]
            for i, (wap, oap, ob) in enumerate(zip(kparts, OTs, OTbs)):
                kb.mm(py[:, :n], wap(oc), oap, i == 0, i == len(OTs) - 1, reads=[Wob, ob], writes=[pyb])
            if oc % 2 == 0:
                kb.stt(XT[:, oc, s0:s0 + n], py[:, :n], MOD[:, 2 * 8 + oc, kk:kk + 1], XT[:, oc, s0:s0 + n],
                       ALU.mult, ALU.add, reads=[pyb, MODb, XTb[oc][j]], writes=[XTb[oc][j]])
            else:
                wt, wtb_ = wo_tmp.next()
                kb.act(wt[:, :n], py[:, :n], AF.Copy, reads=[pyb, MODb], writes=[wtb_], scale=MOD[:, 2 * 8 + oc, kk:kk + 1])
                kb.tt("pool", XT[:, oc, s0:s0 + n], XT[:, oc, s0:s0 + n], wt[:, :n], ALU.add, reads=[wtb_, XTb[oc][j]],
                      writes=[XTb[oc][j]])

    def mixer_na(l, need_ctx):
        with contextlib.ExitStack() as st:
            nb = make_norm_bufs(st)
            for j in range(len(SLABS)):
                norm_slab(nb, j, 0, 0)
            gqk = kb.sb(st, [128, 2], F32, "gqk")
            gqkb = Buf()
            kb.dma("sp", gqk[:], na_g_d[:, :], writes=[gqkb])
            kb.ts("dve", gqk[:, 0:1], gqk[:, 0:1], 128.0 ** -0.5, None, ALU.mult, None, reads=[gqkb], writes=[gqkb])
            maskt = kb.sb(st, [128, 64], BF16, "maskt")
            masktb = Buf()
            kb.dma("pool", maskt[:], na_mask_d[:, :], writes=[masktb])
            identh = kb.sb(st, [128, 128], BF16, "identh")
            identhb = Buf()
            kb.copy("dve", identh[:], ident[:], reads=[identb], writes=[identhb])
            BTm = kb.sb(st, [128, 8, 14, 64], BF16, "BTm")
            BTmb = [Buf() for _ in range(8)]
            btf_rot = Rot([(kb.sb(st, [128, 14, 64], BF16, "btf"), Buf()) for _ in range(2)])
            for h in range(8):
                btf, btfb = btf_rot.next()
                kb.dma("pool", btf[:], na_bt_d[:, h], writes=[btfb])
                for d in range(14):
                    kb.tt("pool", BTm[:, h, d, :], btf[:, d, :], maskt[:], ALU.add, reads=[btfb, masktb], writes=[BTmb[h]])
            Wq = [kb.sb(st, [128, NCH, 128], BF16, "Wq") for _ in range(2)]
            Wk = [kb.sb(st, [128, NCH, 128], BF16, "Wk") for _ in range(2)]
            Wv = [kb.sb(st, [128, NCH, 128], BF16, "Wv") for _ in range(2)]
            Wo = [kb.sb(st, [128, D], BF16, "Wo") for _ in range(2)]
            Wqb, Wkb, Wvb, Wob = [[Buf(), Buf()] for _ in range(4)]
            qf = kb.sb(st, [128, T], F32, "qf")
            kf = kb.sb(st, [128, T], F32, "kf")
            qfb = [Buf() for _ in SLABS]
            kfb = [Buf() for _ in SLABS]
            qT = kb.sb(st, [128, T], BF16, "qT")
            kT = kb.sb(st, [128, T], BF16, "kT")
            qTb = [Buf() for _ in SLABS]
            kTb = [Buf() for _ in SLABS]
            NV = 33
            V = kb.sb(st, [128, NV, 128], BF16, "V")
            Vb = [Buf() for _ in range(NV)]
            voff = [0, 128] + [256 + 128 * a for a in range(16)] + [256 + 64 + 128 * a for a in range(15)]
            pt_rot = Rot([(kb.sb(st, [128, 512], BF16, "PT"), Buf()) for _ in range(4)])
            ot_rot = Rot([(kb.sb(st, [128, 512], BF16, "OT"), Buf()) for _ in range(2)])
            rec_rot = Rot([(kb.sb(st, [128, 512], F32, "rec"), Buf()) for _ in range(2)])

            def load_head(h):
                p = h % 2
                load_cols(Wq[p], Wqb[p], na_wqkv_d, h * 128, 128)
                load_cols(Wk[p], Wkb[p], na_wqkv_d, D + h * 128, 128)
                load_cols(Wv[p], Wvb[p], na_wqkv_d, 2 * D + h * 128, 128)
                kb.dma("pool", Wo[p][:], na_wo_d[h * 128:(h + 1) * 128, :], writes=[Wob[p]])

            def finalize(h, j, numb, denb, nbk, dbk):
                p = h % 2
                s0, n = SLABS[j]
                rec, recb = rec_rot.next()
                kb.op("dve", lambda g: g.reciprocal(out=rec[:, :n], in_=dbk[:, :n]), reads=[denb], writes=[recb])
                ot, otb = ot_rot.next()
                kb.tt("dve", ot[:, :n], nbk[:, :n], rec[:, :n], ALU.mult, reads=[numb, recb], writes=[otb])
                defer(lambda p=p, ot=ot, otb=otb, n=n, j=j: wo_update(
                    Wo[p], Wob[p], [lambda oc: Wo[p][:, oc * 128:(oc + 1) * 128]], [ot[:, :n]], [otb], j))

            load_head(0)
            for h in range(8):
                p = h % 2
                if h + 1 < 8:
                    load_head(h + 1)
                for j, (s0, n) in enumerate(SLABS):
                    for (W_, Wb_, dstf, dstfb) in ((Wq[p], Wqb[p], qf, qfb), (Wk[p], Wkb[p], kf, kfb)):
                        pb, pbb = banks[6], bankb[6]
                        for c in range(NCH):
                            kb.mm(pb[:, :n], W_[:, c, :], HT[:, c, s0:s0 + n], c == 0, c == NCH - 1,
                                  reads=[Wb_, HTb[c][j]], writes=[pbb])
                        kb.copy("act", dstf[:, s0:s0 + n], pb[:, :n], reads=[pbb], writes=[dstfb[j]])
                    pnorm(nb, qf[:, s0:s0 + n], qfb[j], 128, n, gqk[:, 0:1], gqkb, qT[:, s0:s0 + n], qTb[j])
                    pnorm(nb, kf[:, s0:s0 + n], kfb[j], 128, n, gqk[:, 1:2], gqkb, kT[:, s0:s0 + n], kTb[j])
                for vi in range(NV):
                    off = voff[vi]
                    sl = slabs_of(off, 128)
                    pb, pbb = banks[6], bankb[6]
                    for c in range(NCH):
                        kb.mm(pb[:, 0:128], HT[:, c, off:off + 128], Wv[p][:, c, :], c == 0, c == NCH - 1,
                              reads=[Wvb[p]] + [HTb[c][jj] for jj in sl], writes=[pbb])
                    kb.copy("act", V[:, vi, :], pb[:, 0:128], reads=[pbb], writes=[Vb[vi]])
                if need_ctx:
                    sb_, sbb = banks[0], bankb[0]
                    for i in range(2):
                        kb.mm(sb_[:, 256 * i:256 * i + 256], kT[:, 128 * i:128 * i + 128], qT[:, 0:256], True, True,
                              reads=[kTb[0], qTb[0]], writes=[sbb])
                    pt, ptb = pt_rot.next()
                    kb.act(pt[:, :], sb_[:, :], AF.Exp, reads=[sbb], writes=[ptb])
                    nbk, numb = banks[2], bankb[2]
                    dbk, denb = banks[4], bankb[4]
                    for i in range(2):
                        kb.mm(nbk[:, 0:256], V[:, i, :], pt[:, 256 * i:256 * i + 256], i == 0, i == 1,
                              reads=[Vb[i], ptb], writes=[numb])
                    for i in range(2):
                        kb.mm(dbk[:, 0:256], ones_h[:, :], pt[:, 256 * i:256 * i + 256], i == 0, i == 1,
                              reads=[ones_hb, ptb], writes=[denb])
                    finalize(h, 0, numb, denb, nbk, dbk)
                SBK = (0, 1, 6, 7)
                rows_state = {}

                def front(r, h=h):
                    jj = 1 + r // 8
                    r0 = min(max(r - 4, 0), 24)
                    toff = 256 + 64 * r
                    sb_, sbb = banks[SBK[r % 4]], bankb[SBK[r % 4]]
                    vis = []
                    for i in range(4):
                        kr = r0 + 2 * i
                        koff = 256 + 64 * kr
                        d = kr - r + 7
                        kb.mm(sb_[:, 64 * i:64 * i + 64], kT[:, koff:koff + 128], qT[:, toff:toff + 64], True, False,
                              reads=[kTb[x] for x in slabs_of(koff, 128)] + [qTb[jj]], writes=[sbb])
                        kb.mm(sb_[:, 64 * i:64 * i + 64], identh[:, :], BTm[:, h, d, :], False, True,
                              reads=[identhb, BTmb[h]], writes=[sbb])
                        vis.append(2 + kr // 2 if kr % 2 == 0 else 18 + (kr - 1) // 2)
                    for i in range(2):
                        kb.mm(sb_[:, 256 + 64 * i:256 + 64 * i + 64], kT[:, 128 * i:128 * i + 128], qT[:, toff:toff + 64],
                              True, True, reads=[kTb[0], qTb[jj]], writes=[sbb])
                        vis.append(i)
                    pt, ptb = pt_rot.next()
                    kb.act(pt[:, 0:384], sb_[:, 0:384], AF.Exp, reads=[sbb], writes=[ptb])
                    rows_state[r] = (pt, ptb, vis)

                def back(r, h=h):
                    jj = 1 + r // 8
                    rr = r % 8
                    nbk, numb = banks[2 + jj % 2], bankb[2 + jj % 2]
                    dbk, denb = banks[4 + jj % 2], bankb[4 + jj % 2]
                    pt, ptb, vis = rows_state.pop(r)
                    for i, vi in enumerate(vis):
                        kb.mm(nbk[:, 64 * rr:64 * rr + 64], V[:, vi, :], pt[:, 64 * i:64 * i + 64], i == 0, i == 5,
                              reads=[Vb[vi], ptb], writes=[numb])
                    for i, vi in enumerate(vis):
                        kb.mm(dbk[:, 64 * rr:64 * rr + 64], ones_h[:, :], pt[:, 64 * i:64 * i + 64], i == 0, i == 5,
                              reads=[ones_hb, ptb], writes=[denb])
                    if rr == 7:
                        finalize(h, jj, numb, denb, nbk, dbk)

                pipeline(32, front, back, 3)
                run_pending()
            kb.barrier()


    def mixer_sw(l, need_ctx):
        with contextlib.ExitStack() as st:
            nb = make_norm_bufs(st)
            for j in range(len(SLABS)):
                norm_slab(nb, j, 0, 0)
            gqk = kb.sb(st, [64, 2], F32, "gqk")
            gqkb = Buf()
            kb.dma("sp", gqk[:], sw_g_d[:, :], writes=[gqkb])
            kb.ts("dve", gqk[:, 0:1], gqk[:, 0:1], 64.0 ** -0.5, None, ALU.mult, None, reads=[gqkb], writes=[gqkb])
            sinke = kb.sb(st, [128, 16], F32, "sinke")
            sinkb = Buf()
            kb.dma("sp", sinke[:], sw_sink_d[0:1, :].to_broadcast([128, 16]), writes=[sinkb])
            kb.act(sinke[:], sinke[:], AF.Exp, reads=[sinkb], writes=[sinkb])
            cosT = kb.sb(st, [64, TL], F32, "cosT")
            sinT = kb.sb(st, [64, TL], F32, "sinT")
            RT = kb.sb(st, [64, 64], F32, "RT")
            ropeb = Buf()
            kb.dma("sp", cosT[:], sw_cos_d[:, :], writes=[ropeb])
            kb.dma("sp", sinT[:], sw_sin_d[:, :], writes=[ropeb])
            kb.dma("sp", RT[:], sw_rt_d[:, :], writes=[ropeb])
            bmask = kb.sb(st, [128, 2, 128], BF16, "bmask")
            bmaskb = Buf()
            kb.dma("pool", bmask[:], sw_mask_d[:, :, :], writes=[bmaskb])
            identh = kb.sb(st, [128, 128], BF16, "identh")
            identhb = Buf()
            kb.copy("dve", identh[:], ident[:], reads=[identb], writes=[identhb])
            Wq = [kb.sb(st, [128, NCH, 64], BF16, "Wq") for _ in range(2)]
            Wo = [kb.sb(st, [64, D], BF16, "Wo") for _ in range(2)]
            Wk = kb.sb(st, [128, NCH, 64], BF16, "Wk")
            Wv = kb.sb(st, [128, NCH, 64], BF16, "Wv")
            Wqb, Wob = [[Buf(), Buf()] for _ in range(2)]
            Wkb, Wvb = Buf(), Buf()
            xf = kb.sb(st, [64, T], F32, "xf")
            xfb = [Buf() for _ in SLABS]
            xn = kb.sb(st, [64, T], F32, "xn")
            xnb = [Buf() for _ in SLABS]
            qT = kb.sb(st, [64, T], BF16, "qT")
            kT = kb.sb(st, [64, T], BF16, "kT")
            qTb = [Buf() for _ in SLABS]
            kTb = [Buf() for _ in SLABS]
            V = kb.sb(st, [128, 18, 64], BF16, "V")
            Vb = [Buf() for _ in range(18)]
            rt1_rot = Rot([(kb.sb(st, [64, 512], F32, "rt1"), Buf()) for _ in range(2)])
            rt2_rot = Rot([(kb.sb(st, [64, 512], F32, "rt2"), Buf()) for _ in range(2)])
            pt_rot = Rot([(kb.sb(st, [128, 640], BF16, "PT"), Buf()) for _ in range(4)])
            ot_rot = Rot([(kb.sb(st, [64, 512], BF16, "OT"), Buf()) for _ in range(2)])
            rec_rot = Rot([(kb.sb(st, [64, 512], F32, "rec"), Buf()) for _ in range(2)])

            def project_norm_rope(W_, Wb_, gcol, dst, dstb):
                for j, (s0, n) in enumerate(SLABS):
                    pb, pbb = banks[6 + j % 2], bankb[6 + j % 2]
                    for c in range(NCH):
                        kb.mm(pb[:64, :n], W_[:, c, :], HT[:, c, s0:s0 + n], c == 0, c == NCH - 1,
                              reads=[Wb_, HTb[c][j]], writes=[pbb])
                    kb.copy("act", xf[:, s0:s0 + n], pb[:64, :n], reads=[pbb], writes=[xfb[j]])
                for j, (s0, n) in enumerate(SLABS):
                    if j == 0:
                        pnorm(nb, xf[:, s0:s0 + n], xfb[j], 64, n, gcol, gqkb, dst[:, s0:s0 + n], dstb[j])
                    else:
                        pnorm(nb, xf[:, s0:s0 + n], xfb[j], 64, n, gcol, gqkb, xn[:, s0:s0 + n], xnb[j])
                for j, (s0, n) in enumerate(SLABS):
                    if j == 0:
                        continue
                    l0 = s0 - TC
                    pb, pbb = banks[6], bankb[6]
                    kb.mm(pb[:64, :n], RT[:, :], xn[:, s0:s0 + n], True, True, reads=[ropeb, xnb[j]], writes=[pbb])
                    t1, t1b = rt1_rot.next()
                    kb.tt("pool", t1[:, :n], xn[:, s0:s0 + n], cosT[:, l0:l0 + n], ALU.mult, reads=[xnb[j], ropeb],
                          writes=[t1b])
                    t2, t2b = rt2_rot.next()
                    kb.tt("dve", t2[:, :n], pb[:64, :n], sinT[:, l0:l0 + n], ALU.mult, reads=[pbb, ropeb], writes=[t2b])
                    kb.tt("dve", dst[:, s0:s0 + n], t1[:, :n], t2[:, :n], ALU.add, reads=[t1b, t2b], writes=[dstb[j]])

            def finalize(h, j, numb, denb, nbk, dbk):
                p = h % 2
                s0, n = SLABS[j]
                rec, recb = rec_rot.next()
                kb.ts("dve", rec[:, :n], dbk[:64, :n], sinke[:64, h:h + 1], None, ALU.add, None, reads=[denb, sinkb],
                      writes=[recb])
                kb.op("dve", lambda g: g.reciprocal(out=rec[:, :n], in_=rec[:, :n]), reads=[recb], writes=[recb])
                ot, otb = ot_rot.next()
                kb.tt("dve", ot[:, :n], nbk[:64, :n], rec[:, :n], ALU.mult, reads=[numb, recb], writes=[otb])
                defer(lambda p=p, ot=ot, otb=otb, n=n, j=j: wo_update(
                    Wo[p], Wob[p], [lambda oc: Wo[p][:, oc * 128:(oc + 1) * 128]], [ot[:, :n]], [otb], j))

            def load_q(h):
                p = h % 2
                load_cols(Wq[p], Wqb[p], sw_wqkv_d, h * 64, 64)
                kb.dma("pool", Wo[p][:], sw_wo_d[h * 64:(h + 1) * 64, :], writes=[Wob[p]])

            load_q(0)
            for g in range(4):
                load_cols(Wk, Wkb, sw_wqkv_d, 1024 + g * 64, 64)
                load_cols(Wv, Wvb, sw_wqkv_d, 1280 + g * 64, 64)
                project_norm_rope(Wk, Wkb, gqk[:, 1:2], kT, kTb)
                for vi in range(18):
                    off = 128 * vi
                    sl = slabs_of(off, 128)
                    pb, pbb = banks[6], bankb[6]
                    for c in range(NCH):
                        kb.mm(pb[:, 0:64], HT[:, c, off:off + 128], Wv[:, c, :], c == 0, c == NCH - 1,
                              reads=[Wvb] + [HTb[c][jj] for jj in sl], writes=[pbb])
                    kb.copy("act", V[:, vi, :], pb[:, 0:64], reads=[pbb], writes=[Vb[vi]])
                for hh in range(4):
                    h = 4 * g + hh
                    p = h % 2
                    if h + 1 < 16:
                        load_q(h + 1)
                    project_norm_rope(Wq[p], Wqb[p], gqk[:, 0:1], qT, qTb)
                    nbk, numb = banks[4], bankb[4]
                    dbk, denb = banks[5], bankb[5]
                    if need_ctx:
                        sb_, sbb = banks[0], bankb[0]
                        for i in range(2):
                            kb.mm(sb_[:, 256 * i:256 * i + 256], kT[:, 128 * i:128 * i + 128], qT[:, 0:256], True, True,
                                  reads=[kTb[0], qTb[0]], writes=[sbb])
                        pt, ptb = pt_rot.next()
                        kb.act(pt[:, 0:512], sb_[:, :], AF.Exp, reads=[sbb], writes=[ptb])
                        for i in range(2):
                            kb.mm(nbk[:64, 0:256], V[:, i, :], pt[:, 256 * i:256 * i + 256], i == 0, i == 1,
                                  reads=[Vb[i], ptb], writes=[numb])
                        for i in range(2):
                            kb.mm(dbk[:64, 0:256], ones_h[:, 0:64], pt[:, 256 * i:256 * i + 256], i == 0, i == 1,
                                  reads=[ones_hb, ptb], writes=[denb])
                        finalize(h, 0, numb, denb, nbk, dbk)
                    SA = (0, 1, 6)
                    SC = (2, 3, 7)
                    blk_state = {}

                    def front(jb, h=h):
                        jj = 1 + jb // 4
                        toff = TC + 128 * jb
                        sa, sab = banks[SA[jb % 3]], bankb[SA[jb % 3]]
                        sc, scb = banks[SC[jb % 3]], bankb[SC[jb % 3]]
                        tiles = []
                        for w_i, dk_ in enumerate((-1, 0, 1)):
                            kbk = jb + dk_
                            if kbk < 0 or kbk > 15:
                                continue
                            koff = TC + 128 * kbk
                            cs = 128 * w_i
                            kb.mm(sa[:, cs:cs + 128], kT[:, koff:koff + 128], qT[:, toff:toff + 128], True, dk_ == 0,
                                  reads=[kTb[x] for x in slabs_of(koff, 128)] + [qTb[jj]], writes=[sab])
                            if dk_ != 0:
                                kb.mm(sa[:, cs:cs + 128], identh[:, :], bmask[:, 0 if dk_ < 0 else 1, :], False, True,
                                      reads=[identhb, bmaskb], writes=[sab])
                            tiles.append((cs, 2 + kbk))
                        for i in range(2):
                            kb.mm(sc[:, 128 * i:128 * i + 128], kT[:, 128 * i:128 * i + 128], qT[:, toff:toff + 128], True, True,
                                  reads=[kTb[0], qTb[jj]], writes=[scb])
                            tiles.append((384 + 128 * i, i))
                        pt, ptb = pt_rot.next()
                        c_lo = tiles[0][0]
                        c_hi = max(c for c, _ in tiles if c < 384) + 128
                        kb.act(pt[:, c_lo:c_hi], sa[:, c_lo:c_hi], AF.Exp, reads=[sab], writes=[ptb])
                        kb.act(pt[:, 384:640], sc[:, 0:256], AF.Exp, reads=[scb], writes=[ptb])
                        blk_state[jb] = (pt, ptb, tiles)

                    def back(jb, h=h, nbk=nbk, dbk=dbk, numb=numb, denb=denb):
                        jj = 1 + jb // 4
                        pt, ptb, tiles = blk_state.pop(jb)
                        q4 = jb % 4
                        for i, (cs, vi) in enumerate(tiles):
                            kb.mm(nbk[:64, 128 * q4:128 * q4 + 128], V[:, vi, :], pt[:, cs:cs + 128], i == 0, i == len(tiles) - 1,
                                  reads=[Vb[vi], ptb], writes=[numb])
                        for i, (cs, vi) in enumerate(tiles):
                            kb.mm(dbk[:64, 128 * q4:128 * q4 + 128], ones_h[:, 0:64], pt[:, cs:cs + 128], i == 0,
                                  i == len(tiles) - 1, reads=[ones_hb, ptb], writes=[denb])
                        if q4 == 3:
                            finalize(h, jj, numb, denb, nbk, dbk)

                    pipeline(16, front, back, 2)
                    run_pending()
            kb.barrier()


    def tile_list(d, j):
        s0, n = SLABS[j]
        out = []
        for i in range(18):
            k0 = 128 * i
            if d == 0:
                if k0 + 128 <= s0:
                    out.append((i, "full", 0))
                elif k0 < s0 + n:
                    out.append((i, "diag", (k0 - s0) // 128))
            else:
                if j == 0:
                    if i < 2:
                        out.append((i, "diag", i))
                else:
                    if i < 2 or k0 >= s0 + n:
                        out.append((i, "full", 0))
                    elif k0 + 128 > s0:
                        out.append((i, "diag", (k0 - s0) // 128))
        return out

    def mixer_ml(l, need_ctx):
        with contextlib.ExitStack() as st:
            nb = make_norm_bufs(st)
            for j in range(len(SLABS)):
                norm_slab(nb, j, 0, 0)
            F_ = kb.sb(st, [32, T], F32, "F")
            Fb = [Buf() for _ in SLABS]
            BIAS = kb.sb(st, [128, 18, 16], F32, "BIAS")
            BIASb = Buf()
            caps = kb.sb(st, [128, 2, 4, 512], BF16, "caps")
            capsb = Buf()
            kb.dma("pool", caps[:, 0], cap_d[:, 0], writes=[capsb])
            kb.dma("pool", caps[:, 1], cap_d[:, 1], writes=[capsb])
            ngc = kb.sb(st, [128, 8], F32, "ngc")
            ngcb = Buf()
            kb.dma("sp", ngc[:], ml_ng_d[:, :], writes=[ngcb])
            with contextlib.ExitStack() as st2:
                Wg = kb.sb(st2, [128, NCH, 32], BF16, "Wg")
                Wgb = Buf()
                load_cols(Wg, Wgb, ml_win_d, 3072, 32)
                cc = kb.sb(st2, [32, 4], F32, "cc")
                bcol = kb.sb(st2, [32, 1], F32, "bcol")
                ccb = Buf()
                kb.dma("sp", cc[:], ml_cc_d[:, :], writes=[ccb])
                kb.dma("sp", bcol[:], ml_bg_d[:, :], writes=[ccb])
                kb.ts("dve", bcol[:], bcol[:], 1.0 / 15.0, None, ALU.mult, None, reads=[ccb], writes=[ccb])
                G_ = kb.sb(st2, [32, T], F32, "G")
                Gb = [Buf() for _ in SLABS]
                CS = kb.sb(st2, [32, T], F32, "CS")
                CSb = [Buf() for _ in SLABS]
                one32 = kb.sb(st2, [32, 512], F32, "one32")
                one32b = Buf()
                kb.op("pool", lambda g: g.memset(one32[:], 1.0), writes=[one32b])
                pre = kb.sb(st2, [32, 512], F32, "pre")
                lt = kb.sb(st2, [32, 512], F32, "lt")
                tmpb = Buf()
                totc = kb.sb(st2, [32, 1], F32, "totc")
                for j, (s0, n) in enumerate(SLABS):
                    pb, pbb = banks[6], bankb[6]
                    for c in range(NCH):
                        kb.mm(pb[:32, :n], Wg[:, c, :], HT[:, c, s0:s0 + n], c == 0, c == NCH - 1,
                              reads=[Wgb, HTb[c][j]], writes=[pbb])
                    kb.act(pre[:, :n], pb[:32, :n], AF.Tanh, reads=[pbb, ccb], writes=[tmpb], bias=bcol[:], scale=1.0 / 15.0)
                    kb.ts("dve", pre[:, :n], pre[:, :n], 15.0, None, ALU.mult, None, reads=[tmpb], writes=[tmpb])
                    kb.act(lt[:, :n], pre[:, :n], AF.Exp, reads=[tmpb], writes=[tmpb], scale=-1.0)
                    kb.ts("dve", lt[:, :n], lt[:, :n], 1.0, None, ALU.add, None, reads=[tmpb], writes=[tmpb])
                    kb.act(lt[:, :n], lt[:, :n], AF.Ln, reads=[tmpb], writes=[tmpb])
                    kb.stt(lt[:, :n], lt[:, :n], -1.0, pre[:, :n], ALU.mult, ALU.subtract, reads=[tmpb], writes=[tmpb])
                    kb.stt(G_[:, s0:s0 + n], lt[:, :n], cc[:, 0:1], pre[:, :n], ALU.mult, ALU.add, reads=[tmpb, ccb],
                           writes=[Gb[j]])
                    init = 0.0 if j == 0 else CS[:, s0 - 1:s0]
                    kb.op("dve", lambda g: g.tensor_tensor_scan(out=CS[:, s0:s0 + n], data0=one32[:, :n], data1=G_[:, s0:s0 + n],
                                                                 initial=init, op0=ALU.mult, op1=ALU.add),
                          reads=[one32b, Gb[j]] + ([CSb[j - 1]] if j else []), writes=[CSb[j]])
                kb.tt("dve", totc[:], CS[:, T - 1:T], cc[:, 3:4], ALU.mult, reads=[CSb[4], ccb], writes=[tmpb])
                for j, (s0, n) in enumerate(SLABS):
                    kb.ts("dve", F_[:, s0:s0 + n], CS[:, s0:s0 + n], cc[:, 1:2], None, ALU.mult, None, reads=[CSb[j], ccb],
                          writes=[Fb[j]])
                    kb.stt(F_[:, s0:s0 + n], G_[:, s0:s0 + n], cc[:, 2:3], F_[:, s0:s0 + n], ALU.mult, ALU.add,
                           reads=[Gb[j], ccb, Fb[j]], writes=[Fb[j]])
                    if j > 0:
                        kb.ts("dve", F_[:, s0:s0 + n], F_[:, s0:s0 + n], totc[:], None, ALU.add, None, reads=[Fb[j], tmpb],
                              writes=[Fb[j]])
                colF = kb.sb(st2, [128, 32], F32, "colF")
                colFb = Buf()
                for i in range(18):
                    sl = slabs_of(128 * i, 128)
                    pb, pbb = banks[6], bankb[6]
                    kb.op("pe", lambda e: e.transpose(out=pb[:, 0:32], in_=F_[:, 128 * i:128 * i + 128], identity=ident[0:32, 0:32]),
                          reads=[Fb[x] for x in sl] + [identb], writes=[pbb])
                    kb.op("pe", lambda e: e.transpose(out=pb[:, 32:64], in_=G_[:, 128 * i:128 * i + 128], identity=ident[0:32, 0:32]),
                          reads=[Gb[x] for x in sl] + [identb], writes=[pbb])
                    kb.copy("act", colF[:], pb[:, 0:32], reads=[pbb], writes=[colFb])
                    gv = pb[:, 32:64].rearrange("p (d f h) -> p d f h", d=2, f=2)[:, :, 0, :]
                    fv = colF[:].rearrange("p (d f h) -> p d f h", d=2, f=2)[:, :, 1, :]
                    kb.tt("dve", BIAS[:, i, :].rearrange("p (d h) -> p d h", d=2), gv, fv, ALU.subtract,
                          reads=[pbb, colFb], writes=[BIASb])
                kb.barrier()
            Wq = [kb.sb(st, [128, NCH, 64], BF16, "Wq") for _ in range(2)]
            Wk = [kb.sb(st, [128, NCH, 64], BF16, "Wk") for _ in range(2)]
            Wv = [kb.sb(st, [128, NCH, 128], BF16, "Wv") for _ in range(2)]
            Wog = [kb.sb(st, [128, NCH, 128], BF16, "Wog") for _ in range(2)]
            Wo = [kb.sb(st, [128, D], BF16, "Wo") for _ in range(2)]
            Wqb, Wkb, Wvb, Wogb, Wob = [[Buf(), Buf()] for _ in range(5)]
            qT = kb.sb(st, [64, T], BF16, "qT")
            kT = kb.sb(st, [64, T], BF16, "kT")
            qTb = [Buf() for _ in SLABS]
            kTb = [Buf() for _ in SLABS]
            V = kb.sb(st, [128, 18, 128], BF16, "V")
            Vb = [Buf() for _ in range(18)]
            OG = kb.sb(st, [128, 512], F32, "OG")
            OGb = Buf()
            Hs = kb.sb(st, [128, 512], F32, "Hs")
            Hsb = Buf()
            Yn = kb.sb(st, [128, 512], F32, "Yn")
            Ynb = Buf()
            Yh = kb.sb(st, [128, 512], BF16, "Yh")
            Yhb = Buf()
            fm_rot = Rot([(kb.sb(st, [32, 512], F32, "Fm"), Buf()) for _ in range(2)])
            dt_rot = Rot([(kb.sb(st, [128, 512], F32, "Dt"), Buf()) for _ in range(4)])
            pt_rot = Rot([(kb.sb(st, [128, 512], BF16, "PT"), Buf()) for _ in range(5)])
            rec_rot = Rot([(kb.sb(st, [128, 512], F32, "rec"), Buf()) for _ in range(2)])

            def load_head(h):
                p = h % 2
                load_cols(Wq[p], Wqb[p], ml_win_d, h * 64, 64)
                load_cols(Wk[p], Wkb[p], ml_win_d, 512 + h * 64, 64)
                load_cols(Wv[p], Wvb[p], ml_win_d, 1024 + h * 128, 128)
                load_cols(Wog[p], Wogb[p], ml_win_d, 2048 + h * 128, 128)
                kb.dma("pool", Wo[p][:], ml_wo_d[h * 128:(h + 1) * 128, :], writes=[Wob[p]])

            load_head(0)
            for h in range(8):
                p = h % 2
                if h + 1 < 8:
                    load_head(h + 1)
                for j, (s0, n) in enumerate(SLABS):
                    for (W_, Wb_, dst, dstb, sc) in ((Wq[p], Wqb[p], qT, qTb, 0.125), (Wk[p], Wkb[p], kT, kTb, 1.0)):
                        pb, pbb = banks[6], bankb[6]
                        for c in range(NCH):
                            kb.mm(pb[:64, :n], W_[:, c, :], HT[:, c, s0:s0 + n], c == 0, c == NCH - 1,
                                  reads=[Wb_, HTb[c][j]], writes=[pbb])
                        kb.act(dst[:, s0:s0 + n], pb[:64, :n], AF.Copy, reads=[pbb], writes=[dstb[j]], scale=sc)
                for vi in range(18):
                    off = 128 * vi
                    sl = slabs_of(off, 128)
                    pb, pbb = banks[6], bankb[6]
                    for c in range(NCH):
                        kb.mm(pb[:, 0:128], HT[:, c, off:off + 128], Wv[p][:, c, :], c == 0, c == NCH - 1,
                              reads=[Wvb[p]] + [HTb[c][jj] for jj in sl], writes=[pbb])
                    kb.copy("act", V[:, vi, :], pb[:, 0:128], reads=[pbb], writes=[Vb[vi]])
                for j, (s0, n) in enumerate(SLABS):
                    if j == 0 and not need_ctx:
                        continue
                    pb, pbb = banks[6], bankb[6]
                    for c in range(NCH):
                        kb.mm(pb[:, :n], Wog[p][:, c, :], HT[:, c, s0:s0 + n], c == 0, c == NCH - 1,
                              reads=[Wogb[p], HTb[c][j]], writes=[pbb])
                    kb.act(OG[:, :n], pb[:, :n], AF.Sigmoid, reads=[pbb], writes=[OGb])
                    for d in range(2):
                        row = d * 16 + 8 + h
                        fm, fmb = fm_rot.next()
                        kb.ts("dve", fm[:, :n], F_[:, s0:s0 + n], ident[0:32, row:row + 1], None, ALU.mult, None,
                              reads=[Fb[j], identb], writes=[fmb])
                        fr, frb = banks[d], bankb[d]
                        kb.mm(fr[:, :n], ones_f[0:32, :], fm[:, :n], True, True, reads=[ones_fb, fmb], writes=[frb])
                        nbk, numb = banks[4], bankb[4]
                        dbk, denb = banks[5], bankb[5]
                        tl = tile_list(d, j)
                        SB = (2, 3, 6, 7)
                        pts = {}

                        def front(ti, tl=tl, d=d, fr=fr, frb=frb, pts=pts, s0=s0, n=n, j=j):
                            i, kind, o = tl[ti]
                            sb_, sbb = banks[SB[ti % 4]], bankb[SB[ti % 4]]
                            kb.mm(sb_[:, :n], kT[:, 128 * i:128 * i + 128], qT[:, s0:s0 + n], True, True,
                                  reads=[kTb[x] for x in slabs_of(128 * i, 128)] + [qTb[j]], writes=[sbb])
                            bcol_ = BIAS[:, i, d * 8 + h:d * 8 + h + 1]
                            dt, dtb = dt_rot.next()
                            if kind == "full":
                                kb.act(dt[:, :n], fr[:, :n], AF.Exp, reads=[frb, BIASb], writes=[dtb], bias=bcol_, scale=1.0)
                            else:
                                kb.stt(dt[:, :n], fr[:, :n], bcol_, caps[:, d, o, :n], ALU.add, ALU.min,
                                       reads=[frb, BIASb, capsb], writes=[dtb])
                                kb.act(dt[:, :n], dt[:, :n], AF.Exp, reads=[dtb], writes=[dtb])
                            pt, ptb = pt_rot.next()
                            kb.tt("dve", pt[:, :n], dt[:, :n], sb_[:, :n], ALU.mult, reads=[dtb, sbb], writes=[ptb])
                            pts[ti] = (pt, ptb)

                        def back(ti, tl=tl, pts=pts, n=n, nbk=nbk, dbk=dbk, numb=numb, denb=denb):
                            i, kind, o = tl[ti]
                            pt, ptb = pts.pop(ti)
                            kb.mm(nbk[:, :n], V[:, i, :], pt[:, :n], ti == 0, ti == len(tl) - 1, reads=[Vb[i], ptb], writes=[numb])
                            kb.mm(dbk[:, :n], ones_h[:, :], pt[:, :n], ti == 0, ti == len(tl) - 1, reads=[ones_hb, ptb],
                                  writes=[denb])

                        pipeline(len(tl), front, back, 3)
                        rec, recb = rec_rot.next()
                        kb.ts("dve", rec[:, :n], dbk[:, :n], 1.0, None, ALU.max, None, reads=[denb], writes=[recb])
                        kb.stt(rec[:, :n], dbk[:, :n], -1.0, rec[:, :n], ALU.mult, ALU.max, reads=[denb, recb], writes=[recb])
                        kb.op("dve", lambda g: g.reciprocal(out=rec[:, :n], in_=rec[:, :n]), reads=[recb], writes=[recb])
                        if d == 0:
                            kb.tt("dve", Hs[:, :n], nbk[:, :n], rec[:, :n], ALU.mult, reads=[numb, recb], writes=[Hsb])
                        else:
                            kb.tt("dve", rec[:, :n], nbk[:, :n], rec[:, :n], ALU.mult, reads=[numb, recb], writes=[recb])
                            kb.tt("dve", Hs[:, :n], Hs[:, :n], rec[:, :n], ALU.add, reads=[Hsb, recb], writes=[Hsb])
                    pnorm(nb, Hs[:, :n], Hsb, 128, n, ngc[:, h:h + 1], ngcb, Yn[:, :n], Ynb)
                    kb.tt("dve", Yh[:, :n], Yn[:, :n], OG[:, :n], ALU.mult, reads=[Ynb, OGb], writes=[Yhb])
                    defer(lambda p=p, n=n, j=j: wo_update(
                        Wo[p], Wob[p], [lambda oc: Wo[p][:, oc * 128:(oc + 1) * 128]], [Yh[:, :n]], [Yhb], j))
                run_pending()
            kb.barrier()


    def mixer_gl(l, need_ctx):
        assert not need_ctx
        with contextlib.ExitStack() as st:
            with contextlib.ExitStack() as st2:
                nb = make_norm_bufs(st2)
                for j in range(len(SLABS)):
                    norm_slab(nb, j, 0, 0)
                kb.barrier()
            ZR = [kb.sb(st, [16, T], BF16, "ZR%d" % d) for d in range(2)]
            ZRb = [[Buf() for _ in SLABS] for _ in range(2)]
            WA = kb.sb(st, [16, 2, 512], BF16, "WA")
            WAb = Buf()
            kb.dma("pool", WA[:], gl_wa_d[:, :, :], writes=[WAb])
            NBA = kb.sb(st, [128, 2, 4], F32, "NBA")
            NBAb = Buf()
            kb.dma("sp", NBA[:], gl_ba_d[:, :, :], writes=[NBAb])
            kb.ts("dve", NBA[:], NBA[:], -1.0, None, ALU.mult, None, reads=[NBAb], writes=[NBAb])
            m01 = kb.sb(st, [128, 2, 4, 512], BF16, "m01")
            m01b = Buf()
            kb.dma("pool", m01[:, 0], m01_d[:, 0], writes=[m01b])
            kb.dma("pool", m01[:, 1], m01_d[:, 1], writes=[m01b])
            ngc = kb.sb(st, [128, 8], F32, "ngc")
            ngcb = Buf()
            kb.dma("sp", ngc[:], gl_ng_d[:, :], writes=[ngcb])
            one512 = kb.sb(st, [128, 512], F32, "one512")
            one512b = Buf()
            kb.op("pool", lambda g: g.memset(one512[:], 1.0), writes=[one512b])
            with contextlib.ExitStack() as st2:
                Wz = kb.sb(st2, [128, NCH, 32], BF16, "Wz")
                Wzb = Buf()
                load_cols(Wz, Wzb, gl_win_d, 3072, 32)
                for d in range(2):
                    for j, (s0, n) in enumerate(SLABS):
                        pb, pbb = banks[6], bankb[6]
                        for c in range(NCH):
                            kb.mm(pb[:16, :n], Wz[:, c, 16 * d:16 * d + 16], HT[:, c, s0:s0 + n], c == 0, c == NCH - 1,
                                  reads=[Wzb, HTb[c][j]], writes=[pbb])
                        kb.copy("act", ZR[d][:, s0:s0 + n], pb[:16, :n], reads=[pbb], writes=[ZRb[d][j]])
                kb.barrier()
            Wq = kb.sb(st, [128, NCH, 128], BF16, "Wq")
            Wk = kb.sb(st, [128, NCH, 128], BF16, "Wk")
            Wv = kb.sb(st, [128, NCH, 256], BF16, "Wv")
            Wgt = kb.sb(st, [128, NCH, 256], BF16, "Wgt")
            Wo = kb.sb(st, [128, 2, D], BF16, "Wo")
            Wqb, Wkb, Wvb, Wgtb, Wob = Buf(), Buf(), Buf(), Buf(), Buf()
            Bd = [kb.sb(st, [128, T], F32, "B%d" % d) for d in range(2)]
            Bdb = [[Buf() for _ in SLABS] for _ in range(2)]
            NR = kb.sb(st, [128, 2, 18], F32, "NR")
            NRb = Buf()
            csl = kb.sb(st, [128, 1], F32, "csl")
            cslb = Buf()
            qb = kb.sb(st, [128, T], BF16, "qb")
            kbf = kb.sb(st, [128, T], BF16, "kbf")
            qbb = [Buf() for _ in SLABS]
            kbb = [Buf() for _ in SLABS]
            V = kb.sb(st, [128, 18, 256], BF16, "V")
            Vb = [Buf() for _ in range(18)]
            e2_rot = Rot([(kb.sb(st, [128, 512], F32, "e2"), Buf()) for _ in range(2)])
            SG = kb.sb(st, [128, 512], F32, "SG")
            SGb = Buf()
            qt_rot = Rot([(kb.sb(st, [128, 512], BF16, "qt"), Buf()) for _ in range(2)])
            at_rot = Rot([(kb.sb(st, [128, 512], BF16, "AT"), Buf()) for _ in range(3)])
            e1_rot = Rot([(kb.sb(st, [128, 128], F32, "e1"), Buf()) for _ in range(3)])
            kt_rot = Rot([(kb.sb(st, [128, 128], BF16, "kt"), Buf()) for _ in range(3)])
            sq_rot = Rot([(kb.sb(st, [128, 512], F32, "sq"), Buf()) for _ in range(1)])
            rstd = kb.sb(st, [128, 512], F32, "rstd")
            rstdb = Buf()
            ytmp = kb.sb(st, [128, 512], F32, "ytmp")
            ytmpb = Buf()
            Y = kb.sb(st, [128, 2, 512], BF16, "Y")
            Yb = [Buf(), Buf()]
            (tA, tAb), (tB, tBb) = e2_rot.items[0], e2_rot.items[1]
            tC, tCb = SG, SGb
            for h in range(4):
                load_cols(Wq, Wqb, gl_win_d, h * 128, 128)
                load_cols(Wk, Wkb, gl_win_d, 512 + h * 128, 128)
                load_cols(Wv, Wvb, gl_win_d, 1024 + h * 256, 256)
                load_cols(Wgt, Wgtb, gl_win_d, 2048 + h * 256, 256)
                kb.dma("pool", Wo[:], gl_wo_d[h * 256:(h + 1) * 256, :].rearrange("(v p) n -> p v n", p=128), writes=[Wob])
                for d in range(2):
                    for j, (s0, n) in enumerate(SLABS):
                        pb, pbb = banks[4 + j % 2], bankb[4 + j % 2]
                        kb.mm(pb[:, :n], WA[:, d, h * 128:(h + 1) * 128], ZR[d][:, s0:s0 + n], True, True,
                              reads=[WAb, ZRb[d][j]], writes=[pbb])
                        kb.act(tA[:, :n], pb[:, :n], AF.Exp, reads=[pbb, NBAb], writes=[tAb], bias=NBA[:, d, h:h + 1], scale=-1.0)
                        kb.ts("dve", tA[:, :n], tA[:, :n], 1.0, None, ALU.add, None, reads=[tAb], writes=[tAb])
                        kb.act(tA[:, :n], tA[:, :n], AF.Ln, reads=[tAb], writes=[tAb])
                        kb.ts("dve", tB[:, :n], tA[:, :n], -1.0 / 16.0, None, ALU.mult, None, reads=[tAb], writes=[tBb])
                        if d == 0:
                            init = 0.0 if j == 0 else Bd[0][:, s0 - 1:s0]
                            kb.op("dve", lambda g: g.tensor_tensor_scan(out=Bd[0][:, s0:s0 + n], data0=one512[:, :n], data1=tB[:, :n],
                                                                         initial=init, op0=ALU.mult, op1=ALU.add),
                                  reads=[one512b, tBb] + ([Bdb[0][j - 1]] if j else []), writes=[Bdb[0][j]])
                        else:
                            init = 0.0 if j == 0 else csl[:, 0:1]
                            kb.op("dve", lambda g: g.tensor_tensor_scan(out=tC[:, :n], data0=one512[:, :n], data1=tB[:, :n],
                                                                         initial=init, op0=ALU.mult, op1=ALU.add),
                                  reads=[one512b, tBb, cslb], writes=[tCb])
                            kb.copy("dve", csl[:, 0:1], tC[:, n - 1:n], reads=[tCb], writes=[cslb])
                            kb.tt("dve", Bd[1][:, s0:s0 + n], tB[:, :n], tC[:, :n], ALU.subtract, reads=[tBb, tCb],
                                  writes=[Bdb[1][j]])
                    if d == 1:
                        for j in range(1, 5):
                            s0, n = SLABS[j]
                            kb.ts("dve", Bd[1][:, s0:s0 + n], Bd[1][:, s0:s0 + n], csl[:, 0:1], None, ALU.add, None,
                                  reads=[Bdb[1][j], cslb], writes=[Bdb[1][j]])
                    kb.ts("dve", NR[:, d, :], Bd[d][:, 64:T:128], -1.0, None, ALU.mult, None, reads=Bdb[d], writes=[NRb])
                for j, (s0, n) in enumerate(SLABS):
                    for (W_, Wb_, dst, dstb, sc) in ((Wq, Wqb, qb, qbb, 128.0 ** -0.5), (Wk, Wkb, kbf, kbb, 1.0)):
                        if dst is qb and j == 0:
                            continue
                        pb, pbb = banks[6], bankb[6]
                        for c in range(NCH):
                            kb.mm(pb[:, :n], W_[:, c, :], HT[:, c, s0:s0 + n], c == 0, c == NCH - 1,
                                  reads=[Wb_, HTb[c][j]], writes=[pbb])
                        kb.act(dst[:, s0:s0 + n], pb[:, :n], AF.Copy, reads=[pbb], writes=[dstb[j]], scale=sc)
                for vi in range(18):
                    off = 128 * vi
                    sl = slabs_of(off, 128)
                    pb, pbb = banks[6], bankb[6]
                    for c in range(NCH):
                        kb.mm(pb[:, 0:256], HT[:, c, off:off + 128], Wv[:, c, :], c == 0, c == NCH - 1,
                              reads=[Wvb] + [HTb[c][jj] for jj in sl], writes=[pbb])
                    kb.copy("act", V[:, vi, :], pb[:, 0:256], reads=[pbb], writes=[Vb[vi]])
                for j in range(1, 5):
                    s0, n = SLABS[j]
                    pairs = [(d, i, kind, o) for d in range(2) for (i, kind, o) in tile_list(d, j)]
                    ob = [(banks[2], bankb[2]), (banks[3], bankb[3])]
                    AB = (0, 1, 4, 5)
                    ats = {}

                    def front(idx, pairs=pairs, ats=ats, s0=s0, n=n, j=j):
                        d, i, kind, o = pairs[idx]
                        k0 = 128 * i
                        ksl = slabs_of(k0, 128)
                        e1, e1b = e1_rot.next()
                        kb.act(e1[:, :], Bd[d][:, k0:k0 + 128], AF.Exp, reads=[Bdb[d][x] for x in ksl], writes=[e1b],
                               bias=Bd[d][:, k0 + 64:k0 + 65], scale=-1.0)
                        kt, ktb = kt_rot.next()
                        kb.tt("dve", kt[:, :], kbf[:, k0:k0 + 128], e1[:, :], ALU.mult, reads=[kbb[x] for x in ksl] + [e1b],
                              writes=[ktb])
                        e2, e2b = e2_rot.next()
                        kb.act(e2[:, :n], Bd[d][:, s0:s0 + n], AF.Exp, reads=[Bdb[d][j], NRb], writes=[e2b],
                               bias=NR[:, d, i:i + 1], scale=1.0)
                        qt, qtb = qt_rot.next()
                        kb.tt("pool" if idx % 2 == 0 else "dve", qt[:, :n], qb[:, s0:s0 + n], e2[:, :n], ALU.mult,
                              reads=[qbb[j], e2b], writes=[qtb])
                        ab_, abb = banks[AB[idx % 4]], bankb[AB[idx % 4]]
                        kb.mm(ab_[:, :n], kt[:, :], qt[:, :n], True, True, reads=[ktb, qtb], writes=[abb])
                        at, atb = at_rot.next()
                        if kind == "full":
                            kb.copy("act", at[:, :n], ab_[:, :n], reads=[abb], writes=[atb])
                        else:
                            kb.tt("dve", at[:, :n], ab_[:, :n], m01[:, d, o, :n], ALU.mult, reads=[abb, m01b], writes=[atb])
                        ats[idx] = (at, atb)

                    def back(idx, pairs=pairs, ats=ats, n=n, ob=ob):
                        d, i, kind, o = pairs[idx]
                        at, atb = ats.pop(idx)
                        for v in range(2):
                            kb.mm(ob[v][0][:, :n], V[:, i, v * 128:(v + 1) * 128], at[:, :n], idx == 0, idx == len(pairs) - 1,
                                  reads=[Vb[i], atb], writes=[ob[v][1]])

                    pipeline(len(pairs), front, back, 2)
                    ps_, psb = banks[7], bankb[7]
                    for v in range(2):
                        sq, sqb = sq_rot.next()
                        kb.act(sq[:, :n], ob[v][0][:, :n], AF.Square, reads=[ob[v][1]], writes=[sqb])
                        kb.mm(ps_[:, :n], ones_f[:, :], sq[:, :n], v == 0, v == 1, reads=[ones_fb, sqb], writes=[psb])
                    kb.act(rstd[:, :n], ps_[:, :n], AF.Sqrt, reads=[psb, epsb], writes=[rstdb], bias=epsc[:], scale=1.0 / 256.0)
                    kb.op("dve", lambda g: g.reciprocal(out=rstd[:, :n], in_=rstd[:, :n]), reads=[rstdb], writes=[rstdb])
                    for v in range(2):
                        pb, pbb = banks[6], bankb[6]
                        for c in range(NCH):
                            kb.mm(pb[:, :n], Wgt[:, c, v * 128:(v + 1) * 128], HT[:, c, s0:s0 + n], c == 0, c == NCH - 1,
                                  reads=[Wgtb, HTb[c][j]], writes=[pbb])
                        kb.act(SG[:, :n], pb[:, :n], AF.Silu, reads=[pbb], writes=[SGb])
                        kb.stt(ytmp[:, :n], ob[v][0][:, :n], ngc[:, 2 * h + v:2 * h + v + 1], rstd[:, :n], ALU.mult, ALU.mult,
                               reads=[ob[v][1], ngcb, rstdb], writes=[ytmpb])
                        kb.tt("dve", Y[:, v, :n], ytmp[:, :n], SG[:, :n], ALU.mult, reads=[ytmpb, SGb], writes=[Yb[v]])
                    wo_update(Wo, Wob, [lambda oc: Wo[:, 0, oc * 128:(oc + 1) * 128], lambda oc: Wo[:, 1, oc * 128:(oc + 1) * 128]],
                              [Y[:, 0, :n], Y[:, 1, :n]], Yb, j)
            kb.barrier()

    def mixer_stage(l, need_ctx):
        kind = l % 4
        if kind == 0:
            mixer_na(l, need_ctx)
        elif kind == 3:
            mixer_gl(l, need_ctx)
        elif kind == 1:
            mixer_ml(l, need_ctx)
        elif kind == 2:
            mixer_sw(l, need_ctx)
        else:
            raise NotImplementedError

    cur_mod = None
    for (l, what) in stages:
        if cur_mod != l:
            compute_mod(l)
            cur_mod = l
        last = l == 3
        if what == "mix":
            mixer_stage(l, not last)
        else:
            moe_stage(l, not last)

    yT_v = yT_d.rearrange("(c p) t -> p c t", p=128)
    toks = []
    for c in range(NCH):
        for j, (s0, n) in enumerate(SLABS):
            toks.append(kb.dma("sp", yT_v[:, c, s0:s0 + n], XT[:, c, s0:s0 + n], reads=[XTb[c][j]]))
    for key, val in toks:
        kb._wait("sp", key, val)
    return kb


_PROG_CACHE = {}
RUN_KW = {}
LAST_EXEC_NS = None


def _get_prog(stages):
    key = tuple(stages)
    if key not in _PROG_CACHE:
        _PROG_CACHE[key] = build_program(list(stages))
    return _PROG_CACHE[key]


def _common_inputs(inp):
    f = np.float32
    d = {}
    d["ada_w"] = np.ascontiguousarray(inp["ada_w"], dtype=f)
    d["ada_bT"] = np.ascontiguousarray(inp["ada_b"].reshape(4, 48, 128).transpose(0, 2, 1), dtype=f)
    d["norm_mix_gT"] = np.ascontiguousarray(inp["norm_mix_g"].reshape(4, 8, 128).transpose(2, 0, 1), dtype=f)
    d["norm_ffn_gT"] = np.ascontiguousarray(inp["norm_ffn_g"].reshape(4, 8, 128).transpose(2, 0, 1), dtype=f)
    d["ident"] = np.eye(128, dtype=f)
    d["moe_wr"] = np.ascontiguousarray(np.concatenate([inp["moe_w_grp"], inp["moe_w_exp"]], axis=-1), dtype=f)
    d["moe_br"] = np.ascontiguousarray(np.concatenate([inp["moe_b_grp"], inp["moe_b_exp"]], axis=-1), dtype=f)
    if MOE_SPARSE:
        d["moe_w_gate"] = np.ascontiguousarray(inp["moe_w_gate"].reshape(4, 32, 8, 128, 512).transpose(0, 1, 3, 2, 4), dtype=f).reshape(-1, 2048)
        d["moe_w_up"] = np.ascontiguousarray(inp["moe_w_up"].reshape(4, 32, 8, 128, 512).transpose(0, 1, 3, 2, 4), dtype=f).reshape(-1, 2048)
        d["moe_w_down"] = np.ascontiguousarray(inp["moe_w_down"].reshape(4, 32, 4, 128, 1024).transpose(0, 1, 3, 2, 4), dtype=f).reshape(-1, 2048)
        d["pcol2"] = (2.0 * np.arange(128, dtype=f)).reshape(128, 1)
    else:
        d["moe_w_gate"] = np.ascontiguousarray(inp["moe_w_gate"], dtype=f)
        d["moe_w_up"] = np.ascontiguousarray(inp["moe_w_up"], dtype=f)
        d["moe_w_down"] = np.ascontiguousarray(inp["moe_w_down"], dtype=f)
    d["ecap"] = np.ascontiguousarray(np.broadcast_to((np.arange(32) * CAP).astype(f)[None, :], (128, 32)))
    d["umat"] = np.ascontiguousarray(np.triu(np.ones((128, 128), f), 1))
    d["na_w_qkv"] = np.ascontiguousarray(inp["na_w_qkv"][0], dtype=f)
    d["na_w_o"] = np.ascontiguousarray(inp["na_w_o"][0], dtype=f)
    d["na_qk_gT"] = np.ascontiguousarray(inp["na_qk_g"][0].T, dtype=f)
    ck = np.arange(64)[:, None]
    cq = np.arange(64)[None, :]
    dc = np.clip(ck - cq + 15, 0, 30)
    rpb = inp["na_rpb"][0]
    g = rpb[:, :, dc]
    bt = np.stack([g[:, 0:14], g[:, 1:15]], axis=0)
    d["na_bt"] = np.ascontiguousarray(bt.transpose(0, 3, 1, 2, 4).reshape(128, 8, 14, 64), dtype=f)
    c0 = np.clip(cq - 8, 0, 48)
    ok = (ck >= c0) & (ck < c0 + 16)
    m = np.where(ok, 0.0, NEG).astype(f)
    d["na_mask"] = np.ascontiguousarray(np.concatenate([m, m], axis=0))
    d["ml_w_in"] = np.ascontiguousarray(inp["ml_w_in"][0], dtype=f)
    d["ml_w_o"] = np.ascontiguousarray(inp["ml_w_o"][0], dtype=f)
    d["ml_b_gates"] = np.ascontiguousarray(inp["ml_b_gates"][0].reshape(32, 1), dtype=f)
    d["ml_norm_gT"] = np.ascontiguousarray(inp["ml_norm_g"][0].reshape(8, 128).T, dtype=f)
    cc = np.zeros((32, 4), f)
    for r_ in range(32):
        dd, ff = r_ // 16, (r_ // 8) % 2
        cc[r_, 0] = float(ff)
        cc[r_, 1] = 1.0 if dd == 0 else -1.0
        cc[r_, 2] = 0.0 if dd == 0 else 1.0
        cc[r_, 3] = 0.0 if dd == 0 else 1.0
    d["ml_cc"] = cc
    s_ = np.arange(128)[:, None, None]
    o_ = np.arange(4)[None, :, None]
    t_ = np.arange(512)[None, None, :]
    BIGC = 30000.0
    capc = np.where(t_ >= 128 * o_ + s_, BIGC, -BIGC).astype(f)
    capa = np.where(128 * o_ + s_ >= t_, BIGC, -BIGC).astype(f)
    d["caps"] = np.ascontiguousarray(np.stack([capc, capa], axis=1))
    d["gl_w_in"] = np.ascontiguousarray(inp["gl_w_in"][0], dtype=f)
    d["gl_w_o"] = np.ascontiguousarray(inp["gl_w_o"][0], dtype=f)
    d["gl_wa"] = np.ascontiguousarray(inp["gl_w_a2"][0].transpose(1, 0, 2), dtype=f)
    d["gl_baT"] = np.ascontiguousarray(inp["gl_b_a"][0].reshape(2, 4, 128).transpose(2, 0, 1), dtype=f)
    d["gl_norm_gT"] = np.ascontiguousarray(inp["gl_norm_g"][0].reshape(8, 128).T, dtype=f)
    d["mask01"] = np.ascontiguousarray((d["caps"] > 0).astype(f))
    d["sw_w_qkv"] = np.ascontiguousarray(inp["sw_w_qkv"][0], dtype=f)
    d["sw_w_o"] = np.ascontiguousarray(inp["sw_w_o"][0], dtype=f)
    d["sw_qk_gT"] = np.ascontiguousarray(inp["sw_qk_g"][0].T, dtype=f)
    d["sw_sink"] = np.ascontiguousarray(inp["sw_sink"].reshape(1, 16), dtype=f)
    pos = np.arange(TL)
    rows, cols = pos // 64, pos % 64
    inv = (10000.0 ** (-np.arange(16, dtype=np.float64) / 16))
    ang = np.zeros((64, TL), np.float64)
    for p_ in range(64):
        ang[p_] = (rows if p_ < 32 else cols) * inv[p_ % 16]
    d["sw_cos"] = np.cos(ang).astype(f)
    d["sw_sin"] = np.sin(ang).astype(f)
    R = np.zeros((64, 64), f)
    for p_ in range(64):
        if (p_ % 32) < 16:
            R[p_, p_ + 16] = -1.0
        else:
            R[p_, p_ - 16] = 1.0
    d["sw_rt"] = np.ascontiguousarray(R.T)
    sr = np.arange(128)[:, None]
    tr = np.arange(128)[None, :]
    mp = np.where(sr >= tr, 0.0, NEG).astype(f)
    mn = np.where(sr <= tr, 0.0, NEG).astype(f)
    d["sw_mask"] = np.ascontiguousarray(np.stack([mp, mn], axis=1))
    return d


def run_stages(inp, stages, xT_list, n_cores=8):
    kb = _get_prog(stages)
    common = _common_inputs(inp)
    in_maps = []
    for b in range(n_cores):
        m = dict(common)
        m["xT"] = np.ascontiguousarray(xT_list[b], dtype=np.float32)
        m["cond"] = np.ascontiguousarray(np.stack([inp["c_ctx"], inp["c"][b]], axis=1), dtype=np.float32)
        in_maps.append(m)
    res = run_bass_kernel_spmd(kb.nc, in_maps, core_ids=list(range(n_cores)), **RUN_KW)
    global LAST_EXEC_NS
    LAST_EXEC_NS = getattr(res, "exec_time_ns", None)
    if DEBUG:
        global LAST_RES
        LAST_RES = res.results
    return [np.asarray(r["yT"]) for r in res.results]


ALL_STAGES = [(l, w) for l in range(4) for w in ("mix", "moe")]


def kernel(**inp):
    inp = {k: np.asarray(v) for k, v in inp.items()}
    B = inp["x"].shape[0]
    xT = [np.concatenate([inp["ctx"][b], inp["x"][b]], axis=0).T for b in range(B)]
    yT = run_stages(inp, ALL_STAGES, xT, n_cores=B)
    out = np.stack([y[:, TC:].T for y in yT], axis=0)
    return np.ascontiguousarray(out, dtype=np.float32)
```

```python
import contextlib
import numpy as np
import concourse.bass as bass
import concourse.mybir as mybir
from concourse.bass_utils import run_bass_kernel_spmd

F32 = mybir.dt.float32
BF16 = mybir.dt.bfloat16
AF = mybir.ActivationFunctionType
ALU = mybir.AluOpType
AX = mybir.AxisListType

D = 1024
NCH = 8
TC = 256
TL = 2048
T = TC + TL
SLABS = [(0, 256), (256, 512), (768, 512), (1280, 512), (1792, 512)]
NEG = -30000.0
N_DSEM = 48
U32 = mybir.dt.uint32
BS = 256
CAP = BS
NBLK_MAX = (2 * T + 32 * (BS - 1) + BS - 1) // BS
NSLOT = NBLK_MAX * BS
EXPERT_ELEMS = 1024 * 512
MOE_SPARSE = True
ROUTE_INTERLEAVE = True


class Buf:
    __slots__ = ("w", "r")

    def __init__(self):
        self.w = None
        self.r = {}


class KB:
    def __init__(self):
        self.nc = bass.Bass("TRN2", target_bir_lowering=False)
        nc = self.nc
        self.es = contextlib.ExitStack()
        self.eng = dict(pe=nc.tensor, act=nc.scalar, dve=nc.vector, pool=nc.gpsimd, sp=nc.sync)
        self.semobj = {}
        for e in ("pe", "act", "dve", "pool"):
            self.semobj[e] = self.es.enter_context(nc.semaphore("s_" + e))
        self.cnt = {e: 0 for e in ("pe", "act", "dve", "pool")}
        self.dval = [0] * N_DSEM
        for i in range(N_DSEM):
            self.semobj[("d", i)] = self.es.enter_context(nc.semaphore("d_%d" % i))
        self.dnext = 0
        self.waited = {e: {} for e in self.eng}
        self.nalloc = 0
        self.bound_reg = None

    def sb(self, stack, shape, dt, name=None):
        self.nalloc += 1
        return stack.enter_context(self.nc.sbuf_tensor("%s_%d" % (name or "t", self.nalloc), list(shape), dt))

    def psum(self, stack, shape, dt=F32, name=None):
        self.nalloc += 1
        return stack.enter_context(self.nc.psum_tensor("%s_%d" % (name or "p", self.nalloc), list(shape), dt))

    def _wait(self, e, key, val):
        w = self.waited[e]
        if w.get(key, 0) < val:
            self.eng[e].wait_ge(self.semobj[key], val)
            w[key] = val

    def _deps(self, e, reads, writes):
        deps = {}
        for b in reads:
            if b.w is not None:
                k, v = b.w
                if deps.get(k, 0) < v:
                    deps[k] = v
        for b in writes:
            if b.w is not None:
                k, v = b.w
                if deps.get(k, 0) < v:
                    deps[k] = v
            for k, v in b.r.items():
                if deps.get(k, 0) < v:
                    deps[k] = v
        for k, v in deps.items():
            if e == "pe" and k == "pe":
                continue
            self._wait(e, k, v)

    def _mark(self, tok, reads, writes):
        k, v = tok
        for b in reads:
            if b.r.get(k, 0) < v:
                b.r[k] = v
        for b in writes:
            b.w = tok
            b.r = {}

    def op(self, e, fn, reads=(), writes=()):
        self._deps(e, reads, writes)
        inst = fn(self.eng[e])
        self.cnt[e] += 1
        inst.then_inc(self.semobj[e], 1)
        self._mark((e, self.cnt[e]), reads, writes)

    def dma(self, q, out, in_, reads=(), writes=(), **kw):
        self._deps(q, reads, writes)
        i = self.dnext
        self.dnext = (i + 1) % N_DSEM
        key = ("d", i)
        if self.dval[i] > 0:
            self._wait(q, key, self.dval[i])
        self.dval[i] += 16
        self.eng[q].dma_start(out=out, in_=in_, **kw).then_inc(self.semobj[key], 16)
        self._mark((key, self.dval[i]), reads, writes)
        return (key, self.dval[i])

    def idma(self, out, in_, idx_ap, scatter, reads=(), writes=(), bound=None):
        q = "pool"
        self._deps(q, reads, writes)
        i = self.dnext
        self.dnext = (i + 1) % N_DSEM
        key = ("d", i)
        if self.dval[i] > 0:
            self._wait(q, key, self.dval[i])
        self.dval[i] += 16
        off = bass.IndirectOffsetOnAxis(ap=idx_ap, axis=0)
        kw = {}
        if bound is not None:
            if self.bound_reg is None:
                self.bound_reg = self.es.enter_context(self.nc.gpsimd.register("bound_reg"))
                self.nc.gpsimd.reg_mov(self.bound_reg, bound)
                self.bound_val = bound
            assert self.bound_val == bound
            kw = dict(bounds_check=self.bound_reg, oob_is_err=False)
        if scatter:
            inst = self.eng[q].indirect_dma_start(out=out, out_offset=off, in_=in_, in_offset=None, **kw)
        else:
            inst = self.eng[q].indirect_dma_start(out=out, out_offset=None, in_=in_, in_offset=off, **kw)
        inst.then_inc(self.semobj[key], 16)
        self._mark((key, self.dval[i]), reads, writes)

    def dma_dyn(self, q, out, tensor, reg, pattern, reads=(), writes=()):
        self._deps(q, reads, writes)
        i = self.dnext
        self.dnext = (i + 1) % N_DSEM
        key = ("d", i)
        if self.dval[i] > 0:
            self._wait(q, key, self.dval[i])
        self.dval[i] += 16
        src = bass.AP(tensor, reg, [list(x) for x in pattern])
        self.eng[q].dma_start(out=out, in_=src).then_inc(self.semobj[key], 16)
        self._mark((key, self.dval[i]), reads, writes)

    def barrier(self):
        for e in self.eng:
            for k in ("pe", "act", "dve", "pool"):
                if k != e and self.cnt[k] > 0:
                    self._wait(e, k, self.cnt[k])
            for i in range(N_DSEM):
                if self.dval[i] > 0:
                    self._wait(e, ("d", i), self.dval[i])

    def mm(self, out, lhsT, rhs, start, stop, reads, writes):
        self.op("pe", lambda e: e.matmul(out, lhsT, rhs, start=start, stop=stop), reads, writes)

    def act(self, out, in_, func, reads, writes, bias=None, scale=None, accum_out=None, eng="act"):
        kw = {}
        if bias is not None:
            kw["bias"] = bias
        if scale is not None:
            kw["scale"] = scale
        if accum_out is not None:
            kw["accum_out"] = accum_out
        self.op("act", lambda e: e.activation(out=out, in_=in_, func=func, **kw), reads, writes)

    def tt(self, e, out, in0, in1, op, reads, writes):
        self.op(e, lambda g: g.tensor_tensor(out=out, in0=in0, in1=in1, op=op), reads, writes)

    def ts(self, e, out, in0, s1, s2, op0, op1, reads, writes):
        if op1 is None:
            self.op(e, lambda g: g.tensor_scalar(out=out, in0=in0, scalar1=s1, scalar2=None, op0=op0), reads, writes)
        else:
            self.op(e, lambda g: g.tensor_scalar(out=out, in0=in0, scalar1=s1, scalar2=s2, op0=op0, op1=op1), reads, writes)

    def stt(self, out, in0, scalar, in1, op0, op1, reads, writes):
        self.op("dve", lambda g: g.scalar_tensor_tensor(out=out, in0=in0, scalar=scalar, in1=in1, op0=op0, op1=op1),
                reads, writes)

    def copy(self, e, out, in_, reads, writes):
        if e == "act":
            self.op("act", lambda g: g.copy(out=out, in_=in_), reads, writes)
        else:
            self.op(e, lambda g: g.tensor_copy(out=out, in_=in_), reads, writes)


PENDING = []


def defer(fn):
    PENDING.append([0, fn])


def run_pending(min_age=0):
    keep = []
    for item in list(PENDING):
        if item[0] >= min_age:
            PENDING.remove(item)
            item[1]()


def pipeline(n, front, back, lag):
    for i in range(n + lag):
        if i < n:
            front(i)
        for item in PENDING:
            item[0] += 1
        run_pending(5)
        if i >= lag:
            back(i - lag)


class Rot:
    def __init__(self, items):
        self.items = items
        self.i = 0

    def next(self):
        it = self.items[self.i]
        self.i = (self.i + 1) % len(self.items)
        return it


DEBUG = False


def build_program(stages, out_lat_only=True):
    kb = KB()
    nc = kb.nc
    es = kb.es

    def din(name, shape):
        return nc.dram_tensor(name, list(shape), F32, kind="ExternalInput").ap()

    xT_d = din("xT", [D, T])
    cond_d = din("cond", [D, 2])
    ada_w_d = din("ada_w", [4, D, 6 * D])
    ada_b_d = din("ada_bT", [4, 128, 48])
    ngm_d = din("norm_mix_gT", [128, 4, 8])
    ngf_d = din("norm_ffn_gT", [128, 4, 8])
    ident_d = din("ident", [128, 128])
    wr_d = din("moe_wr", [4, D, 36])
    br_d = din("moe_br", [4, 36])
    if MOE_SPARSE:
        wg_d = din("moe_w_gate", [4 * 32 * 128 * 2, 2048])
        wu_d = din("moe_w_up", [4 * 32 * 128 * 2, 2048])
        wd_d = din("moe_w_down", [4 * 32 * 128 * 2, 2048])
        pcol2_d = din("pcol2", [128, 1])
    else:
        wg_d = din("moe_w_gate", [4, 32, D, 512])
        wu_d = din("moe_w_up", [4, 32, D, 512])
        wd_d = din("moe_w_down", [4, 32, 512, D])
    na_wqkv_d = din("na_w_qkv", [D, 3 * D])
    na_wo_d = din("na_w_o", [D, D])
    na_g_d = din("na_qk_gT", [128, 2])
    na_bt_d = din("na_bt", [128, 8, 14, 64])
    na_mask_d = din("na_mask", [128, 64])
    sw_wqkv_d = din("sw_w_qkv", [D, 1536])
    sw_wo_d = din("sw_w_o", [D, D])
    sw_g_d = din("sw_qk_gT", [64, 2])
    sw_sink_d = din("sw_sink", [1, 16])
    sw_cos_d = din("sw_cos", [64, TL])
    sw_sin_d = din("sw_sin", [64, TL])
    sw_rt_d = din("sw_rt", [64, 64])
    sw_mask_d = din("sw_mask", [128, 2, 128])
    ml_win_d = din("ml_w_in", [D, 3104])
    ml_wo_d = din("ml_w_o", [D, D])
    ml_bg_d = din("ml_b_gates", [32, 1])
    ml_cc_d = din("ml_cc", [32, 4])
    ml_ng_d = din("ml_norm_gT", [128, 8])
    cap_d = din("caps", [128, 2, 4, 512])
    gl_win_d = din("gl_w_in", [D, 3104])
    gl_wo_d = din("gl_w_o", [D, D])
    gl_wa_d = din("gl_wa", [16, 2, 512])
    gl_ba_d = din("gl_baT", [128, 2, 4])
    gl_ng_d = din("gl_norm_gT", [128, 8])
    m01_d = din("mask01", [128, 2, 4, 512])
    ecap_d = din("ecap", [128, 32])
    umat_d = din("umat", [128, 128])
    xs_d = nc.dram_tensor("moe_xs", [NSLOT, D], BF16).ap()
    ys_d = nc.dram_tensor("moe_ys", [NSLOT, D], BF16).ap()
    yT_d = nc.dram_tensor("yT", [D, T], F32, kind="ExternalOutput").ap()
    dbg = {}
    if DEBUG:
        for nm, shp in (("dbg_mod", [128, 96]), ("dbg_ht", [128, NCH * T]), ("dbg_wt", [32, T]), ("dbg_a1", [128, 32]), ("dbg_wg", [128, NCH * 512]), ("dbg_act", [128, 4 * 512]), ("dbg_pw", [128, 512])):
            dbg[nm] = nc.dram_tensor(nm, shp, F32, kind="ExternalOutput").ap()

    XT = kb.sb(es, [128, NCH, T], F32, "XT")
    XTb = [[Buf() for _ in SLABS] for _ in range(NCH)]
    HT = kb.sb(es, [128, NCH, T], BF16, "HT")
    HTb = [[Buf() for _ in SLABS] for _ in range(NCH)]
    ident = kb.sb(es, [128, 128], F32, "ident")
    identb = Buf()
    ones_f = kb.sb(es, [128, 128], F32, "ones_f")
    ones_fb = Buf()
    ones_h = kb.sb(es, [128, 128], BF16, "ones_h")
    ones_hb = Buf()
    condT = kb.sb(es, [128, NCH, 2], F32, "condT")
    condb = Buf()
    MOD = kb.sb(es, [128, 48, 2], F32, "MOD")
    MODb = Buf()
    A1 = kb.sb(es, [128, 2, NCH, 2], F32, "A1")
    A1b = Buf()
    ngm = kb.sb(es, [128, 4, 8], F32, "ngm")
    ngf = kb.sb(es, [128, 4, 8], F32, "ngf")
    ngb = Buf()
    adab = kb.sb(es, [128, 48], F32, "adab")
    adabb = Buf()
    epsc = kb.sb(es, [128, 1], F32, "epsc")
    epsb = Buf()
    banks = [kb.psum(es, [128, 512], F32, "bank%d" % i) for i in range(8)]
    bankb = [Buf() for _ in range(8)]

    xT_v = xT_d.rearrange("(c p) t -> p c t", p=128)
    for c in range(NCH):
        for j, (s0, n) in enumerate(SLABS):
            kb.dma("sp", XT[:, c, s0:s0 + n], xT_v[:, c, s0:s0 + n], writes=[XTb[c][j]])
    kb.dma("sp", ident[:], ident_d[:, :], writes=[identb])
    kb.dma("sp", condT[:], cond_d.rearrange("(c p) k -> p c k", p=128), writes=[condb])
    kb.dma("sp", ngm[:], ngm_d[:, :, :], writes=[ngb])
    kb.dma("sp", ngf[:], ngf_d[:, :, :], writes=[ngb])
    kb.op("pool", lambda g: g.memset(ones_f[:], 1.0), writes=[ones_fb])
    kb.op("pool", lambda g: g.memset(ones_h[:], 1.0), writes=[ones_hb])
    kb.op("pool", lambda g: g.memset(epsc[:], 1e-6), writes=[epsb])
    kb.act(condT[:], condT[:], AF.Silu, reads=[condb], writes=[condb])

    def compute_mod(l):
        with contextlib.ExitStack() as st:
            NB = 768
            wts = [kb.sb(st, [128, NCH, NB], BF16, "adaw") for _ in range(2)]
            wtb = [Buf(), Buf()]
            condh = kb.sb(st, [128, NCH, 2], BF16, "condh")
            condhb = Buf()
            kb.copy("dve", condh[:], condT[:], reads=[condb], writes=[condhb])
            kb.dma("sp", adab[:], ada_b_d[l], writes=[adabb])
            for blk in range(6 * D // NB):
                w, wb = wts[blk % 2], wtb[blk % 2]
                kb.dma("pool", w[:], ada_w_d[l, :, blk * NB:(blk + 1) * NB].rearrange("(c p) n -> p c n", p=128),
                       writes=[wb])
                for o in range(NB // 128):
                    oc = blk * (NB // 128) + o
                    pb = banks[oc % 2]
                    pbb = bankb[oc % 2]
                    for c in range(NCH):
                        kb.mm(pb[:, 0:2], w[:, c, o * 128:(o + 1) * 128], condh[:, c, :], c == 0, c == NCH - 1,
                              reads=[wb, condhb], writes=[pbb])
                    kb.ts("dve", MOD[:, oc, :], pb[:, 0:2], adab[:, oc:oc + 1], None, ALU.add, None,
                          reads=[pbb, adabb], writes=[MODb])
            for which, (gt, m) in enumerate(((ngm, 1), (ngf, 4))):
                for k in range(2):
                    kb.stt(A1[:, which, :, k], MOD[:, m * 8:(m + 1) * 8, k], 1.0, gt[:, l, :], ALU.add, ALU.mult,
                           reads=[MODb, ngb], writes=[A1b])
            kb.barrier()

    def norm_slab(st_bufs, j, which, shift_m, fs=None, fsb=None, want_ht=True):
        sq_rot, t1_rot, rstd_rot = st_bufs
        s0, n = SLABS[j]
        k = 0 if j == 0 else 1
        pb, pbb = banks[7], bankb[7]
        for c in range(NCH):
            sq, sqb = sq_rot.next()
            kb.act(sq[:, :n], XT[:, c, s0:s0 + n], AF.Square, reads=[XTb[c][j]], writes=[sqb])
            kb.mm(pb[:, :n], ones_h[:], sq[:, :n], c == 0, c == NCH - 1, reads=[ones_hb, sqb], writes=[pbb])
        rstd, rstdb = rstd_rot.next()
        kb.act(rstd[:, :n], pb[:, :n], AF.Sqrt, reads=[pbb, epsb], writes=[rstdb], bias=epsc[:], scale=1.0 / D)
        kb.op("dve", lambda g: g.reciprocal(out=rstd[:, :n], in_=rstd[:, :n]), reads=[rstdb], writes=[rstdb])
        for c in range(NCH):
            t1, t1b = t1_rot.next()
            kb.stt(t1[:, :n], XT[:, c, s0:s0 + n], A1[:, which, c, k:k + 1], rstd[:, :n], ALU.mult, ALU.mult,
                   reads=[XTb[c][j], A1b, rstdb], writes=[t1b])
            sh = MOD[:, shift_m * 8 + c, k:k + 1]
            if fs is None:
                kb.act(HT[:, c, s0:s0 + n], t1[:, :n], AF.Identity, reads=[t1b, MODb], writes=[HTb[c][j]],
                       bias=sh, scale=1.0)
            else:
                kb.act(fs[:, c, :n], t1[:, :n], AF.Identity, reads=[t1b, MODb], writes=[fsb[c]], bias=sh, scale=1.0)
                if want_ht:
                    kb.copy("pool", HT[:, c, s0:s0 + n], fs[:, c, :n], reads=[fsb[c]], writes=[HTb[c][j]])

    def make_norm_bufs(st):
        sq_rot = Rot([(kb.sb(st, [128, 512], BF16, "sq"), Buf()) for _ in range(2)])
        t1_rot = Rot([(kb.sb(st, [128, 512], F32, "t1"), Buf()) for _ in range(2)])
        rstd_rot = Rot([(kb.sb(st, [128, 512], F32, "rstd"), Buf()) for _ in range(2)])
        return sq_rot, t1_rot, rstd_rot

    def moe_stage(l, do_ctx):
        if MOE_SPARSE:
            return moe_stage_sparse(l, do_ctx)
        with contextlib.ExitStack() as st_outer:
            WT = kb.sb(st_outer, [32, T], BF16, "WT")
            WTb = [Buf() for _ in SLABS]
            moe_stage_inner(l, do_ctx, WT, WTb)


    I32 = mybir.dt.int32

    def moe_stage_sparse(l, do_ctx):
        first_slab = 0 if do_ctx else 1
        tiles = list(range(0 if do_ctx else 2, 18))
        ntok = 128 * len(tiles)
        nblk = (2 * ntok + 32 * (BS - 1) + BS - 1) // BS
        with contextlib.ExitStack() as so:
            SLOT = kb.sb(so, [128, 36], U32, "SLOT")
            SLOTb = Buf()
            WK = kb.sb(so, [128, 18, 2], F32, "WK")
            WKb = [Buf() for _ in range(18)]
            BLKI = kb.sb(so, [128, NBLK_MAX, 2], U32, "BLKI")
            BLKIb = Buf()
            wg = [kb.sb(so, [128, NCH, 512], BF16, "wg") for _ in range(2)]
            wu = [kb.sb(so, [128, NCH, 512], BF16, "wu") for _ in range(2)]
            wdn = [kb.sb(so, [128, 4, D], BF16, "wd") for _ in range(2)]
            wgb, wub, wdb = [[Buf(), Buf()] for _ in range(3)]
            identh = kb.sb(so, [128, 128], BF16, "identh")
            identhb = Buf()
            kb.copy("dve", identh[:], ident[:], reads=[identb], writes=[identhb])
            xsb = [[Buf(), Buf()] for _ in range(18)]
            ysb = [[Buf() for _ in range(BS // 128)] for _ in range(NBLK_MAX)]
            ROWS = HT[:].rearrange("p c t -> p (c t)")
            rowsb = [Buf() for _ in range(18)]

            def load_block(b):
                p = b % 2
                for h in range(2):
                    ix = BLKI[:, b, h:h + 1]
                    wmax = 4 * 32 * 128 * 2 - 1
                    kb.idma(wg[p][:, 4 * h:4 * h + 4, :].rearrange("p c n -> p (c n)"), wg_d, ix, False, reads=[BLKIb], writes=[wgb[p]],
                            bound=wmax)
                    kb.idma(wu[p][:, 4 * h:4 * h + 4, :].rearrange("p c n -> p (c n)"), wu_d, ix, False, reads=[BLKIb], writes=[wub[p]],
                            bound=wmax)
                    kb.idma(wdn[p][:, 2 * h:2 * h + 2, :].rearrange("p c n -> p (c n)"), wd_d, ix, False, reads=[BLKIb], writes=[wdb[p]],
                            bound=wmax)

            with contextlib.ExitStack() as st:
                nb = make_norm_bufs(st)
                FS = kb.sb(st, [128, NCH, 512], F32, "FS")
                FSb = [Buf() for _ in range(NCH)]
                WR = kb.sb(st, [128, NCH, 36], F32, "WR")
                WRb = Buf()
                BR = kb.sb(st, [128, 36], F32, "BR")
                BRb = Buf()
                umat = kb.sb(st, [128, 128], F32, "umat")
                cstb = Buf()
                base = kb.sb(st, [128, 32], F32, "base")
                baseb = Buf()
                MS = kb.sb(st, [128, 36, 32], F32, "MS")
                MSb = Buf()
                PK = kb.sb(st, [128, 36], F32, "PK")
                PKb = Buf()
                kb.dma("sp", WR[:], wr_d[l].rearrange("(c p) n -> p c n", p=128), writes=[WRb])
                kb.dma("sp", BR[:], br_d[l:l + 1, :].to_broadcast([128, 36]), writes=[BRb])
                kb.dma("sp", umat[:], umat_d[:, :], writes=[cstb])
                kb.op("pool", lambda g: g.memset(base[:], 0.0), writes=[baseb])
                kb.op("pool", lambda g: g.memset(MS[:], 0.0), writes=[MSb])
                kb.op("pool", lambda g: g.memset(PK[:], 0.0), writes=[PKb])
                rsets = []
                for _ in range(4):
                    d = dict(
                        lgs=kb.sb(st, [128, 36], F32), gmax=kb.sb(st, [128, 1], F32), ngmax=kb.sb(st, [128, 1], F32),
                        ge=kb.sb(st, [128, 4], F32), gsum=kb.sb(st, [128, 1], F32), gw=kb.sb(st, [128, 1], F32),
                        gm=kb.sb(st, [128, 4], F32), pen=kb.sb(st, [128, 4], F32), lem=kb.sb(st, [128, 32], F32),
                        top8=kb.sb(st, [128, 8], F32), dv=kb.sb(st, [128, 1], F32), e21=kb.sb(st, [128, 1], F32),
                        w1=kb.sb(st, [128, 1], F32), mm_=kb.sb(st, [128, 32], F32), pos=kb.sb(st, [128, 32], F32),
                        tmp=kb.sb(st, [128, 32], F32), b=Buf())
                    rsets.append(d)
                rrot = Rot(rsets)
                for j in range(first_slab, len(SLABS)):
                    s0, n = SLABS[j]
                    norm_slab(nb, j, 1, 3, fs=FS, fsb=FSb, want_ht=False)
                    def tile_gen(tt, s0=s0, n=n, j=j):
                        gi = s0 // 128 + tt
                        r = rrot.next()
                        rb = r["b"]
                        pb, pbb = banks[6], bankb[6]
                        for c in range(NCH):
                            kb.mm(pb[:, 0:36], FS[:, c, tt * 128:(tt + 1) * 128], WR[:, c, :], c == 0, c == NCH - 1,
                                  reads=[FSb[c], WRb], writes=[pbb])
                        kb.tt("dve", r["lgs"][:], pb[:, 0:36], BR[:], ALU.add, reads=[pbb, BRb], writes=[rb])
                        yield
                        kb.op("dve", lambda g: g.tensor_reduce(out=r["gmax"][:], in_=r["lgs"][:, 0:4], axis=AX.X, op=ALU.max),
                              reads=[rb], writes=[rb])
                        yield
                        kb.ts("dve", r["ngmax"][:], r["gmax"][:], -1.0, None, ALU.mult, None, reads=[rb], writes=[rb])
                        yield
                        kb.act(r["ge"][:], r["lgs"][:, 0:4], AF.Exp, reads=[rb], writes=[rb], bias=r["ngmax"][:], scale=1.0,
                               accum_out=r["gsum"][:])
                        yield
                        kb.op("dve", lambda g: g.reciprocal(out=r["gw"][:], in_=r["gsum"][:]), reads=[rb], writes=[rb])
                        yield
                        kb.ts("dve", r["gm"][:], r["lgs"][:, 0:4], r["gmax"][:], None, ALU.is_ge, None, reads=[rb], writes=[rb])
                        yield
                        kb.ts("dve", r["pen"][:], r["gm"][:], -1.0, 1e30, ALU.add, ALU.mult, reads=[rb], writes=[rb])
                        yield
                        for g4 in range(4):
                            kb.ts("dve", r["lem"][:, 8 * g4:8 * g4 + 8], r["lgs"][:, 4 + 8 * g4:12 + 8 * g4],
                                  r["gm"][:, g4:g4 + 1], r["pen"][:, g4:g4 + 1], ALU.mult, ALU.add, reads=[rb], writes=[rb])
                        kb.op("dve", lambda g: g.max(out=r["top8"][:], in_=r["lem"][:]), reads=[rb], writes=[rb])
                        yield
                        kb.tt("dve", r["dv"][:], r["top8"][:, 1:2], r["top8"][:, 0:1], ALU.subtract, reads=[rb], writes=[rb])
                        yield
                        kb.act(r["e21"][:], r["dv"][:], AF.Exp, reads=[rb], writes=[rb])
                        yield
                        kb.ts("dve", r["w1"][:], r["e21"][:], 1.0, None, ALU.add, None, reads=[rb], writes=[rb])
                        yield
                        kb.op("dve", lambda g: g.reciprocal(out=r["w1"][:], in_=r["w1"][:]), reads=[rb], writes=[rb])
                        yield
                        kb.tt("dve", WK[:, gi, 0:1], r["w1"][:], r["gw"][:], ALU.mult, reads=[rb], writes=[WKb[gi]])
                        yield
                        kb.tt("dve", WK[:, gi, 1:2], WK[:, gi, 0:1], r["e21"][:], ALU.mult, reads=[rb, WKb[gi]], writes=[WKb[gi]])
                        yield
                        for k in range(2):
                            kb.ts("dve", MS[:, 2 * gi + k, :], r["lem"][:], r["top8"][:, k:k + 1], None, ALU.is_equal, None,
                                  reads=[rb], writes=[MSb])
                        kb.tt("dve", r["mm_"][:], MS[:, 2 * gi, :], MS[:, 2 * gi + 1, :], ALU.add, reads=[MSb], writes=[rb])
                        yield
                        pp, ppb = banks[5], bankb[5]
                        kb.mm(pp[:, 0:32], umat[:, :], r["mm_"][:], True, True, reads=[cstb, rb], writes=[ppb])
                        kb.mm(pp[:, 32:64], ones_f[:, :], r["mm_"][:], True, True, reads=[ones_fb, rb], writes=[ppb])
                        kb.tt("dve", r["pos"][:], pp[:, 0:32], base[:], ALU.add, reads=[ppb, baseb], writes=[rb])
                        kb.tt("dve", base[:], pp[:, 32:64], base[:], ALU.add, reads=[ppb, baseb], writes=[baseb])
                        yield
                        for k in range(2):
                            kb.tt("dve", r["tmp"][:], MS[:, 2 * gi + k, :], r["pos"][:], ALU.mult, reads=[rb, MSb], writes=[rb])
                            kb.op("dve", lambda g: g.tensor_reduce(out=PK[:, 2 * gi + k:2 * gi + k + 1], in_=r["tmp"][:], axis=AX.X,
                                                                    op=ALU.add), reads=[rb], writes=[PKb])
                        for half in range(2):
                            pt_, ptb_ = banks[half], bankb[half]
                            for q in range(4):
                                c = half * 4 + q
                                kb.op("pe", lambda e: e.transpose(out=pt_[:, q * 128:(q + 1) * 128],
                                                                  in_=FS[:, c, tt * 128:(tt + 1) * 128], identity=ident[:]),
                                      reads=[FSb[c], identb], writes=[ptb_])
                            kb.copy("act", ROWS[:, gi * D + half * 512:gi * D + (half + 1) * 512], pt_[:, :], reads=[ptb_],
                                    writes=[rowsb[gi]])


                    gens = [tile_gen(tt) for tt in range(n // 128)]
                    if not ROUTE_INTERLEAVE:
                        for g_ in gens:
                            for _ in g_:
                                pass
                        gens = []
                    while gens:
                        for g_ in list(gens):
                            try:
                                next(g_)
                            except StopIteration:
                                gens.remove(g_)
                padded = kb.sb(st, [128, 32], F32, "padded")
                pend = kb.sb(st, [128, 32], F32, "pend")
                pstart = kb.sb(st, [128, 32], F32, "pstart")
                one32 = kb.sb(st, [128, 32], F32, "one32")
                lay = Buf()
                kb.op("pool", lambda g: g.memset(one32[:], 1.0), writes=[lay])
                kb.ts("dve", padded[:], base[:], 0.0, None, ALU.is_gt, None, reads=[baseb], writes=[lay])
                for m_ in range(1, (2 * T) // BS + 1):
                    kb.stt(padded[:], base[:], float(m_ * BS), padded[:], ALU.is_gt, ALU.add, reads=[baseb, lay], writes=[lay])
                kb.ts("dve", padded[:], padded[:], float(BS), None, ALU.mult, None, reads=[lay], writes=[lay])
                kb.op("dve", lambda g: g.tensor_tensor_scan(out=pend[:], data0=one32[:], data1=padded[:], initial=0.0,
                                                             op0=ALU.mult, op1=ALU.add), reads=[lay], writes=[lay])
                kb.tt("dve", pstart[:], pend[:], padded[:], ALU.subtract, reads=[lay], writes=[lay])
                slotf = kb.sb(st, [128, 36], F32, "slotf")
                tmp32 = kb.sb(st, [128, 32], F32, "tmp32")
                for gi in tiles:
                    for k in range(2):
                        kb.tt("dve", tmp32[:], MS[:, 2 * gi + k, :], pstart[:], ALU.mult, reads=[MSb, lay], writes=[lay])
                        kb.op("dve", lambda g: g.tensor_reduce(out=slotf[:, 2 * gi + k:2 * gi + k + 1], in_=tmp32[:], axis=AX.X,
                                                                op=ALU.add), reads=[lay], writes=[lay])
                kb.tt("dve", slotf[:], slotf[:], PK[:], ALU.add, reads=[lay, PKb], writes=[lay])
                kb.copy("dve", SLOT[:], slotf[:], reads=[lay], writes=[SLOTb])
                blkf = kb.sb(st, [128, NBLK_MAX], F32, "blkf")
                tb = kb.sb(st, [128, 32], F32, "tb")
                pcol2 = kb.sb(st, [128, 1], F32, "pcol2")
                kb.dma("sp", pcol2[:], pcol2_d[:, :], writes=[lay])
                kb.op("pool", lambda g: g.memset(blkf[:], 0.0), writes=[lay])
                for b in range(nblk):
                    kb.ts("dve", tb[:], pend[:, :], float(BS * b), None, ALU.is_le, None, reads=[lay], writes=[lay])
                    kb.op("dve", lambda g: g.tensor_reduce(out=blkf[:, b:b + 1], in_=tb[:], axis=AX.X, op=ALU.add),
                          reads=[lay], writes=[lay])
                blke = kb.sb(st, [128, NBLK_MAX], F32, "blke")
                kb.ts("dve", blke[:], blkf[:], 31.5, 1.0e7, ALU.is_gt, ALU.mult, reads=[lay], writes=[lay])
                kb.ts("dve", blkf[:], blkf[:], 31.0, float(l * 32), ALU.min, ALU.add, reads=[lay], writes=[lay])
                kb.ts("dve", blkf[:], blkf[:], 256.0, pcol2[:, 0:1], ALU.mult, ALU.add, reads=[lay], writes=[lay])
                kb.tt("dve", blkf[:], blkf[:], blke[:], ALU.add, reads=[lay], writes=[lay])
                blkh = kb.sb(st, [128, NBLK_MAX], F32, "blkh")
                kb.copy("dve", BLKI[:, :, 0], blkf[:], reads=[lay], writes=[BLKIb])
                kb.ts("dve", blkh[:], blkf[:], 1.0, None, ALU.add, None, reads=[lay], writes=[lay])
                kb.copy("dve", BLKI[:, :, 1], blkh[:], reads=[lay], writes=[BLKIb])
                load_block(0)
                load_block(1)
                for gi in tiles:
                    for k in range(2):
                        kb.idma(xs_d, ROWS[:, gi * D:(gi + 1) * D], SLOT[:, 2 * gi + k:2 * gi + k + 1], True,
                                reads=[rowsb[gi], SLOTb], writes=[xsb[gi][k]])
                kb.barrier()
            with contextlib.ExitStack() as st:
                XeS = [kb.sb(st, [128, 2, D], BF16, "XeS") for _ in range(2)]
                XeSb = [Buf(), Buf()]
                XeT = [kb.sb(st, [128, NCH, CAP], BF16, "XeT") for _ in range(2)]
                XeTb = [[Buf(), Buf()] for _ in range(2)]
                actT = [kb.sb(st, [128, 4, CAP], BF16, "actT") for _ in range(2)]
                actb = [[Buf() for _ in range(4)] for _ in range(2)]
                sg_rot = Rot([(kb.sb(st, [128, CAP], F32, "sg"), Buf()) for _ in range(2)])
                yb_rot = Rot([(kb.sb(st, [128, D], BF16, "ybuf"), Buf()) for _ in range(2)])
                all_xs = [xsb[gi][k] for gi in tiles for k in range(2)]

                def load_x(e):
                    p = e % 2
                    kb.dma("sp", XeS[p][:], xs_d[e * CAP:(e + 1) * CAP, :].rearrange("(a p) f -> p a f", p=128),
                           reads=all_xs, writes=[XeSb[p]])

                def xpose(e):
                    p = e % 2
                    for a in range(CAP // 128):
                        pt_, ptb_ = banks[6 + a % 2], bankb[6 + a % 2]
                        pv = pt_[:].bitcast(BF16)
                        for c in range(NCH):
                            kb.op("pe", lambda en: en.transpose(out=pv[:, c * 128:(c + 1) * 128], in_=XeS[p][:, a, c * 128:(c + 1) * 128],
                                                                identity=identh[:]),
                                  reads=[XeSb[p], identhb], writes=[ptb_])
                        kb.copy("dve", XeT[p][:, :, a * 128:(a + 1) * 128], pv[:, :].rearrange("p (c s) -> p c s", c=NCH),
                                reads=[ptb_], writes=[XeTb[p][a]])

                load_x(0)
                if nblk > 1:
                    load_x(1)
                xpose(0)
                bi = 0
                for e in range(nblk):
                    p = e % 2
                    if e + 1 < nblk:
                        xpose(e + 1)
                    if e + 2 < nblk:
                        load_x(e + 2)
                    a_ = actT[e % 2]
                    ab = actb[e % 2]
                    for f in range(4):
                        pg, pgb = banks[bi % 3], bankb[bi % 3]
                        bi += 1
                        for c in range(NCH):
                            kb.mm(pg[:, 0:CAP], wg[p][:, c, f * 128:(f + 1) * 128], XeT[p][:, c, :], c == 0, c == NCH - 1,
                                  reads=[wgb[p]] + XeTb[p], writes=[pgb])
                        for c in range(NCH):
                            kb.mm(pg[:, CAP:2 * CAP], wu[p][:, c, f * 128:(f + 1) * 128], XeT[p][:, c, :], c == 0, c == NCH - 1,
                                  reads=[wub[p]] + XeTb[p], writes=[pgb])
                        sg, sgb = sg_rot.next()
                        kb.act(sg[:, :], pg[:, 0:CAP], AF.Silu, reads=[pgb], writes=[sgb])
                        kb.tt("dve", a_[:, f, :], sg[:, :], pg[:, CAP:2 * CAP], ALU.mult, reads=[sgb, pgb], writes=[ab[f]])
                    for a in range(CAP // 128):
                        yb, ybb = yb_rot.next()
                        for half in range(2):
                            py, pyb = banks[3 + half], bankb[3 + half]
                            for f in range(4):
                                kb.mm(py[:, :], a_[:, f, a * 128:(a + 1) * 128], wdn[p][:, f, half * 512:(half + 1) * 512],
                                      f == 0, f == 3, reads=[ab[f], wdb[p]], writes=[pyb])
                            kb.copy("act" if half == 0 else "dve", yb[:, half * 512:(half + 1) * 512], py[:, :], reads=[pyb],
                                    writes=[ybb])
                        kb.dma("sp", ys_d[e * CAP + a * 128:e * CAP + (a + 1) * 128, :], yb[:, :], reads=[ybb], writes=[ysb[e][a]])
                    if e + 2 < nblk:
                        load_block(e + 2)
                kb.barrier()
            with contextlib.ExitStack() as st:
                y_rot = Rot([(kb.sb(st, [128, 2, D], BF16, "ygath"), Buf()) for _ in range(6)])
                ys_rot = Rot([(kb.sb(st, [128, D], F32, "ysum"), Buf()) for _ in range(2)])
                all_ys = [ysb[e][a] for e in range(nblk) for a in range(CAP // 128)]
                for gi in tiles:
                    j = slabs_of(gi * 128, 128)[0]
                    kk = 0 if j == 0 else 1
                    yg, ygb = y_rot.next()
                    for k in range(2):
                        kb.idma(yg[:, k, :], ys_d, SLOT[:, 2 * gi + k:2 * gi + k + 1], False, reads=all_ys + [SLOTb], writes=[ygb])
                    ysum, ysumb = ys_rot.next()
                    kb.ts("dve", ysum[:, :], yg[:, 0, :], WK[:, gi, 0:1], None, ALU.mult, None, reads=[ygb, WKb[gi]], writes=[ysumb])
                    kb.stt(ysum[:, :], yg[:, 1, :], WK[:, gi, 1:2], ysum[:, :], ALU.mult, ALU.add, reads=[ygb, WKb[gi], ysumb],
                           writes=[ysumb])
                    for half in range(2):
                        pt_, ptb_ = banks[half], bankb[half]
                        for q in range(4):
                            c = half * 4 + q
                            kb.op("pe", lambda e: e.transpose(out=pt_[:, q * 128:(q + 1) * 128], in_=ysum[:, c * 128:(c + 1) * 128],
                                                              identity=ident[:]),
                                  reads=[ysumb, identb], writes=[ptb_])
                        for q in range(4):
                            c = half * 4 + q
                            kb.stt(XT[:, c, gi * 128:(gi + 1) * 128], pt_[:, q * 128:(q + 1) * 128], MOD[:, 5 * 8 + c, kk:kk + 1],
                                   XT[:, c, gi * 128:(gi + 1) * 128], ALU.mult, ALU.add, reads=[ptb_, MODb, XTb[c][j]],
                                   writes=[XTb[c][j]])
                kb.barrier()

    def moe_stage_inner(l, do_ctx, WT, WTb):
        with contextlib.ExitStack() as st:
            nb = make_norm_bufs(st)
            FS = kb.sb(st, [128, NCH, 512], F32, "FS")
            FSb = [Buf() for _ in range(NCH)]
            WR = kb.sb(st, [128, NCH, 36], F32, "WR")
            WRb = Buf()
            BR = kb.sb(st, [128, 36], F32, "BR")
            BRb = Buf()
            kb.dma("sp", WR[:], wr_d[l].rearrange("(c p) n -> p c n", p=128), writes=[WRb])
            kb.dma("sp", BR[:], br_d[l:l + 1, :].to_broadcast([128, 36]), writes=[BRb])
            rsets = []
            for _ in range(2):
                d = dict(
                    lgs=kb.sb(st, [128, 36], F32), gmax=kb.sb(st, [128, 1], F32), ngmax=kb.sb(st, [128, 1], F32),
                    ge=kb.sb(st, [128, 4], F32), gsum=kb.sb(st, [128, 1], F32), gw=kb.sb(st, [128, 1], F32),
                    gm=kb.sb(st, [128, 4], F32), pen=kb.sb(st, [128, 4], F32), lem=kb.sb(st, [128, 32], F32),
                    top8=kb.sb(st, [128, 8], F32), dv=kb.sb(st, [128, 1], F32), e21=kb.sb(st, [128, 1], F32),
                    w1=kb.sb(st, [128, 1], F32), w2=kb.sb(st, [128, 1], F32), wd1=kb.sb(st, [128, 32], F32),
                    wd2=kb.sb(st, [128, 32], F32), b=Buf())
                rsets.append(d)
            rrot = Rot(rsets)
            first_slab = 0 if do_ctx else 1
            for j in range(first_slab, len(SLABS)):
                s0, n = SLABS[j]
                norm_slab(nb, j, 1, 3, fs=FS, fsb=FSb)
                for tt in range(n // 128):
                    r = rrot.next()
                    rb = r["b"]
                    pb, pbb = banks[6], bankb[6]
                    for c in range(NCH):
                        kb.mm(pb[:, 0:36], FS[:, c, tt * 128:(tt + 1) * 128], WR[:, c, :], c == 0, c == NCH - 1,
                              reads=[FSb[c], WRb], writes=[pbb])
                    kb.tt("dve", r["lgs"][:], pb[:, 0:36], BR[:], ALU.add, reads=[pbb, BRb], writes=[rb])
                    kb.op("dve", lambda g: g.tensor_reduce(out=r["gmax"][:], in_=r["lgs"][:, 0:4], axis=AX.X, op=ALU.max),
                          reads=[rb], writes=[rb])
                    kb.ts("dve", r["ngmax"][:], r["gmax"][:], -1.0, None, ALU.mult, None, reads=[rb], writes=[rb])
                    kb.act(r["ge"][:], r["lgs"][:, 0:4], AF.Exp, reads=[rb], writes=[rb], bias=r["ngmax"][:], scale=1.0,
                           accum_out=r["gsum"][:])
                    kb.op("dve", lambda g: g.reciprocal(out=r["gw"][:], in_=r["gsum"][:]), reads=[rb], writes=[rb])
                    kb.ts("dve", r["gm"][:], r["lgs"][:, 0:4], r["gmax"][:], None, ALU.is_ge, None, reads=[rb], writes=[rb])
                    kb.ts("dve", r["pen"][:], r["gm"][:], -1.0, 1e30, ALU.add, ALU.mult, reads=[rb], writes=[rb])
                    for g4 in range(4):
                        kb.ts("dve", r["lem"][:, 8 * g4:8 * g4 + 8], r["lgs"][:, 4 + 8 * g4:12 + 8 * g4],
                              r["gm"][:, g4:g4 + 1], r["pen"][:, g4:g4 + 1], ALU.mult, ALU.add, reads=[rb], writes=[rb])
                    kb.op("dve", lambda g: g.max(out=r["top8"][:], in_=r["lem"][:]), reads=[rb], writes=[rb])
                    kb.tt("dve", r["dv"][:], r["top8"][:, 1:2], r["top8"][:, 0:1], ALU.subtract, reads=[rb], writes=[rb])
                    kb.act(r["e21"][:], r["dv"][:], AF.Exp, reads=[rb], writes=[rb])
                    kb.ts("dve", r["w1"][:], r["e21"][:], 1.0, None, ALU.add, None, reads=[rb], writes=[rb])
                    kb.op("dve", lambda g: g.reciprocal(out=r["w1"][:], in_=r["w1"][:]), reads=[rb], writes=[rb])
                    kb.tt("dve", r["w1"][:], r["w1"][:], r["gw"][:], ALU.mult, reads=[rb], writes=[rb])
                    kb.tt("dve", r["w2"][:], r["w1"][:], r["e21"][:], ALU.mult, reads=[rb], writes=[rb])
                    kb.ts("dve", r["wd1"][:], r["lem"][:], r["top8"][:, 0:1], r["w1"][:], ALU.is_equal, ALU.mult,
                          reads=[rb], writes=[rb])
                    kb.ts("dve", r["wd2"][:], r["lem"][:], r["top8"][:, 1:2], r["w2"][:], ALU.is_equal, ALU.mult,
                          reads=[rb], writes=[rb])
                    kb.tt("dve", r["wd1"][:], r["wd1"][:], r["wd2"][:], ALU.add, reads=[rb], writes=[rb])
                    pt, ptb = banks[5], bankb[5]
                    kb.op("pe", lambda e: e.transpose(out=pt[0:32, 0:128], in_=r["wd1"][:], identity=ident[:]),
                          reads=[rb, identb], writes=[ptb])
                    kb.copy("act", WT[:, s0 + tt * 128:s0 + (tt + 1) * 128], pt[0:32, 0:128], reads=[ptb], writes=[WTb[j]])
            kb.barrier()
            if DEBUG:
                kb.dma("pool", dbg["dbg_mod"], MOD[:].rearrange("p a b -> p (a b)"), reads=[MODb])
                kb.dma("pool", dbg["dbg_a1"], A1[:].rearrange("p a b c -> p (a b c)"), reads=[A1b])
                kb.dma("pool", dbg["dbg_ht"], HT[:].rearrange("p a b -> p (a b)"), reads=[x for y in HTb for x in y])
                kb.dma("pool", dbg["dbg_wt"], WT[:], reads=WTb)
                kb.barrier()

        with contextlib.ExitStack() as st:
            wg = [kb.sb(st, [128, NCH, 512], BF16, "wg") for _ in range(2)]
            wu = [kb.sb(st, [128, NCH, 512], BF16, "wu") for _ in range(2)]
            wdn = [kb.sb(st, [128, 4, D], BF16, "wd") for _ in range(2)]
            wgb = [Buf(), Buf()]
            wub = [Buf(), Buf()]
            wdb = [Buf(), Buf()]
            wm_rot = Rot([(kb.sb(st, [32, 512], BF16, "wm"), Buf()) for _ in range(2)])
            sg_rot = Rot([(kb.sb(st, [128, 512], F32, "sg"), Buf()) for _ in range(2)])
            m2_rot = Rot([(kb.sb(st, [128, 512], F32, "m2"), Buf()) for _ in range(2)])
            actT = [kb.sb(st, [128, 4, 512], BF16, "actT") for _ in range(2)]
            actb = [[Buf() for _ in range(4)] for _ in range(2)]
            identh = kb.sb(st, [32, 32], F32, "identh")
            first_slab = 0 if do_ctx else 1
            ai = 0
            for e in range(32):
                p = e % 2
                kb.dma("pool", wg[p][:], wg_d[l, e].rearrange("(c p) n -> p c n", p=128), writes=[wgb[p]])
                kb.dma("pool", wu[p][:], wu_d[l, e].rearrange("(c p) n -> p c n", p=128), writes=[wub[p]])
                kb.dma("pool", wdn[p][:], wd_d[l, e].rearrange("(c p) n -> p c n", p=128), writes=[wdb[p]])
                for j in range(first_slab, len(SLABS)):
                    s0, n = SLABS[j]
                    k = 0 if j == 0 else 1
                    wm, wmb = wm_rot.next()
                    kb.ts("dve", wm[:, :n], WT[:, s0:s0 + n], ident[0:32, e:e + 1], None, ALU.mult, None,
                          reads=[WTb[j], identb], writes=[wmb])
                    pw, pwb = banks[4], bankb[4]
                    kb.mm(pw[:, :n], ones_h[0:32, :], wm[:, :n], True, True, reads=[ones_hb, wmb], writes=[pwb])
                    a = actT[ai % 2]
                    ab = actb[ai % 2]
                    ai += 1
                    for f in range(4):
                        pg, pgb = banks[f % 2], bankb[f % 2]
                        pu, pub = banks[2 + f % 2], bankb[2 + f % 2]
                        for c in range(NCH):
                            kb.mm(pg[:, :n], wg[p][:, c, f * 128:(f + 1) * 128], HT[:, c, s0:s0 + n], c == 0, c == NCH - 1,
                                  reads=[wgb[p], HTb[c][j]], writes=[pgb])
                        for c in range(NCH):
                            kb.mm(pu[:, :n], wu[p][:, c, f * 128:(f + 1) * 128], HT[:, c, s0:s0 + n], c == 0, c == NCH - 1,
                                  reads=[wub[p], HTb[c][j]], writes=[pub])
                        sg, sgb = sg_rot.next()
                        kb.act(sg[:, :n], pg[:, :n], AF.Silu, reads=[pgb], writes=[sgb])
                        m2, m2b = m2_rot.next()
                        kb.tt("dve", m2[:, :n], sg[:, :n], pu[:, :n], ALU.mult, reads=[sgb, pub], writes=[m2b])
                        kb.tt("dve", a[:, f, :n], m2[:, :n], pw[:, :n], ALU.mult, reads=[m2b, pwb], writes=[ab[f]])
                    for oc in range(NCH):
                        py, pyb = banks[5 + oc % 2], bankb[5 + oc % 2]
                        for f in range(4):
                            kb.mm(py[:, :n], wdn[p][:, f, oc * 128:(oc + 1) * 128], a[:, f, :n], f == 0, f == 3,
                                  reads=[wdb[p], ab[f]], writes=[pyb])
                        kb.stt(XT[:, oc, s0:s0 + n], py[:, :n], MOD[:, 5 * 8 + oc, k:k + 1], XT[:, oc, s0:s0 + n],
                               ALU.mult, ALU.add, reads=[pyb, MODb, XTb[oc][j]], writes=[XTb[oc][j]])
            kb.barrier()
            if DEBUG:
                kb.dma("pool", dbg["dbg_wg"], wg[1][:].rearrange("p a b -> p (a b)"))
                kb.dma("pool", dbg["dbg_act"], a[:].rearrange("p a b -> p (a b)"))
                sgx, _ = sg_rot.next()
                kb.copy("dve", sgx[:], banks[4][:], reads=[], writes=[])
                kb.barrier()
                kb.dma("pool", dbg["dbg_pw"], sgx[:])
                kb.barrier()


    wo_tmp = Rot([(kb.sb(es, [128, 512], F32, "wo_tmp"), Buf()) for _ in range(1)])
    def slabs_of(off, n):
        return [j for j, (s0, m) in enumerate(SLABS) if off < s0 + m and off + n > s0]

    def load_cols(t, b, w2d, col0, ncols):
        kb.dma("pool", t[:], w2d[:, col0:col0 + ncols].rearrange("(c p) n -> p c n", p=128), writes=[b])

    def pnorm(nb, src, srcb, P, n, gcol, gb, dst, dstb):
        sq, sqb = nb[0].next()
        kb.act(sq[:P, :n], src, AF.Square, reads=[srcb], writes=[sqb])
        pb, pbb = banks[7], bankb[7]
        kb.mm(pb[:P, :n], ones_h[:P, :P], sq[:P, :n], True, True, reads=[ones_hb, sqb], writes=[pbb])
        rstd, rstdb = nb[2].next()
        kb.act(rstd[:P, :n], pb[:P, :n], AF.Sqrt, reads=[pbb, epsb], writes=[rstdb], bias=epsc[:P], scale=1.0 / P)
        kb.op("dve", lambda g: g.reciprocal(out=rstd[:P, :n], in_=rstd[:P, :n]), reads=[rstdb], writes=[rstdb])
        kb.stt(dst, src, gcol, rstd[:P, :n], ALU.mult, ALU.mult, reads=[srcb, gb, rstdb], writes=[dstb])

    def wo_update(Wo, Wob, kparts, OTs, OTbs, j, py_bank=6):
        s0, n = SLABS[j]
        kk = 0 if j == 0 else 1
        for oc in range(NCH):
            pbk = (6, 7)[oc % 2]
            py, pyb = banks[pbk], bankb[pbk]
            for i, (wap, oap, ob) in enumerate(zip(kparts, OTs, OTbs)):
                kb.mm(py[:, :n], wap(oc), oap, i == 0, i == len(OTs) - 1, reads=[Wob, ob], writes=[pyb])
            if oc % 2 == 0:
                kb.stt(XT[:, oc, s0:s0 + n], py[:, :n], MOD[:, 2 * 8 + oc, kk:kk + 1], XT[:, oc, s0:s0 + n],
                       ALU.mult, ALU.add, reads=[pyb, MODb, XTb[oc][j]], writes=[XTb[oc][j]])
            else:
                wt, wtb_ = wo_tmp.next()
                kb.act(wt[:, :n], py[:, :n], AF.Copy, reads=[pyb, MODb], writes=[wtb_], scale=MOD[:, 2 * 8 + oc, kk:kk + 1])
                kb.tt("pool", XT[:, oc, s0:s0 + n], XT[:, oc, s0:s0 + n], wt[:, :n], ALU.add, reads=[wtb_, XTb[oc][j]],
                      writes=[XTb[oc][j]])

    def mixer_na(l, need_ctx):
        with contextlib.ExitStack() as st:
            nb = make_norm_bufs(st)
            for j in range(len(SLABS)):
                norm_slab(nb, j, 0, 0)
            gqk = kb.sb(st, [128, 2], F32, "gqk")
            gqkb = Buf()
            kb.dma("sp", gqk[:], na_g_d[:, :], writes=[gqkb])
            kb.ts("dve", gqk[:, 0:1], gqk[:, 0:1], 128.0 ** -0.5, None, ALU.mult, None, reads=[gqkb], writes=[gqkb])
            maskt = kb.sb(st, [128, 64], BF16, "maskt")
            masktb = Buf()
            kb.dma("pool", maskt[:], na_mask_d[:, :], writes=[masktb])
            identh = kb.sb(st, [128, 128], BF16, "identh")
            identhb = Buf()
            kb.copy("dve", identh[:], ident[:], reads=[identb], writes=[identhb])
            BTm = kb.sb(st, [128, 8, 14, 64], BF16, "BTm")
            BTmb = [Buf() for _ in range(8)]
            btf_rot = Rot([(kb.sb(st, [128, 14, 64], BF16, "btf"), Buf()) for _ in range(2)])
            for h in range(8):
                btf, btfb = btf_rot.next()
                kb.dma("pool", btf[:], na_bt_d[:, h], writes=[btfb])
                for d in range(14):
                    kb.tt("pool", BTm[:, h, d, :], btf[:, d, :], maskt[:], ALU.add, reads=[btfb, masktb], writes=[BTmb[h]])
            Wq = [kb.sb(st, [128, NCH, 128], BF16, "Wq") for _ in range(2)]
            Wk = [kb.sb(st, [128, NCH, 128], BF16, "Wk") for _ in range(2)]
            Wv = [kb.sb(st, [128, NCH, 128], BF16, "Wv") for _ in range(2)]
            Wo = [kb.sb(st, [128, D], BF16, "Wo") for _ in range(2)]
            Wqb, Wkb, Wvb, Wob = [[Buf(), Buf()] for _ in range(4)]
            qf = kb.sb(st, [128, T], F32, "qf")
            kf = kb.sb(st, [128, T], F32, "kf")
            qfb = [Buf() for _ in SLABS]
            kfb = [Buf() for _ in SLABS]
            qT = kb.sb(st, [128, T], BF16, "qT")
            kT = kb.sb(st, [128, T], BF16, "kT")
            qTb = [Buf() for _ in SLABS]
            kTb = [Buf() for _ in SLABS]
            NV = 33
            V = kb.sb(st, [128, NV, 128], BF16, "V")
            Vb = [Buf() for _ in range(NV)]
            voff = [0, 128] + [256 + 128 * a for a in range(16)] + [256 + 64 + 128 * a for a in range(15)]
            pt_rot = Rot([(kb.sb(st, [128, 512], BF16, "PT"), Buf()) for _ in range(4)])
            ot_rot = Rot([(kb.sb(st, [128, 512], BF16, "OT"), Buf()) for _ in range(3)])
            rec_rot = Rot([(kb.sb(st, [128, 512], F32, "rec"), Buf()) for _ in range(2)])

            def load_head(h):
                p = h % 2
                load_cols(Wq[p], Wqb[p], na_wqkv_d, h * 128, 128)
                load_cols(Wk[p], Wkb[p], na_wqkv_d, D + h * 128, 128)
                load_cols(Wv[p], Wvb[p], na_wqkv_d, 2 * D + h * 128, 128)
                kb.dma("pool", Wo[p][:], na_wo_d[h * 128:(h + 1) * 128, :], writes=[Wob[p]])

            def finalize(h, j, numb, denb, nbk, dbk):
                p = h % 2
                s0, n = SLABS[j]
                rec, recb = rec_rot.next()
                kb.op("dve", lambda g: g.reciprocal(out=rec[:, :n], in_=dbk[:, :n]), reads=[denb], writes=[recb])
                ot, otb = ot_rot.next()
                kb.tt("dve", ot[:, :n], nbk[:, :n], rec[:, :n], ALU.mult, reads=[numb, recb], writes=[otb])
                defer(lambda p=p, ot=ot, otb=otb, n=n, j=j: wo_update(
                    Wo[p], Wob[p], [lambda oc: Wo[p][:, oc * 128:(oc + 1) * 128]], [ot[:, :n]], [otb], j))

            load_head(0)
            for h in range(8):
                p = h % 2
                if h + 1 < 8:
                    load_head(h + 1)
                for j, (s0, n) in enumerate(SLABS):
                    for (W_, Wb_, dstf, dstfb) in ((Wq[p], Wqb[p], qf, qfb), (Wk[p], Wkb[p], kf, kfb)):
                        pb, pbb = banks[6], bankb[6]
                        for c in range(NCH):
                            kb.mm(pb[:, :n], W_[:, c, :], HT[:, c, s0:s0 + n], c == 0, c == NCH - 1,
                                  reads=[Wb_, HTb[c][j]], writes=[pbb])
                        kb.copy("act", dstf[:, s0:s0 + n], pb[:, :n], reads=[pbb], writes=[dstfb[j]])
                    pnorm(nb, qf[:, s0:s0 + n], qfb[j], 128, n, gqk[:, 0:1], gqkb, qT[:, s0:s0 + n], qTb[j])
                    pnorm(nb, kf[:, s0:s0 + n], kfb[j], 128, n, gqk[:, 1:2], gqkb, kT[:, s0:s0 + n], kTb[j])
                for vi in range(NV):
                    off = voff[vi]
                    sl = slabs_of(off, 128)
                    pb, pbb = banks[6], bankb[6]
                    for c in range(NCH):
                        kb.mm(pb[:, 0:128], HT[:, c, off:off + 128], Wv[p][:, c, :], c == 0, c == NCH - 1,
                              reads=[Wvb[p]] + [HTb[c][jj] for jj in sl], writes=[pbb])
                    kb.copy("act", V[:, vi, :], pb[:, 0:128], reads=[pbb], writes=[Vb[vi]])
                if need_ctx:
                    sb_, sbb = banks[0], bankb[0]
                    for i in range(2):
                        kb.mm(sb_[:, 256 * i:256 * i + 256], kT[:, 128 * i:128 * i + 128], qT[:, 0:256], True, True,
                              reads=[kTb[0], qTb[0]], writes=[sbb])
                    pt, ptb = pt_rot.next()
                    kb.act(pt[:, :], sb_[:, :], AF.Exp, reads=[sbb], writes=[ptb])
                    nbk, numb = banks[2], bankb[2]
                    dbk, denb = banks[4], bankb[4]
                    for i in range(2):
                        kb.mm(nbk[:, 0:256], V[:, i, :], pt[:, 256 * i:256 * i + 256], i == 0, i == 1,
                              reads=[Vb[i], ptb], writes=[numb])
                    for i in range(2):
                        kb.mm(dbk[:, 0:256], ones_h[:, :], pt[:, 256 * i:256 * i + 256], i == 0, i == 1,
                              reads=[ones_hb, ptb], writes=[denb])
                    finalize(h, 0, numb, denb, nbk, dbk)
                SBK = (0, 1, 6, 7)
                rows_state = {}

                def front(r, h=h):
                    jj = 1 + r // 8
                    r0 = min(max(r - 4, 0), 24)
                    toff = 256 + 64 * r
                    sb_, sbb = banks[SBK[r % 4]], bankb[SBK[r % 4]]
                    vis = []
                    for i in range(4):
                        kr = r0 + 2 * i
                        koff = 256 + 64 * kr
                        d = kr - r + 7
                        kb.mm(sb_[:, 64 * i:64 * i + 64], kT[:, koff:koff + 128], qT[:, toff:toff + 64], True, False,
                              reads=[kTb[x] for x in slabs_of(koff, 128)] + [qTb[jj]], writes=[sbb])
                        kb.mm(sb_[:, 64 * i:64 * i + 64], identh[:, :], BTm[:, h, d, :], False, True,
                              reads=[identhb, BTmb[h]], writes=[sbb])
                        vis.append(2 + kr // 2 if kr % 2 == 0 else 18 + (kr - 1) // 2)
                    for i in range(2):
                        kb.mm(sb_[:, 256 + 64 * i:256 + 64 * i + 64], kT[:, 128 * i:128 * i + 128], qT[:, toff:toff + 64],
                              True, True, reads=[kTb[0], qTb[jj]], writes=[sbb])
                        vis.append(i)
                    pt, ptb = pt_rot.next()
                    kb.act(pt[:, 0:384], sb_[:, 0:384], AF.Exp, reads=[sbb], writes=[ptb])
                    rows_state[r] = (pt, ptb, vis)

                def back(r, h=h):
                    jj = 1 + r // 8
                    rr = r % 8
                    nbk, numb = banks[2 + jj % 2], bankb[2 + jj % 2]
                    dbk, denb = banks[4 + jj % 2], bankb[4 + jj % 2]
                    pt, ptb, vis = rows_state.pop(r)
                    for i, vi in enumerate(vis):
                        kb.mm(nbk[:, 64 * rr:64 * rr + 64], V[:, vi, :], pt[:, 64 * i:64 * i + 64], i == 0, i == 5,
                              reads=[Vb[vi], ptb], writes=[numb])
                    for i, vi in enumerate(vis):
                        kb.mm(dbk[:, 64 * rr:64 * rr + 64], ones_h[:, :], pt[:, 64 * i:64 * i + 64], i == 0, i == 5,
                              reads=[ones_hb, ptb], writes=[denb])
                    if rr == 7:
                        finalize(h, jj, numb, denb, nbk, dbk)

                pipeline(32, front, back, 3)
                run_pending()
            kb.barrier()


    def mixer_sw(l, need_ctx):
        with contextlib.ExitStack() as st:
            nb = make_norm_bufs(st)
            for j in range(len(SLABS)):
                norm_slab(nb, j, 0, 0)
            gqk = kb.sb(st, [64, 2], F32, "gqk")
            gqkb = Buf()
            kb.dma("sp", gqk[:], sw_g_d[:, :], writes=[gqkb])
            kb.ts("dve", gqk[:, 0:1], gqk[:, 0:1], 64.0 ** -0.5, None, ALU.mult, None, reads=[gqkb], writes=[gqkb])
            sinke = kb.sb(st, [128, 16], F32, "sinke")
            sinkb = Buf()
            kb.dma("sp", sinke[:], sw_sink_d[0:1, :].to_broadcast([128, 16]), writes=[sinkb])
            kb.act(sinke[:], sinke[:], AF.Exp, reads=[sinkb], writes=[sinkb])
            cosT = kb.sb(st, [64, TL], F32, "cosT")
            sinT = kb.sb(st, [64, TL], F32, "sinT")
            RT = kb.sb(st, [64, 64], F32, "RT")
            ropeb = Buf()
            kb.dma("sp", cosT[:], sw_cos_d[:, :], writes=[ropeb])
            kb.dma("sp", sinT[:], sw_sin_d[:, :], writes=[ropeb])
            kb.dma("sp", RT[:], sw_rt_d[:, :], writes=[ropeb])
            bmask = kb.sb(st, [128, 2, 128], BF16, "bmask")
            bmaskb = Buf()
            kb.dma("pool", bmask[:], sw_mask_d[:, :, :], writes=[bmaskb])
            identh = kb.sb(st, [128, 128], BF16, "identh")
            identhb = Buf()
            kb.copy("dve", identh[:], ident[:], reads=[identb], writes=[identhb])
            Wq = [kb.sb(st, [128, NCH, 64], BF16, "Wq") for _ in range(2)]
            Wo = [kb.sb(st, [64, D], BF16, "Wo") for _ in range(2)]
            Wk = kb.sb(st, [128, NCH, 64], BF16, "Wk")
            Wv = kb.sb(st, [128, NCH, 64], BF16, "Wv")
            Wqb, Wob = [[Buf(), Buf()] for _ in range(2)]
            Wkb, Wvb = Buf(), Buf()
            xf = kb.sb(st, [64, T], F32, "xf")
            xfb = [Buf() for _ in SLABS]
            xn = kb.sb(st, [64, T], F32, "xn")
            xnb = [Buf() for _ in SLABS]
            qT = kb.sb(st, [64, T], BF16, "qT")
            kT = kb.sb(st, [64, T], BF16, "kT")
            qTb = [Buf() for _ in SLABS]
            kTb = [Buf() for _ in SLABS]
            V = kb.sb(st, [128, 18, 64], BF16, "V")
            Vb = [Buf() for _ in range(18)]
            rt1_rot = Rot([(kb.sb(st, [64, 512], F32, "rt1"), Buf()) for _ in range(2)])
            rt2_rot = Rot([(kb.sb(st, [64, 512], F32, "rt2"), Buf()) for _ in range(2)])
            pt_rot = Rot([(kb.sb(st, [128, 640], BF16, "PT"), Buf()) for _ in range(4)])
            ot_rot = Rot([(kb.sb(st, [64, 512], BF16, "OT"), Buf()) for _ in range(3)])
            rec_rot = Rot([(kb.sb(st, [64, 512], F32, "rec"), Buf()) for _ in range(2)])

            def project_norm_rope(W_, Wb_, gcol, dst, dstb):
                for j, (s0, n) in enumerate(SLABS):
                    pb, pbb = banks[6 + j % 2], bankb[6 + j % 2]
                    for c in range(NCH):
                        kb.mm(pb[:64, :n], W_[:, c, :], HT[:, c, s0:s0 + n], c == 0, c == NCH - 1,
                              reads=[Wb_, HTb[c][j]], writes=[pbb])
                    kb.copy("act", xf[:, s0:s0 + n], pb[:64, :n], reads=[pbb], writes=[xfb[j]])
                for j, (s0, n) in enumerate(SLABS):
                    if j == 0:
                        pnorm(nb, xf[:, s0:s0 + n], xfb[j], 64, n, gcol, gqkb, dst[:, s0:s0 + n], dstb[j])
                    else:
                        pnorm(nb, xf[:, s0:s0 + n], xfb[j], 64, n, gcol, gqkb, xn[:, s0:s0 + n], xnb[j])
                for j, (s0, n) in enumerate(SLABS):
                    if j == 0:
                        continue
                    l0 = s0 - TC
                    pb, pbb = banks[6], bankb[6]
                    kb.mm(pb[:64, :n], RT[:, :], xn[:, s0:s0 + n], True, True, reads=[ropeb, xnb[j]], writes=[pbb])
                    t1, t1b = rt1_rot.next()
                    kb.tt("pool", t1[:, :n], xn[:, s0:s0 + n], cosT[:, l0:l0 + n], ALU.mult, reads=[xnb[j], ropeb],
                          writes=[t1b])
                    t2, t2b = rt2_rot.next()
                    kb.tt("dve", t2[:, :n], pb[:64, :n], sinT[:, l0:l0 + n], ALU.mult, reads=[pbb, ropeb], writes=[t2b])
                    kb.tt("dve", dst[:, s0:s0 + n], t1[:, :n], t2[:, :n], ALU.add, reads=[t1b, t2b], writes=[dstb[j]])

            def finalize(h, j, numb, denb, nbk, dbk):
                p = h % 2
                s0, n = SLABS[j]
                rec, recb = rec_rot.next()
                kb.ts("dve", rec[:, :n], dbk[:64, :n], sinke[:64, h:h + 1], None, ALU.add, None, reads=[denb, sinkb],
                      writes=[recb])
                kb.op("dve", lambda g: g.reciprocal(out=rec[:, :n], in_=rec[:, :n]), reads=[recb], writes=[recb])
                ot, otb = ot_rot.next()
                kb.tt("dve", ot[:, :n], nbk[:64, :n], rec[:, :n], ALU.mult, reads=[numb, recb], writes=[otb])
                defer(lambda p=p, ot=ot, otb=otb, n=n, j=j: wo_update(
                    Wo[p], Wob[p], [lambda oc: Wo[p][:, oc * 128:(oc + 1) * 128]], [ot[:, :n]], [otb], j))

            def load_q(h):
                p = h % 2
                load_cols(Wq[p], Wqb[p], sw_wqkv_d, h * 64, 64)
                kb.dma("pool", Wo[p][:], sw_wo_d[h * 64:(h + 1) * 64, :], writes=[Wob[p]])

            load_q(0)
            for g in range(4):
                load_cols(Wk, Wkb, sw_wqkv_d, 1024 + g * 64, 64)
                load_cols(Wv, Wvb, sw_wqkv_d, 1280 + g * 64, 64)
                project_norm_rope(Wk, Wkb, gqk[:, 1:2], kT, kTb)
                for vi in range(18):
                    off = 128 * vi
                    sl = slabs_of(off, 128)
                    pb, pbb = banks[6], bankb[6]
                    for c in range(NCH):
                        kb.mm(pb[:, 0:64], HT[:, c, off:off + 128], Wv[:, c, :], c == 0, c == NCH - 1,
                              reads=[Wvb] + [HTb[c][jj] for jj in sl], writes=[pbb])
                    kb.copy("act", V[:, vi, :], pb[:, 0:64], reads=[pbb], writes=[Vb[vi]])
                for hh in range(4):
                    h = 4 * g + hh
                    p = h % 2
                    if h + 1 < 16:
                        load_q(h + 1)
                    project_norm_rope(Wq[p], Wqb[p], gqk[:, 0:1], qT, qTb)
                    nbk, numb = banks[4], bankb[4]
                    dbk, denb = banks[5], bankb[5]
                    if need_ctx:
                        sb_, sbb = banks[0], bankb[0]
                        for i in range(2):
                            kb.mm(sb_[:, 256 * i:256 * i + 256], kT[:, 128 * i:128 * i + 128], qT[:, 0:256], True, True,
                                  reads=[kTb[0], qTb[0]], writes=[sbb])
                        pt, ptb = pt_rot.next()
                        kb.act(pt[:, 0:512], sb_[:, :], AF.Exp, reads=[sbb], writes=[ptb])
                        for i in range(2):
                            kb.mm(nbk[:64, 0:256], V[:, i, :], pt[:, 256 * i:256 * i + 256], i == 0, i == 1,
                                  reads=[Vb[i], ptb], writes=[numb])
                        for i in range(2):
                            kb.mm(dbk[:64, 0:256], ones_h[:, 0:64], pt[:, 256 * i:256 * i + 256], i == 0, i == 1,
                                  reads=[ones_hb, ptb], writes=[denb])
                        finalize(h, 0, numb, denb, nbk, dbk)
                    SA = (0, 1, 6)
                    SC = (2, 3, 7)
                    blk_state = {}

                    def front(jb, h=h):
                        jj = 1 + jb // 4
                        toff = TC + 128 * jb
                        sa, sab = banks[SA[jb % 3]], bankb[SA[jb % 3]]
                        sc, scb = banks[SC[jb % 3]], bankb[SC[jb % 3]]
                        tiles = []
                        for w_i, dk_ in enumerate((-1, 0, 1)):
                            kbk = jb + dk_
                            if kbk < 0 or kbk > 15:
                                continue
                            koff = TC + 128 * kbk
                            cs = 128 * w_i
                            kb.mm(sa[:, cs:cs + 128], kT[:, koff:koff + 128], qT[:, toff:toff + 128], True, dk_ == 0,
                                  reads=[kTb[x] for x in slabs_of(koff, 128)] + [qTb[jj]], writes=[sab])
                            if dk_ != 0:
                                kb.mm(sa[:, cs:cs + 128], identh[:, :], bmask[:, 0 if dk_ < 0 else 1, :], False, True,
                                      reads=[identhb, bmaskb], writes=[sab])
                            tiles.append((cs, 2 + kbk))
                        for i in range(2):
                            kb.mm(sc[:, 128 * i:128 * i + 128], kT[:, 128 * i:128 * i + 128], qT[:, toff:toff + 128], True, True,
                                  reads=[kTb[0], qTb[jj]], writes=[scb])
                            tiles.append((384 + 128 * i, i))
                        pt, ptb = pt_rot.next()
                        c_lo = tiles[0][0]
                        c_hi = max(c for c, _ in tiles if c < 384) + 128
                        kb.act(pt[:, c_lo:c_hi], sa[:, c_lo:c_hi], AF.Exp, reads=[sab], writes=[ptb])
                        kb.act(pt[:, 384:640], sc[:, 0:256], AF.Exp, reads=[scb], writes=[ptb])
                        blk_state[jb] = (pt, ptb, tiles)

                    def back(jb, h=h, nbk=nbk, dbk=dbk, numb=numb, denb=denb):
                        jj = 1 + jb // 4
                        pt, ptb, tiles = blk_state.pop(jb)
                        q4 = jb % 4
                        for i, (cs, vi) in enumerate(tiles):
                            kb.mm(nbk[:64, 128 * q4:128 * q4 + 128], V[:, vi, :], pt[:, cs:cs + 128], i == 0, i == len(tiles) - 1,
                                  reads=[Vb[vi], ptb], writes=[numb])
                        for i, (cs, vi) in enumerate(tiles):
                            kb.mm(dbk[:64, 128 * q4:128 * q4 + 128], ones_h[:, 0:64], pt[:, cs:cs + 128], i == 0,
                                  i == len(tiles) - 1, reads=[ones_hb, ptb], writes=[denb])
                        if q4 == 3:
                            finalize(h, jj, numb, denb, nbk, dbk)

                    pipeline(16, front, back, 2)
                    run_pending()
            kb.barrier()


    def tile_list(d, j):
        s0, n = SLABS[j]
        out = []
        for i in range(18):
            k0 = 128 * i
            if d == 0:
                if k0 + 128 <= s0:
                    out.append((i, "full", 0))
                elif k0 < s0 + n:
                    out.append((i, "diag", (k0 - s0) // 128))
            else:
                if j == 0:
                    if i < 2:
                        out.append((i, "diag", i))
                else:
                    if i < 2 or k0 >= s0 + n:
                        out.append((i, "full", 0))
                    elif k0 + 128 > s0:
                        out.append((i, "diag", (k0 - s0) // 128))
        return out

    def mixer_ml(l, need_ctx):
        with contextlib.ExitStack() as st:
            nb = make_norm_bufs(st)
            for j in range(len(SLABS)):
                norm_slab(nb, j, 0, 0)
            F_ = kb.sb(st, [32, T], F32, "F")
            Fb = [Buf() for _ in SLABS]
            BIAS = kb.sb(st, [128, 18, 16], F32, "BIAS")
            BIASb = Buf()
            caps = kb.sb(st, [128, 2, 4, 512], BF16, "caps")
            capsb = Buf()
            kb.dma("pool", caps[:, 0], cap_d[:, 0], writes=[capsb])
            kb.dma("pool", caps[:, 1], cap_d[:, 1], writes=[capsb])
            ngc = kb.sb(st, [128, 8], F32, "ngc")
            ngcb = Buf()
            kb.dma("sp", ngc[:], ml_ng_d[:, :], writes=[ngcb])
            with contextlib.ExitStack() as st2:
                Wg = kb.sb(st2, [128, NCH, 32], BF16, "Wg")
                Wgb = Buf()
                load_cols(Wg, Wgb, ml_win_d, 3072, 32)
                cc = kb.sb(st2, [32, 4], F32, "cc")
                bcol = kb.sb(st2, [32, 1], F32, "bcol")
                ccb = Buf()
                kb.dma("sp", cc[:], ml_cc_d[:, :], writes=[ccb])
                kb.dma("sp", bcol[:], ml_bg_d[:, :], writes=[ccb])
                kb.ts("dve", bcol[:], bcol[:], 1.0 / 15.0, None, ALU.mult, None, reads=[ccb], writes=[ccb])
                G_ = kb.sb(st2, [32, T], F32, "G")
                Gb = [Buf() for _ in SLABS]
                CS = kb.sb(st2, [32, T], F32, "CS")
                CSb = [Buf() for _ in SLABS]
                one32 = kb.sb(st2, [32, 512], F32, "one32")
                one32b = Buf()
                kb.op("pool", lambda g: g.memset(one32[:], 1.0), writes=[one32b])
                pre = kb.sb(st2, [32, 512], F32, "pre")
                lt = kb.sb(st2, [32, 512], F32, "lt")
                tmpb = Buf()
                totc = kb.sb(st2, [32, 1], F32, "totc")
                for j, (s0, n) in enumerate(SLABS):
                    pb, pbb = banks[6], bankb[6]
                    for c in range(NCH):
                        kb.mm(pb[:32, :n], Wg[:, c, :], HT[:, c, s0:s0 + n], c == 0, c == NCH - 1,
                              reads=[Wgb, HTb[c][j]], writes=[pbb])
                    kb.act(pre[:, :n], pb[:32, :n], AF.Tanh, reads=[pbb, ccb], writes=[tmpb], bias=bcol[:], scale=1.0 / 15.0)
                    kb.ts("dve", pre[:, :n], pre[:, :n], 15.0, None, ALU.mult, None, reads=[tmpb], writes=[tmpb])
                    kb.act(lt[:, :n], pre[:, :n], AF.Exp, reads=[tmpb], writes=[tmpb], scale=-1.0)
                    kb.ts("dve", lt[:, :n], lt[:, :n], 1.0, None, ALU.add, None, reads=[tmpb], writes=[tmpb])
                    kb.act(lt[:, :n], lt[:, :n], AF.Ln, reads=[tmpb], writes=[tmpb])
                    kb.stt(lt[:, :n], lt[:, :n], -1.0, pre[:, :n], ALU.mult, ALU.subtract, reads=[tmpb], writes=[tmpb])
                    kb.stt(G_[:, s0:s0 + n], lt[:, :n], cc[:, 0:1], pre[:, :n], ALU.mult, ALU.add, reads=[tmpb, ccb],
                           writes=[Gb[j]])
                    init = 0.0 if j == 0 else CS[:, s0 - 1:s0]
                    kb.op("dve", lambda g: g.tensor_tensor_scan(out=CS[:, s0:s0 + n], data0=one32[:, :n], data1=G_[:, s0:s0 + n],
                                                                 initial=init, op0=ALU.mult, op1=ALU.add),
                          reads=[one32b, Gb[j]] + ([CSb[j - 1]] if j else []), writes=[CSb[j]])
                kb.tt("dve", totc[:], CS[:, T - 1:T], cc[:, 3:4], ALU.mult, reads=[CSb[4], ccb], writes=[tmpb])
                for j, (s0, n) in enumerate(SLABS):
                    kb.ts("dve", F_[:, s0:s0 + n], CS[:, s0:s0 + n], cc[:, 1:2], None, ALU.mult, None, reads=[CSb[j], ccb],
                          writes=[Fb[j]])
                    kb.stt(F_[:, s0:s0 + n], G_[:, s0:s0 + n], cc[:, 2:3], F_[:, s0:s0 + n], ALU.mult, ALU.add,
                           reads=[Gb[j], ccb, Fb[j]], writes=[Fb[j]])
                    if j > 0:
                        kb.ts("dve", F_[:, s0:s0 + n], F_[:, s0:s0 + n], totc[:], None, ALU.add, None, reads=[Fb[j], tmpb],
                              writes=[Fb[j]])
                colF = kb.sb(st2, [128, 32], F32, "colF")
                colFb = Buf()
                for i in range(18):
                    sl = slabs_of(128 * i, 128)
                    pb, pbb = banks[6], bankb[6]
                    kb.op("pe", lambda e: e.transpose(out=pb[:, 0:32], in_=F_[:, 128 * i:128 * i + 128], identity=ident[0:32, 0:32]),
                          reads=[Fb[x] for x in sl] + [identb], writes=[pbb])
                    kb.op("pe", lambda e: e.transpose(out=pb[:, 32:64], in_=G_[:, 128 * i:128 * i + 128], identity=ident[0:32, 0:32]),
                          reads=[Gb[x] for x in sl] + [identb], writes=[pbb])
                    kb.copy("act", colF[:], pb[:, 0:32], reads=[pbb], writes=[colFb])
                    gv = pb[:, 32:64].rearrange("p (d f h) -> p d f h", d=2, f=2)[:, :, 0, :]
                    fv = colF[:].rearrange("p (d f h) -> p d f h", d=2, f=2)[:, :, 1, :]
                    kb.tt("dve", BIAS[:, i, :].rearrange("p (d h) -> p d h", d=2), gv, fv, ALU.subtract,
                          reads=[pbb, colFb], writes=[BIASb])
                kb.barrier()
            Wq = [kb.sb(st, [128, NCH, 64], BF16, "Wq") for _ in range(2)]
            Wk = [kb.sb(st, [128, NCH, 64], BF16, "Wk") for _ in range(2)]
            Wv = [kb.sb(st, [128, NCH, 128], BF16, "Wv") for _ in range(2)]
            Wog = [kb.sb(st, [128, NCH, 128], BF16, "Wog") for _ in range(2)]
            Wo = [kb.sb(st, [128, D], BF16, "Wo") for _ in range(2)]
            Wqb, Wkb, Wvb, Wogb, Wob = [[Buf(), Buf()] for _ in range(5)]
            qT = kb.sb(st, [64, T], BF16, "qT")
            kT = kb.sb(st, [64, T], BF16, "kT")
            qTb = [Buf() for _ in SLABS]
            kTb = [Buf() for _ in SLABS]
            V = kb.sb(st, [128, 18, 128], BF16, "V")
            Vb = [Buf() for _ in range(18)]
            OG = kb.sb(st, [128, 512], F32, "OG")
            OGb = Buf()
            Hs = kb.sb(st, [128, 512], F32, "Hs")
            Hsb = Buf()
            Yn = kb.sb(st, [128, 512], F32, "Yn")
            Ynb = Buf()
            Yh = kb.sb(st, [128, 512], BF16, "Yh")
            Yhb = Buf()
            fm_rot = Rot([(kb.sb(st, [32, 512], F32, "Fm"), Buf()) for _ in range(2)])
            dt_rot = Rot([(kb.sb(st, [128, 512], F32, "Dt"), Buf()) for _ in range(4)])
            pt_rot = Rot([(kb.sb(st, [128, 512], BF16, "PT"), Buf()) for _ in range(5)])
            rec_rot = Rot([(kb.sb(st, [128, 512], F32, "rec"), Buf()) for _ in range(2)])

            def load_head(h):
                p = h % 2
                load_cols(Wq[p], Wqb[p], ml_win_d, h * 64, 64)
                load_cols(Wk[p], Wkb[p], ml_win_d, 512 + h * 64, 64)
                load_cols(Wv[p], Wvb[p], ml_win_d, 1024 + h * 128, 128)
                load_cols(Wog[p], Wogb[p], ml_win_d, 2048 + h * 128, 128)
                kb.dma("pool", Wo[p][:], ml_wo_d[h * 128:(h + 1) * 128, :], writes=[Wob[p]])

            fr_done = set()
            load_head(0)
            for h in range(8):
                p = h % 2
                if h + 1 < 8:
                    load_head(h + 1)
                for j, (s0, n) in enumerate(SLABS):
                    for (W_, Wb_, dst, dstb, sc) in ((Wq[p], Wqb[p], qT, qTb, 0.125), (Wk[p], Wkb[p], kT, kTb, 1.0)):
                        pb, pbb = banks[6], bankb[6]
                        for c in range(NCH):
                            kb.mm(pb[:64, :n], W_[:, c, :], HT[:, c, s0:s0 + n], c == 0, c == NCH - 1,
                                  reads=[Wb_, HTb[c][j]], writes=[pbb])
                        kb.act(dst[:, s0:s0 + n], pb[:64, :n], AF.Copy, reads=[pbb], writes=[dstb[j]], scale=sc)
                for vi in range(18):
                    off = 128 * vi
                    sl = slabs_of(off, 128)
                    pb, pbb = banks[6], bankb[6]
                    for c in range(NCH):
                        kb.mm(pb[:, 0:128], HT[:, c, off:off + 128], Wv[p][:, c, :], c == 0, c == NCH - 1,
                              reads=[Wvb[p]] + [HTb[c][jj] for jj in sl], writes=[pbb])
                    kb.copy("act", V[:, vi, :], pb[:, 0:128], reads=[pbb], writes=[Vb[vi]])
                for j, (s0, n) in enumerate(SLABS):
                    if j == 0 and not need_ctx:
                        continue
                    pb, pbb = banks[6], bankb[6]
                    for c in range(NCH):
                        kb.mm(pb[:, :n], Wog[p][:, c, :], HT[:, c, s0:s0 + n], c == 0, c == NCH - 1,
                              reads=[Wogb[p], HTb[c][j]], writes=[pbb])
                    kb.act(OG[:, :n], pb[:, :n], AF.Sigmoid, reads=[pbb], writes=[OGb])
                    for d in range(2):
                        def emit_fr(j_, d_):
                            if (h, j_, d_) in fr_done:
                                return
                            fr_done.add((h, j_, d_))
                            s0_, n_ = SLABS[j_]
                            row = d_ * 16 + 8 + h
                            fm, fmb = fm_rot.next()
                            kb.ts("dve", fm[:, :n_], F_[:, s0_:s0_ + n_], ident[0:32, row:row + 1], None, ALU.mult, None,
                                  reads=[Fb[j_], identb], writes=[fmb])
                            kb.mm(banks[d_][:, :n_], ones_f[0:32, :], fm[:, :n_], True, True, reads=[ones_fb, fmb], writes=[bankb[d_]])

                        emit_fr(j, d)
                        if d == 0:
                            emit_fr(j, 1)
                        elif j + 1 < len(SLABS):
                            emit_fr(j + 1, 0)
                        fr, frb = banks[d], bankb[d]
                        nbk, numb = banks[4], bankb[4]
                        dbk, denb = banks[5], bankb[5]
                        tl = tile_list(d, j)
                        SB = (2, 3, 6, 7)
                        pts = {}

                        def front(ti, tl=tl, d=d, fr=fr, frb=frb, pts=pts, s0=s0, n=n, j=j):
                            i, kind, o = tl[ti]
                            sb_, sbb = banks[SB[ti % 4]], bankb[SB[ti % 4]]
                            kb.mm(sb_[:, :n], kT[:, 128 * i:128 * i + 128], qT[:, s0:s0 + n], True, True,
                                  reads=[kTb[x] for x in slabs_of(128 * i, 128)] + [qTb[j]], writes=[sbb])
                            bcol_ = BIAS[:, i, d * 8 + h:d * 8 + h + 1]
                            dt, dtb = dt_rot.next()
                            if kind == "full":
                                kb.act(dt[:, :n], fr[:, :n], AF.Exp, reads=[frb, BIASb], writes=[dtb], bias=bcol_, scale=1.0)
                            else:
                                kb.stt(dt[:, :n], fr[:, :n], bcol_, caps[:, d, o, :n], ALU.add, ALU.min,
                                       reads=[frb, BIASb, capsb], writes=[dtb])
                                kb.act(dt[:, :n], dt[:, :n], AF.Exp, reads=[dtb], writes=[dtb])
                            pt, ptb = pt_rot.next()
                            kb.tt("dve", pt[:, :n], dt[:, :n], sb_[:, :n], ALU.mult, reads=[dtb, sbb], writes=[ptb])
                            pts[ti] = (pt, ptb)

                        def back(ti, tl=tl, pts=pts, n=n, nbk=nbk, dbk=dbk, numb=numb, denb=denb):
                            i, kind, o = tl[ti]
                            pt, ptb = pts.pop(ti)
                            kb.mm(nbk[:, :n], V[:, i, :], pt[:, :n], ti == 0, ti == len(tl) - 1, reads=[Vb[i], ptb], writes=[numb])
                            kb.mm(dbk[:, :n], ones_h[:, :], pt[:, :n], ti == 0, ti == len(tl) - 1, reads=[ones_hb, ptb],
                                  writes=[denb])

                        pipeline(len(tl), front, back, 3)
                        rec, recb = rec_rot.next()
                        kb.ts("dve", rec[:, :n], dbk[:, :n], 1.0, None, ALU.max, None, reads=[denb], writes=[recb])
                        kb.stt(rec[:, :n], dbk[:, :n], -1.0, rec[:, :n], ALU.mult, ALU.max, reads=[denb, recb], writes=[recb])
                        kb.op("dve", lambda g: g.reciprocal(out=rec[:, :n], in_=rec[:, :n]), reads=[recb], writes=[recb])
                        if d == 0:
                            kb.tt("dve", Hs[:, :n], nbk[:, :n], rec[:, :n], ALU.mult, reads=[numb, recb], writes=[Hsb])
                        else:
                            kb.tt("dve", rec[:, :n], nbk[:, :n], rec[:, :n], ALU.mult, reads=[numb, recb], writes=[recb])
                            kb.tt("dve", Hs[:, :n], Hs[:, :n], rec[:, :n], ALU.add, reads=[Hsb, recb], writes=[Hsb])
                    pnorm(nb, Hs[:, :n], Hsb, 128, n, ngc[:, h:h + 1], ngcb, Yn[:, :n], Ynb)
                    kb.tt("dve", Yh[:, :n], Yn[:, :n], OG[:, :n], ALU.mult, reads=[Ynb, OGb], writes=[Yhb])
                    defer(lambda p=p, n=n, j=j: wo_update(
                        Wo[p], Wob[p], [lambda oc: Wo[p][:, oc * 128:(oc + 1) * 128]], [Yh[:, :n]], [Yhb], j))
                run_pending()
            kb.barrier()


    def mixer_gl(l, need_ctx):
        assert not need_ctx
        with contextlib.ExitStack() as st:
            with contextlib.ExitStack() as st2:
                nb = make_norm_bufs(st2)
                for j in range(len(SLABS)):
                    norm_slab(nb, j, 0, 0)
                kb.barrier()
            ZR = [kb.sb(st, [16, T], BF16, "ZR%d" % d) for d in range(2)]
            ZRb = [[Buf() for _ in SLABS] for _ in range(2)]
            WA = kb.sb(st, [16, 2, 512], BF16, "WA")
            WAb = Buf()
            kb.dma("pool", WA[:], gl_wa_d[:, :, :], writes=[WAb])
            NBA = kb.sb(st, [128, 2, 4], F32, "NBA")
            NBAb = Buf()
            kb.dma("sp", NBA[:], gl_ba_d[:, :, :], writes=[NBAb])
            kb.ts("dve", NBA[:], NBA[:], -1.0, None, ALU.mult, None, reads=[NBAb], writes=[NBAb])
            m01 = kb.sb(st, [128, 2, 4, 512], BF16, "m01")
            m01b = Buf()
            kb.dma("pool", m01[:, 0], m01_d[:, 0], writes=[m01b])
            kb.dma("pool", m01[:, 1], m01_d[:, 1], writes=[m01b])
            ngc = kb.sb(st, [128, 8], F32, "ngc")
            ngcb = Buf()
            kb.dma("sp", ngc[:], gl_ng_d[:, :], writes=[ngcb])
            one512 = kb.sb(st, [128, 512], F32, "one512")
            one512b = Buf()
            kb.op("pool", lambda g: g.memset(one512[:], 1.0), writes=[one512b])
            with contextlib.ExitStack() as st2:
                Wz = kb.sb(st2, [128, NCH, 32], BF16, "Wz")
                Wzb = Buf()
                load_cols(Wz, Wzb, gl_win_d, 3072, 32)
                for d in range(2):
                    for j, (s0, n) in enumerate(SLABS):
                        pb, pbb = banks[6], bankb[6]
                        for c in range(NCH):
                            kb.mm(pb[:16, :n], Wz[:, c, 16 * d:16 * d + 16], HT[:, c, s0:s0 + n], c == 0, c == NCH - 1,
                                  reads=[Wzb, HTb[c][j]], writes=[pbb])
                        kb.copy("act", ZR[d][:, s0:s0 + n], pb[:16, :n], reads=[pbb], writes=[ZRb[d][j]])
                kb.barrier()
            Wq = kb.sb(st, [128, NCH, 128], BF16, "Wq")
            Wk = kb.sb(st, [128, NCH, 128], BF16, "Wk")
            Wv = kb.sb(st, [128, NCH, 256], BF16, "Wv")
            Wgt = kb.sb(st, [128, NCH, 256], BF16, "Wgt")
            Wo = kb.sb(st, [128, 2, D], BF16, "Wo")
            Wqb, Wkb, Wvb, Wgtb, Wob = Buf(), Buf(), Buf(), Buf(), Buf()
            Bd = [kb.sb(st, [128, T], F32, "B%d" % d) for d in range(2)]
            Bdb = [[Buf() for _ in SLABS] for _ in range(2)]
            NR = kb.sb(st, [128, 2, 18], F32, "NR")
            NRb = Buf()
            csl = kb.sb(st, [128, 1], F32, "csl")
            cslb = Buf()
            qb = kb.sb(st, [128, T], BF16, "qb")
            kbf = kb.sb(st, [128, T], BF16, "kbf")
            qbb = [Buf() for _ in SLABS]
            kbb = [Buf() for _ in SLABS]
            V = kb.sb(st, [128, 18, 256], BF16, "V")
            Vb = [Buf() for _ in range(18)]
            e2_rot = Rot([(kb.sb(st, [128, 512], F32, "e2"), Buf()) for _ in range(2)])
            SG = kb.sb(st, [128, 512], F32, "SG")
            SGb = Buf()
            qt_rot = Rot([(kb.sb(st, [128, 512], BF16, "qt"), Buf()) for _ in range(2)])
            at_rot = Rot([(kb.sb(st, [128, 512], BF16, "AT"), Buf()) for _ in range(3)])
            e1_rot = Rot([(kb.sb(st, [128, 128], F32, "e1"), Buf()) for _ in range(3)])
            kt_rot = Rot([(kb.sb(st, [128, 128], BF16, "kt"), Buf()) for _ in range(3)])
            sq_rot = Rot([(kb.sb(st, [128, 512], F32, "sq"), Buf()) for _ in range(1)])
            rstd = kb.sb(st, [128, 512], F32, "rstd")
            rstdb = Buf()
            ytmp = kb.sb(st, [128, 512], F32, "ytmp")
            ytmpb = Buf()
            Y = kb.sb(st, [128, 2, 512], BF16, "Y")
            Yb = [Buf(), Buf()]
            (tA, tAb), (tB, tBb) = e2_rot.items[0], e2_rot.items[1]
            tC, tCb = SG, SGb
            for h in range(4):
                load_cols(Wq, Wqb, gl_win_d, h * 128, 128)
                load_cols(Wk, Wkb, gl_win_d, 512 + h * 128, 128)
                load_cols(Wv, Wvb, gl_win_d, 1024 + h * 256, 256)
                load_cols(Wgt, Wgtb, gl_win_d, 2048 + h * 256, 256)
                kb.dma("pool", Wo[:], gl_wo_d[h * 256:(h + 1) * 256, :].rearrange("(v p) n -> p v n", p=128), writes=[Wob])
                for d in range(2):
                    for j, (s0, n) in enumerate(SLABS):
                        pb, pbb = banks[4 + j % 2], bankb[4 + j % 2]
                        kb.mm(pb[:, :n], WA[:, d, h * 128:(h + 1) * 128], ZR[d][:, s0:s0 + n], True, True,
                              reads=[WAb, ZRb[d][j]], writes=[pbb])
                        kb.act(tA[:, :n], pb[:, :n], AF.Exp, reads=[pbb, NBAb], writes=[tAb], bias=NBA[:, d, h:h + 1], scale=-1.0)
                        kb.ts("dve", tA[:, :n], tA[:, :n], 1.0, None, ALU.add, None, reads=[tAb], writes=[tAb])
                        kb.act(tA[:, :n], tA[:, :n], AF.Ln, reads=[tAb], writes=[tAb])
                        kb.ts("dve", tB[:, :n], tA[:, :n], -1.0 / 16.0, None, ALU.mult, None, reads=[tAb], writes=[tBb])
                        if d == 0:
                            init = 0.0 if j == 0 else Bd[0][:, s0 - 1:s0]
                            kb.op("dve", lambda g: g.tensor_tensor_scan(out=Bd[0][:, s0:s0 + n], data0=one512[:, :n], data1=tB[:, :n],
                                                                         initial=init, op0=ALU.mult, op1=ALU.add),
                                  reads=[one512b, tBb] + ([Bdb[0][j - 1]] if j else []), writes=[Bdb[0][j]])
                        else:
                            init = 0.0 if j == 0 else csl[:, 0:1]
                            kb.op("dve", lambda g: g.tensor_tensor_scan(out=tC[:, :n], data0=one512[:, :n], data1=tB[:, :n],
                                                                         initial=init, op0=ALU.mult, op1=ALU.add),
                                  reads=[one512b, tBb, cslb], writes=[tCb])
                            kb.copy("dve", csl[:, 0:1], tC[:, n - 1:n], reads=[tCb], writes=[cslb])
                            kb.tt("dve", Bd[1][:, s0:s0 + n], tB[:, :n], tC[:, :n], ALU.subtract, reads=[tBb, tCb],
                                  writes=[Bdb[1][j]])
                    if d == 1:
                        for j in range(1, 5):
                            s0, n = SLABS[j]
                            kb.ts("dve", Bd[1][:, s0:s0 + n], Bd[1][:, s0:s0 + n], csl[:, 0:1], None, ALU.add, None,
                                  reads=[Bdb[1][j], cslb], writes=[Bdb[1][j]])
                    kb.ts("dve", NR[:, d, :], Bd[d][:, 64:T:128], -1.0, None, ALU.mult, None, reads=Bdb[d], writes=[NRb])
                for j, (s0, n) in enumerate(SLABS):
                    for (W_, Wb_, dst, dstb, sc) in ((Wq, Wqb, qb, qbb, 128.0 ** -0.5), (Wk, Wkb, kbf, kbb, 1.0)):
                        if dst is qb and j == 0:
                            continue
                        pb, pbb = banks[6], bankb[6]
                        for c in range(NCH):
                            kb.mm(pb[:, :n], W_[:, c, :], HT[:, c, s0:s0 + n], c == 0, c == NCH - 1,
                                  reads=[Wb_, HTb[c][j]], writes=[pbb])
                        kb.act(dst[:, s0:s0 + n], pb[:, :n], AF.Copy, reads=[pbb], writes=[dstb[j]], scale=sc)
                for vi in range(18):
                    off = 128 * vi
                    sl = slabs_of(off, 128)
                    pb, pbb = banks[6], bankb[6]
                    for c in range(NCH):
                        kb.mm(pb[:, 0:256], HT[:, c, off:off + 128], Wv[:, c, :], c == 0, c == NCH - 1,
                              reads=[Wvb] + [HTb[c][jj] for jj in sl], writes=[pbb])
                    kb.copy("act", V[:, vi, :], pb[:, 0:256], reads=[pbb], writes=[Vb[vi]])
                for j in range(1, 5):
                    s0, n = SLABS[j]
                    pairs = [(d, i, kind, o) for d in range(2) for (i, kind, o) in tile_list(d, j)]
                    ob = [(banks[2], bankb[2]), (banks[3], bankb[3])]
                    AB = (0, 1, 4, 5)
                    ats = {}

                    def front(idx, pairs=pairs, ats=ats, s0=s0, n=n, j=j):
                        d, i, kind, o = pairs[idx]
                        k0 = 128 * i
                        ksl = slabs_of(k0, 128)
                        e1, e1b = e1_rot.next()
                        kb.act(e1[:, :], Bd[d][:, k0:k0 + 128], AF.Exp, reads=[Bdb[d][x] for x in ksl], writes=[e1b],
                               bias=Bd[d][:, k0 + 64:k0 + 65], scale=-1.0)
                        kt, ktb = kt_rot.next()
                        kb.tt("dve", kt[:, :], kbf[:, k0:k0 + 128], e1[:, :], ALU.mult, reads=[kbb[x] for x in ksl] + [e1b],
                              writes=[ktb])
                        e2, e2b = e2_rot.next()
                        kb.act(e2[:, :n], Bd[d][:, s0:s0 + n], AF.Exp, reads=[Bdb[d][j], NRb], writes=[e2b],
                               bias=NR[:, d, i:i + 1], scale=1.0)
                        qt, qtb = qt_rot.next()
                        kb.tt("pool" if idx % 2 == 0 else "dve", qt[:, :n], qb[:, s0:s0 + n], e2[:, :n], ALU.mult,
                              reads=[qbb[j], e2b], writes=[qtb])
                        ab_, abb = banks[AB[idx % 4]], bankb[AB[idx % 4]]
                        kb.mm(ab_[:, :n], kt[:, :], qt[:, :n], True, True, reads=[ktb, qtb], writes=[abb])
                        at, atb = at_rot.next()
                        if kind == "full":
                            kb.copy("act", at[:, :n], ab_[:, :n], reads=[abb], writes=[atb])
                        else:
                            kb.tt("dve", at[:, :n], ab_[:, :n], m01[:, d, o, :n], ALU.mult, reads=[abb, m01b], writes=[atb])
                        ats[idx] = (at, atb)

                    def back(idx, pairs=pairs, ats=ats, n=n, ob=ob):
                        d, i, kind, o = pairs[idx]
                        at, atb = ats.pop(idx)
                        for v in range(2):
                            kb.mm(ob[v][0][:, :n], V[:, i, v * 128:(v + 1) * 128], at[:, :n], idx == 0, idx == len(pairs) - 1,
                                  reads=[Vb[i], atb], writes=[ob[v][1]])

                    pipeline(len(pairs), front, back, 2)
                    ps_, psb = banks[7], bankb[7]
                    for v in range(2):
                        sq, sqb = sq_rot.next()
                        kb.act(sq[:, :n], ob[v][0][:, :n], AF.Square, reads=[ob[v][1]], writes=[sqb])
                        kb.mm(ps_[:, :n], ones_f[:, :], sq[:, :n], v == 0, v == 1, reads=[ones_fb, sqb], writes=[psb])
                    kb.act(rstd[:, :n], ps_[:, :n], AF.Sqrt, reads=[psb, epsb], writes=[rstdb], bias=epsc[:], scale=1.0 / 256.0)
                    kb.op("dve", lambda g: g.reciprocal(out=rstd[:, :n], in_=rstd[:, :n]), reads=[rstdb], writes=[rstdb])
                    for v in range(2):
                        pb, pbb = banks[6], bankb[6]
                        for c in range(NCH):
                            kb.mm(pb[:, :n], Wgt[:, c, v * 128:(v + 1) * 128], HT[:, c, s0:s0 + n], c == 0, c == NCH - 1,
                                  reads=[Wgtb, HTb[c][j]], writes=[pbb])
                        kb.act(SG[:, :n], pb[:, :n], AF.Silu, reads=[pbb], writes=[SGb])
                        kb.stt(ytmp[:, :n], ob[v][0][:, :n], ngc[:, 2 * h + v:2 * h + v + 1], rstd[:, :n], ALU.mult, ALU.mult,
                               reads=[ob[v][1], ngcb, rstdb], writes=[ytmpb])
                        kb.tt("dve", Y[:, v, :n], ytmp[:, :n], SG[:, :n], ALU.mult, reads=[ytmpb, SGb], writes=[Yb[v]])
                    wo_update(Wo, Wob, [lambda oc: Wo[:, 0, oc * 128:(oc + 1) * 128], lambda oc: Wo[:, 1, oc * 128:(oc + 1) * 128]],
                              [Y[:, 0, :n], Y[:, 1, :n]], Yb, j)
            kb.barrier()

    def mixer_stage(l, need_ctx):
        kind = l % 4
        if kind == 0:
            mixer_na(l, need_ctx)
        elif kind == 3:
            mixer_gl(l, need_ctx)
        elif kind == 1:
            mixer_ml(l, need_ctx)
        elif kind == 2:
            mixer_sw(l, need_ctx)
        else:
            raise NotImplementedError

    cur_mod = None
    for (l, what) in stages:
        if cur_mod != l:
            compute_mod(l)
            cur_mod = l
        last = l == 3
        if what == "mix":
            mixer_stage(l, not last)
        else:
            moe_stage(l, not last)

    yT_v = yT_d.rearrange("(c p) t -> p c t", p=128)
    toks = []
    for c in range(NCH):
        for j, (s0, n) in enumerate(SLABS):
            toks.append(kb.dma("sp", yT_v[:, c, s0:s0 + n], XT[:, c, s0:s0 + n], reads=[XTb[c][j]]))
    for key, val in toks:
        kb._wait("sp", key, val)
    return kb


_PROG_CACHE = {}
RUN_KW = {}
LAST_EXEC_NS = None


def _get_prog(stages):
    key = tuple(stages)
    if key not in _PROG_CACHE:
        _PROG_CACHE[key] = build_program(list(stages))
    return _PROG_CACHE[key]


def _common_inputs(inp):
    f = np.float32
    d = {}
    d["ada_w"] = np.ascontiguousarray(inp["ada_w"], dtype=f)
    d["ada_bT"] = np.ascontiguousarray(inp["ada_b"].reshape(4, 48, 128).transpose(0, 2, 1), dtype=f)
    d["norm_mix_gT"] = np.ascontiguousarray(inp["norm_mix_g"].reshape(4, 8, 128).transpose(2, 0, 1), dtype=f)
    d["norm_ffn_gT"] = np.ascontiguousarray(inp["norm_ffn_g"].reshape(4, 8, 128).transpose(2, 0, 1), dtype=f)
    d["ident"] = np.eye(128, dtype=f)
    d["moe_wr"] = np.ascontiguousarray(np.concatenate([inp["moe_w_grp"], inp["moe_w_exp"]], axis=-1), dtype=f)
    d["moe_br"] = np.ascontiguousarray(np.concatenate([inp["moe_b_grp"], inp["moe_b_exp"]], axis=-1), dtype=f)
    if MOE_SPARSE:
        d["moe_w_gate"] = np.ascontiguousarray(inp["moe_w_gate"].reshape(4, 32, 8, 128, 512).transpose(0, 1, 3, 2, 4), dtype=f).reshape(-1, 2048)
        d["moe_w_up"] = np.ascontiguousarray(inp["moe_w_up"].reshape(4, 32, 8, 128, 512).transpose(0, 1, 3, 2, 4), dtype=f).reshape(-1, 2048)
        d["moe_w_down"] = np.ascontiguousarray(inp["moe_w_down"].reshape(4, 32, 4, 128, 1024).transpose(0, 1, 3, 2, 4), dtype=f).reshape(-1, 2048)
        d["pcol2"] = (2.0 * np.arange(128, dtype=f)).reshape(128, 1)
    else:
        d["moe_w_gate"] = np.ascontiguousarray(inp["moe_w_gate"], dtype=f)
        d["moe_w_up"] = np.ascontiguousarray(inp["moe_w_up"], dtype=f)
        d["moe_w_down"] = np.ascontiguousarray(inp["moe_w_down"], dtype=f)
    d["ecap"] = np.ascontiguousarray(np.broadcast_to((np.arange(32) * CAP).astype(f)[None, :], (128, 32)))
    d["umat"] = np.ascontiguousarray(np.triu(np.ones((128, 128), f), 1))
    d["na_w_qkv"] = np.ascontiguousarray(inp["na_w_qkv"][0], dtype=f)
    d["na_w_o"] = np.ascontiguousarray(inp["na_w_o"][0], dtype=f)
    d["na_qk_gT"] = np.ascontiguousarray(inp["na_qk_g"][0].T, dtype=f)
    ck = np.arange(64)[:, None]
    cq = np.arange(64)[None, :]
    dc = np.clip(ck - cq + 15, 0, 30)
    rpb = inp["na_rpb"][0]
    g = rpb[:, :, dc]
    bt = np.stack([g[:, 0:14], g[:, 1:15]], axis=0)
    d["na_bt"] = np.ascontiguousarray(bt.transpose(0, 3, 1, 2, 4).reshape(128, 8, 14, 64), dtype=f)
    c0 = np.clip(cq - 8, 0, 48)
    ok = (ck >= c0) & (ck < c0 + 16)
    m = np.where(ok, 0.0, NEG).astype(f)
    d["na_mask"] = np.ascontiguousarray(np.concatenate([m, m], axis=0))
    d["ml_w_in"] = np.ascontiguousarray(inp["ml_w_in"][0], dtype=f)
    d["ml_w_o"] = np.ascontiguousarray(inp["ml_w_o"][0], dtype=f)
    d["ml_b_gates"] = np.ascontiguousarray(inp["ml_b_gates"][0].reshape(32, 1), dtype=f)
    d["ml_norm_gT"] = np.ascontiguousarray(inp["ml_norm_g"][0].reshape(8, 128).T, dtype=f)
    cc = np.zeros((32, 4), f)
    for r_ in range(32):
        dd, ff = r_ // 16, (r_ // 8) % 2
        cc[r_, 0] = float(ff)
        cc[r_, 1] = 1.0 if dd == 0 else -1.0
        cc[r_, 2] = 0.0 if dd == 0 else 1.0
        cc[r_, 3] = 0.0 if dd == 0 else 1.0
    d["ml_cc"] = cc
    s_ = np.arange(128)[:, None, None]
    o_ = np.arange(4)[None, :, None]
    t_ = np.arange(512)[None, None, :]
    BIGC = 30000.0
    capc = np.where(t_ >= 128 * o_ + s_, BIGC, -BIGC).astype(f)
    capa = np.where(128 * o_ + s_ >= t_, BIGC, -BIGC).astype(f)
    d["caps"] = np.ascontiguousarray(np.stack([capc, capa], axis=1))
    d["gl_w_in"] = np.ascontiguousarray(inp["gl_w_in"][0], dtype=f)
    d["gl_w_o"] = np.ascontiguousarray(inp["gl_w_o"][0], dtype=f)
    d["gl_wa"] = np.ascontiguousarray(inp["gl_w_a2"][0].transpose(1, 0, 2), dtype=f)
    d["gl_baT"] = np.ascontiguousarray(inp["gl_b_a"][0].reshape(2, 4, 128).transpose(2, 0, 1), dtype=f)
    d["gl_norm_gT"] = np.ascontiguousarray(inp["gl_norm_g"][0].reshape(8, 128).T, dtype=f)
    d["mask01"] = np.ascontiguousarray((d["caps"] > 0).astype(f))
    d["sw_w_qkv"] = np.ascontiguousarray(inp["sw_w_qkv"][0], dtype=f)
    d["sw_w_o"] = np.ascontiguousarray(inp["sw_w_o"][0], dtype=f)
    d["sw_qk_gT"] = np.ascontiguousarray(inp["sw_qk_g"][0].T, dtype=f)
    d["sw_sink"] = np.ascontiguousarray(inp["sw_sink"].reshape(1, 16), dtype=f)
    pos = np.arange(TL)
    rows, cols = pos // 64, pos % 64
    inv = (10000.0 ** (-np.arange(16, dtype=np.float64) / 16))
    ang = np.zeros((64, TL), np.float64)
    for p_ in range(64):
        ang[p_] = (rows if p_ < 32 else cols) * inv[p_ % 16]
    d["sw_cos"] = np.cos(ang).astype(f)
    d["sw_sin"] = np.sin(ang).astype(f)
    R = np.zeros((64, 64), f)
    for p_ in range(64):
        if (p_ % 32) < 16:
            R[p_, p_ + 16] = -1.0
        else:
            R[p_, p_ - 16] = 1.0
    d["sw_rt"] = np.ascontiguousarray(R.T)
    sr = np.arange(128)[:, None]
    tr = np.arange(128)[None, :]
    mp = np.where(sr >= tr, 0.0, NEG).astype(f)
    mn = np.where(sr <= tr, 0.0, NEG).astype(f)
    d["sw_mask"] = np.ascontiguousarray(np.stack([mp, mn], axis=1))
    return d


def run_stages(inp, stages, xT_list, n_cores=8):
    kb = _get_prog(stages)
    common = _common_inputs(inp)
    in_maps = []
    for b in range(n_cores):
        m = dict(common)
        m["xT"] = np.ascontiguousarray(xT_list[b], dtype=np.float32)
        m["cond"] = np.ascontiguousarray(np.stack([inp["c_ctx"], inp["c"][b]], axis=1), dtype=np.float32)
        in_maps.append(m)
    res = run_bass_kernel_spmd(kb.nc, in_maps, core_ids=list(range(n_cores)), **RUN_KW)
    global LAST_EXEC_NS
    LAST_EXEC_NS = getattr(res, "exec_time_ns", None)
    if DEBUG:
        global LAST_RES
        LAST_RES = res.results
    return [np.asarray(r["yT"]) for r in res.results]


ALL_STAGES = [(l, w) for l in range(4) for w in ("mix", "moe")]


def kernel(**inp):
    inp = {k: np.asarray(v) for k, v in inp.items()}
    B = inp["x"].shape[0]
    xT = [np.concatenate([inp["ctx"][b], inp["x"][b]], axis=0).T for b in range(B)]
    yT = run_stages(inp, ALL_STAGES, xT, n_cores=B)
    out = np.stack([y[:, TC:].T for y in yT], axis=0)
    return np.ascontiguousarray(out, dtype=np.float32)
```
